# Optimizing a Trainium2 kernel written in Bass

```python
import math
import jax
import jax.numpy as jnp
from jax import lax
import numpy as np

D_MODEL = 1024
BATCH = 8
SEQ = 8192
DEPTH = 2

GRID_W = 64
CTX_LEN = 256
RMS_EPS = 1e-6

LRU_WIDTH = D_MODEL // 2
LRU_HEADS = 8
LRU_HEAD_DIM = LRU_WIDTH // LRU_HEADS
LRU_CONV = 4
CONV_LEFT = LRU_CONV // 2
CONV_RIGHT = LRU_CONV - 1 - CONV_LEFT
LRU_C = 8.0

S5_WIDTH = D_MODEL // 2
S5_GROUP = 16
S5_GROUPS = S5_WIDTH // S5_GROUP
S5_STATE = 64

REC_IN = 2 * LRU_WIDTH + S5_WIDTH
REC_MIX = LRU_WIDTH + S5_WIDTH

NA_HEADS = 16
NA_HEAD_DIM = D_MODEL // NA_HEADS
NA_KR = 8
NA_KC = 16
NEG_INF = -1e30

MOE_GROUPS = 4
MOE_PER_GROUP = 8
MOE_EXPERTS = MOE_GROUPS * MOE_PER_GROUP
MOE_TOP_K = 2
MOE_HIDDEN = 512
MOE_BLOCK = 1024

kernel_name = 'hybrid_rglru_s5_natten_hmoe_dit'


def _rmsnorm(x, g):
    xf = x.astype(jnp.float32)
    xf = xf * lax.rsqrt(jnp.mean(xf * xf, axis=-1, keepdims=True) + RMS_EPS)
    return xf.astype(x.dtype) * g


def _modulate(h, shift, scale):
    return h * (1 + scale) + shift


def _dwconv(u, w, b):
    out = lax.conv_general_dilated(u, w[:, None, :], window_strides=(1,),
                                   padding=[(CONV_LEFT, CONV_RIGHT)],
                                   dimension_numbers=('NWC', 'WIO', 'NWC'),
                                   feature_group_count=u.shape[-1])
    return out + b


def _real_combine(e1, e2):
    a1, b1 = e1
    a2, b2 = e2
    return a1 * a2, a2 * b1 + b2


def _real_scan(a, b, h0):
    if h0 is not None:
        b = b.at[:, 0].add(a[:, 0] * h0)
    return lax.associative_scan(_real_combine, (a, b), axis=1)[1]


def _cplx_combine(e1, e2):
    ar1, ai1, br1, bi1 = e1
    ar2, ai2, br2, bi2 = e2
    return (ar2 * ar1 - ai2 * ai1, ar2 * ai1 + ai2 * ar1,
            ar2 * br1 - ai2 * bi1 + br2, ar2 * bi1 + ai2 * br1 + bi2)


def _cplx_scan(a_re, a_im, b_re, b_im, h0):
    if h0 is not None:
        h_re, h_im = h0
        b_re = b_re.at[:, 0].add(a_re[:, 0] * h_re - a_im[:, 0] * h_im)
        b_im = b_im.at[:, 0].add(a_re[:, 0] * h_im + a_im[:, 0] * h_re)
    _, _, s_re, s_im = lax.associative_scan(_cplx_combine, (a_re, a_im, b_re, b_im), axis=1)
    return s_re, s_im


def _rglru_dir(u_c, u_l, wa, ba, wx, bx, lam, reverse):
    def coeffs(u):
        uf = u.astype(jnp.float32)
        uh = uf.reshape(uf.shape[:-1] + (LRU_HEADS, LRU_HEAD_DIM))
        r = jax.nn.sigmoid(jnp.einsum('bthi,hij->bthj', uh, wa.astype(jnp.float32)).reshape(uf.shape) + ba)
        i = jax.nn.sigmoid(jnp.einsum('bthi,hij->bthj', uh, wx.astype(jnp.float32)).reshape(uf.shape) + bx)
        log_a = -LRU_C * r * jax.nn.softplus(-lam.astype(jnp.float32))
        return jnp.exp(log_a), jnp.sqrt(-jnp.expm1(2.0 * log_a)) * (i * uf)
    if reverse:
        u_c, u_l = jnp.flip(u_c, 1), jnp.flip(u_l, 1)
    a_c, b_c = coeffs(u_c)
    h_c = _real_scan(a_c, b_c, None)
    a_l, b_l = coeffs(u_l)
    h_l = _real_scan(a_l, b_l, h_c[:, -1])
    if reverse:
        h_c, h_l = jnp.flip(h_c, 1), jnp.flip(h_l, 1)
    return h_c, h_l


def _s5_dir(u_c, u_l, a_re, a_im, log_dt, b_re, b_im, c_re, c_im, reverse):
    a_re = a_re.astype(jnp.float32)
    a_im = a_im.astype(jnp.float32)
    dt = jnp.exp(log_dt.astype(jnp.float32))[:, None]
    mag = jnp.exp(a_re * dt)
    lb_re = mag * jnp.cos(a_im * dt)
    lb_im = mag * jnp.sin(a_im * dt)
    den = a_re * a_re + a_im * a_im
    q_re = ((lb_re - 1.0) * a_re + lb_im * a_im) / den
    q_im = (lb_im * a_re - (lb_re - 1.0) * a_im) / den
    b_re = b_re.astype(jnp.float32)
    b_im = b_im.astype(jnp.float32)
    bb_re = q_re[..., None] * b_re - q_im[..., None] * b_im
    bb_im = q_re[..., None] * b_im + q_im[..., None] * b_re
    c_re = c_re.astype(jnp.float32)
    c_im = c_im.astype(jnp.float32)

    def run(u, h0):
        t = u.shape[1]
        d_re = jnp.einsum('btgp,gnp->btgn', u, bb_re)
        d_im = jnp.einsum('btgp,gnp->btgn', u, bb_im)
        a_r = jnp.broadcast_to(lb_re, (1, t) + lb_re.shape)
        a_i = jnp.broadcast_to(lb_im, (1, t) + lb_im.shape)
        return _cplx_scan(a_r, a_i, d_re, d_im, h0)

    def readout(h_re, h_im):
        y = jnp.einsum('gpn,btgn->btgp', c_re, h_re) - jnp.einsum('gpn,btgn->btgp', c_im, h_im)
        return jnp.flip(y, 1) if reverse else y

    if reverse:
        u_c, u_l = jnp.flip(u_c, 1), jnp.flip(u_l, 1)
    hc_re, hc_im = run(u_c, None)
    hl_re, hl_im = run(u_l, (hc_re[:, -1], hc_im[:, -1]))
    return readout(hc_re, hc_im), readout(hl_re, hl_im)


def _recurrent_mixer(h_l, h_c, need_ctx, w_in, conv_w, conv_b, lru_wa, lru_ba, lru_wx, lru_bx,
                     lru_lambda, s5_a_re, s5_a_im, s5_log_dt, s5_b_re, s5_b_im, s5_c_re, s5_c_im,
                     s5_d, s5_glu_w, s5_glu_b, w_out):
    xa_l, ga_l, ub_l = jnp.split(h_l @ w_in, [LRU_WIDTH, 2 * LRU_WIDTH], axis=-1)
    xa_c, ga_c, ub_c = jnp.split(h_c @ w_in, [LRU_WIDTH, 2 * LRU_WIDTH], axis=-1)
    xa_l = _dwconv(xa_l, conv_w, conv_b)
    xa_c = _dwconv(xa_c, conv_w, conv_b)
    hf_c, hf_l = _rglru_dir(xa_c, xa_l, lru_wa[0], lru_ba[0], lru_wx[0], lru_bx[0], lru_lambda[0], False)
    hb_c, hb_l = _rglru_dir(xa_c, xa_l, lru_wa[1], lru_ba[1], lru_wx[1], lru_bx[1], lru_lambda[1], True)
    def groups(u):
        return u.astype(jnp.float32).reshape(u.shape[:-1] + (S5_GROUPS, S5_GROUP))
    u_c, u_l = groups(ub_c), groups(ub_l)
    sf_c, sf_l = _s5_dir(u_c, u_l, s5_a_re[0], s5_a_im[0], s5_log_dt[0], s5_b_re[0], s5_b_im[0],
                         s5_c_re[0], s5_c_im[0], False)
    sb_c, sb_l = _s5_dir(u_c, u_l, s5_a_re[1], s5_a_im[1], s5_log_dt[1], s5_b_re[1], s5_b_im[1],
                         s5_c_re[1], s5_c_im[1], True)

    def merge(ga, h_f, h_b, ub, s_f, s_b):
        y_a = (h_f + h_b).astype(ga.dtype) * jax.nn.gelu(ga)
        y_s = (s_f + s_b).reshape(ub.shape).astype(ub.dtype) + s5_d * ub
        y_s = jax.nn.gelu(y_s)
        y_s = y_s * jax.nn.sigmoid(y_s @ s5_glu_w + s5_glu_b)
        return jnp.concatenate([y_a, y_s], axis=-1) @ w_out

    y_l = merge(ga_l, hf_l, hb_l, ub_l, sf_l, sb_l)
    y_c = merge(ga_c, hf_c, hb_c, ub_c, sf_c, sb_c) if need_ctx else None
    return y_l, y_c


def _na_mixer(h_l, h_c, need_ctx, w_qkv, w_out, rpb):
    bsz, seq_len, _ = h_l.shape
    rows = seq_len // GRID_W
    kr = min(NA_KR, rows)
    scale = NA_HEAD_DIM ** -0.5
    grid = (bsz, rows, GRID_W, NA_HEADS, NA_HEAD_DIM)
    q, k, v = jnp.split(h_l @ w_qkv, 3, axis=-1)
    q = (q * scale).reshape(grid)
    k = k.reshape(grid)
    v = v.reshape(grid)
    ctx_shape = (bsz, h_c.shape[1], NA_HEADS, NA_HEAD_DIM)
    k_c, v_c = jnp.split(h_c @ w_qkv[:, D_MODEL:], 2, axis=-1)
    k_c = k_c.reshape(ctx_shape)
    v_c = v_c.reshape(ctx_shape)
    col = jnp.arange(GRID_W)
    c_start = jnp.clip(col - NA_KC // 2, 0, GRID_W - NA_KC)
    col_ok = (col[None, :] >= c_start[:, None]) & (col[None, :] < c_start[:, None] + NA_KC)
    dc_idx = jnp.clip(col[None, :] - col[:, None] + NA_KC - 1, 0, 2 * NA_KC - 2)
    rpb_cols = rpb[:, :, dc_idx].astype(jnp.float32)
    n_loc = kr * GRID_W

    def row_block(args):
        r, q_r = args
        r_start = jnp.clip(r - kr // 2, 0, rows - kr)
        k_b = lax.dynamic_slice_in_dim(k, r_start, kr, axis=1)
        v_b = lax.dynamic_slice_in_dim(v, r_start, kr, axis=1)
        dr_idx = r_start + jnp.arange(kr) - r + NA_KR - 1
        bias = jnp.transpose(jnp.take(rpb_cols, dr_idx, axis=1), (0, 2, 1, 3))
        s_loc = jnp.einsum('bqhd,bkwhd->bhqkw', q_r, k_b).astype(jnp.float32) + bias
        s_loc = jnp.where(col_ok[:, None, :], s_loc, NEG_INF)
        s_ctx = jnp.einsum('bqhd,bchd->bhqc', q_r, k_c).astype(jnp.float32)
        s = jnp.concatenate([s_loc.reshape(bsz, NA_HEADS, GRID_W, n_loc), s_ctx], axis=-1)
        p = jax.nn.softmax(s, axis=-1).astype(v.dtype)
        p_loc = p[..., :n_loc].reshape(bsz, NA_HEADS, GRID_W, kr, GRID_W)
        return (jnp.einsum('bhqkw,bkwhd->bqhd', p_loc, v_b)
                + jnp.einsum('bhqc,bchd->bqhd', p[..., n_loc:], v_c))

    o = lax.map(row_block, (jnp.arange(rows), jnp.moveaxis(q, 1, 0)))
    o = jnp.moveaxis(o, 0, 1).reshape(bsz, seq_len, D_MODEL)
    y_l = o @ w_out
    y_c = None
    if need_ctx:
        q_c = (h_c @ w_qkv[:, :D_MODEL]).reshape(ctx_shape) * scale
        p_c = jax.nn.softmax(jnp.einsum('bqhd,bkhd->bhqk', q_c, k_c).astype(jnp.float32), axis=-1)
        o_c = jnp.einsum('bhqk,bkhd->bqhd', p_c.astype(v_c.dtype), v_c)
        y_c = o_c.reshape(bsz, h_c.shape[1], D_MODEL) @ w_out
    return y_l, y_c


def _moe(h, r1_w, r1_b, r2_w, r2_b, w_gate, w_up, w_down):
    tokens = h.reshape(-1, D_MODEL)
    blk = math.gcd(tokens.shape[0], MOE_BLOCK)

    def block(tb):
        p1 = jax.nn.softmax((tb @ r1_w + r1_b).astype(jnp.float32), axis=-1)
        g_val, g_idx = lax.top_k(p1, 1)
        l2 = (jnp.einsum('nd,gde->nge', tb, r2_w) + r2_b).astype(jnp.float32)
        l2 = jnp.einsum('nge,ng->ne', l2, jax.nn.one_hot(g_idx[:, 0], MOE_GROUPS, dtype=jnp.float32))
        e_val, e_idx = lax.top_k(l2, MOE_TOP_K)
        w = jax.nn.softmax(e_val, axis=-1) * g_val
        eid = g_idx * MOE_PER_GROUP + e_idx
        gates = jnp.einsum('nk,nke->ne', w, jax.nn.one_hot(eid, MOE_EXPERTS, dtype=jnp.float32))
        hid = jax.nn.silu(jnp.einsum('nd,edf->nef', tb, w_gate)) * jnp.einsum('nd,edf->nef', tb, w_up)
        return jnp.einsum('nef,efd->nd', hid * gates.astype(hid.dtype)[:, :, None], w_down)

    return lax.map(block, tokens.reshape(-1, blk, D_MODEL)).reshape(h.shape)


def setup_inputs(seed: int = 0) -> dict:
    key = jax.random.key(seed)
    keys = jax.random.split(key, 40)
    f32 = jnp.float32
    n_even = (DEPTH + 1) // 2
    n_odd = DEPTH // 2

    def nrm(i, shape, scale):
        return jax.random.normal(keys[i], shape, f32) * scale

    u = jax.random.uniform(keys[15], (n_even, 2, LRU_WIDTH), f32, 0.9, 0.999)
    a = u ** (1.0 / LRU_C)
    lru_lambda = jnp.log(a) - jnp.log1p(-a)
    s5_shape = (n_even, 2, S5_GROUPS, S5_STATE)
    return {
        'x': nrm(0, (BATCH, SEQ, D_MODEL), 1.0),
        'c': nrm(1, (BATCH, D_MODEL), 1.0),
        'ctx': nrm(2, (BATCH, CTX_LEN, D_MODEL), 1.0),
        'c_ctx': nrm(3, (D_MODEL,), 1.0),
        'ada_w': nrm(4, (DEPTH, D_MODEL, 6 * D_MODEL), 0.3 * D_MODEL ** -0.5),
        'ada_b': nrm(5, (DEPTH, 6 * D_MODEL), 0.02),
        'norm1_g': 1.0 + nrm(6, (DEPTH, D_MODEL), 0.02),
        'norm2_g': 1.0 + nrm(7, (DEPTH, D_MODEL), 0.02),
        'rec_w_in': nrm(8, (n_even, D_MODEL, REC_IN), D_MODEL ** -0.5),
        'rec_conv_w': nrm(9, (n_even, LRU_CONV, LRU_WIDTH), 0.5),
        'rec_conv_b': nrm(10, (n_even, LRU_WIDTH), 0.02),
        'lru_wa': nrm(11, (n_even, 2, LRU_HEADS, LRU_HEAD_DIM, LRU_HEAD_DIM), LRU_HEAD_DIM ** -0.5),
        'lru_ba': nrm(12, (n_even, 2, LRU_WIDTH), 0.02),
        'lru_wx': nrm(13, (n_even, 2, LRU_HEADS, LRU_HEAD_DIM, LRU_HEAD_DIM), LRU_HEAD_DIM ** -0.5),
        'lru_bx': nrm(14, (n_even, 2, LRU_WIDTH), 0.02),
        'lru_lambda': lru_lambda,
        's5_a_re': -0.5 + nrm(16, s5_shape, 0.01),
        's5_a_im': jnp.pi * jnp.arange(S5_STATE, dtype=f32) + nrm(17, s5_shape, 0.01),
        's5_log_dt': jax.random.uniform(keys[18], (n_even, 2, S5_GROUPS), f32, math.log(1e-3), math.log(1e-1)),
        's5_b_re': nrm(19, (n_even, 2, S5_GROUPS, S5_STATE, S5_GROUP), (2 * S5_GROUP) ** -0.5),
        's5_b_im': nrm(20, (n_even, 2, S5_GROUPS, S5_STATE, S5_GROUP), (2 * S5_GROUP) ** -0.5),
        's5_c_re': nrm(21, (n_even, 2, S5_GROUPS, S5_GROUP, S5_STATE), S5_STATE ** -0.5),
        's5_c_im': nrm(22, (n_even, 2, S5_GROUPS, S5_GROUP, S5_STATE), S5_STATE ** -0.5),
        's5_d': nrm(23, (n_even, S5_WIDTH), 0.5),
        's5_glu_w': nrm(24, (n_even, S5_WIDTH, S5_WIDTH), S5_WIDTH ** -0.5),
        's5_glu_b': nrm(25, (n_even, S5_WIDTH), 0.02),
        'rec_w_out': nrm(26, (n_even, REC_MIX, D_MODEL), REC_MIX ** -0.5),
        'na_w_qkv': nrm(27, (n_odd, D_MODEL, 3 * D_MODEL), D_MODEL ** -0.5),
        'na_w_out': nrm(28, (n_odd, D_MODEL, D_MODEL), D_MODEL ** -0.5),
        'na_rpb': nrm(29, (n_odd, NA_HEADS, 2 * NA_KR - 1, 2 * NA_KC - 1), 0.1),
        'moe_r1_w': nrm(30, (DEPTH, D_MODEL, MOE_GROUPS), D_MODEL ** -0.5),
        'moe_r1_b': nrm(31, (DEPTH, MOE_GROUPS), 0.01),
        'moe_r2_w': nrm(32, (DEPTH, MOE_GROUPS, D_MODEL, MOE_PER_GROUP), D_MODEL ** -0.5),
        'moe_r2_b': nrm(33, (DEPTH, MOE_GROUPS, MOE_PER_GROUP), 0.01),
        'moe_w_gate': nrm(34, (DEPTH, MOE_EXPERTS, D_MODEL, MOE_HIDDEN), D_MODEL ** -0.5),
        'moe_w_up': nrm(35, (DEPTH, MOE_EXPERTS, D_MODEL, MOE_HIDDEN), D_MODEL ** -0.5),
        'moe_w_down': nrm(36, (DEPTH, MOE_EXPERTS, MOE_HIDDEN, D_MODEL), MOE_HIDDEN ** -0.5),
        'final_norm_g': 1.0 + nrm(37, (D_MODEL,), 0.02),
    }


def reference(x, c, ctx, c_ctx, ada_w, ada_b, norm1_g, norm2_g, rec_w_in, rec_conv_w, rec_conv_b,
              lru_wa, lru_ba, lru_wx, lru_bx, lru_lambda, s5_a_re, s5_a_im, s5_log_dt, s5_b_re,
              s5_b_im, s5_c_re, s5_c_im, s5_d, s5_glu_w, s5_glu_b, rec_w_out, na_w_qkv, na_w_out,
              na_rpb, moe_r1_w, moe_r1_b, moe_r2_w, moe_r2_b, moe_w_gate, moe_w_up, moe_w_down,
              final_norm_g):
    silu_c = jax.nn.silu(c)
    silu_cc = jax.nn.silu(c_ctx)
    x_l, x_c = x, ctx
    for layer in range(DEPTH):
        need_ctx = layer < DEPTH - 1
        i = layer // 2
        mod_l = [m[:, None, :] for m in jnp.split(silu_c @ ada_w[layer] + ada_b[layer], 6, axis=-1)]
        mod_c = jnp.split(silu_cc @ ada_w[layer] + ada_b[layer], 6, axis=-1)
        h_l = _modulate(_rmsnorm(x_l, norm1_g[layer]), mod_l[0], mod_l[1])
        h_c = _modulate(_rmsnorm(x_c, norm1_g[layer]), mod_c[0], mod_c[1])
        if layer % 2 == 0:
            y_l, y_c = _recurrent_mixer(h_l, h_c, need_ctx, rec_w_in[i], rec_conv_w[i], rec_conv_b[i],
                                        lru_wa[i], lru_ba[i], lru_wx[i], lru_bx[i], lru_lambda[i],
                                        s5_a_re[i], s5_a_im[i], s5_log_dt[i], s5_b_re[i], s5_b_im[i],
                                        s5_c_re[i], s5_c_im[i], s5_d[i], s5_glu_w[i], s5_glu_b[i],
                                        rec_w_out[i])
        else:
            y_l, y_c = _na_mixer(h_l, h_c, need_ctx, na_w_qkv[i], na_w_out[i], na_rpb[i])
        moe_args = (moe_r1_w[layer], moe_r1_b[layer], moe_r2_w[layer], moe_r2_b[layer],
                    moe_w_gate[layer], moe_w_up[layer], moe_w_down[layer])
        x_l = x_l + mod_l[2] * y_l
        x_l = x_l + mod_l[5] * _moe(_modulate(_rmsnorm(x_l, norm2_g[layer]), mod_l[3], mod_l[4]), *moe_args)
        if need_ctx:
            x_c = x_c + mod_c[2] * y_c
            x_c = x_c + mod_c[5] * _moe(_modulate(_rmsnorm(x_c, norm2_g[layer]), mod_c[3], mod_c[4]), *moe_args)
    return _rmsnorm(x_l, final_norm_g)
```

```python
import math
import os
import numpy as np
import concourse.bass as bass
import concourse.mybir as mybir
from concourse.bass_utils import run_bass_kernel_spmd

F32 = mybir.dt.float32
BF16 = mybir.dt.bfloat16
I32 = mybir.dt.int32
AF = mybir.ActivationFunctionType
ALU = mybir.AluOpType
AX = mybir.AxisListType
SEM_ROT = 30000
D = 1024
CT = 256
LCH = 128


class Buf:
    __slots__ = ("name", "w", "r", "dsem", "dcnt", "gen")

    def __init__(self, name):
        self.name = name
        self.w = None
        self.r = []
        self.dsem = None
        self.dcnt = 0
        self.gen = -1


class Sched:
    def __init__(self, nc):
        self.nc = nc
        self.eobj = {"pe": nc.tensor, "act": nc.scalar, "dve": nc.vector, "pool": nc.gpsimd, "sp": nc.sync}
        self.prog = {k: [] for k in self.eobj}
        self.sem = {}
        self.cnt = {}
        self.seen = {k: {} for k in self.eobj}
        self.nsem = 0
        self.dsemval = {}
        self.dsems = []
        self.allsems = []
        self.freed = []
        self.freed_sw = []
        self.semsw = {}
        self.gen = 0
        for k in self.eobj:
            self._newsem(k)
        self.ninstr = {k: 0 for k in self.eobj}

    def _alloc_sem(self, name):
        self.nsem += 1
        s = self.nc.alloc_semaphore(name=name)
        return s

    def _newsem(self, k):
        self.sem[k] = self._alloc_sem(f"e_{k}_{self.nsem}")
        self.cnt[k] = 0
        self.allsems.append((k, self.sem[k]))

    def _wait(self, eng, ev):
        sem, val = ev
        sid = id(sem)
        if sid in self.dsemval:
            val = self.dsemval[sid]
        if self.seen[eng].get(sid, 0) >= val:
            return
        self.seen[eng][sid] = val
        self.prog[eng].append(lambda e, sem=sem, val=val: e.wait_ge(sem, val))

    def op(self, eng, fn, reads=(), writes=()):
        my = self.sem[eng]
        pe = eng == "pe"
        for b in reads:
            if b.w is not None and not (pe and b.w[0] is my):
                self._wait(eng, b.w)
        for b in writes:
            if b.w is not None and not (pe and b.w[0] is my):
                self._wait(eng, b.w)
            for ev in b.r:
                if not (pe and ev[0] is my):
                    self._wait(eng, ev)
        if self.cnt[eng] >= SEM_ROT:
            self._newsem(eng)
            my = self.sem[eng]
        self.cnt[eng] += 1
        ev = (my, self.cnt[eng])
        self.prog[eng].append(lambda e, fn=fn, my=my: fn(e).then_inc(my, 1))
        self.ninstr[eng] += 1
        for b in writes:
            b.w = ev
            b.r = []
        for b in reads:
            if b not in writes:
                b.r.append(ev)
                if len(b.r) > 16:
                    b.r = b.r[-16:]
        return ev

    def dma(self, q, out_ap, in_ap, dst, src, **kw):
        b = dst if dst is not None else src
        sw = q == "pool"
        if b.dsem is None or b.gen != self.gen or b.dcnt * 16 >= SEM_ROT or self.semsw.get(id(b.dsem)) != sw:
            pool_ = self.freed_sw if sw else self.freed
            while pool_ and pool_[-1][1] * 16 >= SEM_ROT - 4000:
                pool_.pop()
            if pool_:
                b.dsem, b.dcnt = pool_.pop()
            else:
                b.dsem = self._alloc_sem(f"d_{b.name}_{self.nsem}")
                b.dcnt = 0
            self.semsw[id(b.dsem)] = sw
            b.gen = self.gen
            self.dsems.append(b.dsem)
        if src is not None and src.w is not None:
            self._wait(q, src.w)
        if dst is not None:
            if dst.w is not None:
                self._wait(q, dst.w)
            for ev in dst.r:
                self._wait(q, ev)
        b.dcnt += 1
        sem = b.dsem
        ev = (sem, b.dcnt * 16)
        self.dsemval[id(sem)] = b.dcnt * 16
        self.prog[q].append(
            lambda e, o=out_ap, i=in_ap, sem=sem, kw=kw: e.dma_start(out=o, in_=i, **kw).then_inc(sem, 16))
        self.ninstr[q] += 1
        if dst is not None:
            dst.w = ev
            dst.r = []
        if src is not None:
            src.r.append(ev)
        return ev

    def barrier(self):
        evs = [(self.sem[k], self.cnt[k]) for k in self.eobj if self.cnt[k] > 0]
        devs = [(s, self.dsemval[id(s)]) for s in self.dsems]
        for e in self.eobj:
            for ev in evs:
                if ev[0] is self.sem[e] and e == "pe":
                    continue
                self._wait(e, ev)
            for ev in devs:
                self._wait(e, ev)
        for sm_ in self.dsems:
            (self.freed_sw if self.semsw[id(sm_)] else self.freed).append((sm_, self.dsemval[id(sm_)] // 16))
        self.dsems = []
        self.gen += 1

    def finish(self):
        self.barrier()
        nc = self.nc
        prog = self.prog
        with nc.Block() as block:
            @block.sync
            def _(e):
                for t in prog["sp"]:
                    t(e)

            @block.tensor
            def _(e):
                for t in prog["pe"]:
                    t(e)

            @block.scalar
            def _(e):
                for t in prog["act"]:
                    t(e)

            @block.vector
            def _(e):
                for t in prog["dve"]:
                    t(e)

            @block.gpsimd
            def _(e):
                for t in prog["pool"]:
                    t(e)


def rv(ap):
    a = [list(x) for x in ap.ap]
    st, n = a[-1]
    a[-1] = [-st, n]
    return bass.AP(ap.tensor, ap.offset + st * (n - 1), a)


class K:
    def __init__(self, T, dbg=()):
        self.T = T
        self.TT = T + CT
        self.NT = self.TT // 128
        self.dbg = set(dbg)
        self.nc = bass.Bass("TRN2", target_bir_lowering=False)
        self.S = Sched(self.nc)
        self.uid = 0
        self.inp = {}

    def din(self, name, shape):
        t = self.nc.dram_tensor(name, list(shape), F32, kind="ExternalInput").ap()
        self.inp[name] = t
        return t

    def dscr(self, name, shape, dt=F32):
        kind = "ExternalOutput" if name in self.dbg else "Internal"
        return self.nc.dram_tensor(name, list(shape), dt, kind=kind).ap(), Buf(name)

    def sb(self, st, name, shape, dt=F32):
        self.uid += 1
        t = st.enter_context(self.nc.sbuf_tensor(f"{name}_{self.uid}", list(shape), dt))
        return t.ap(), Buf(name)

    def act(self, out, in_, func, r, w, scale=None, bias=None, accum=None):
        kw = {}
        if scale is not None:
            kw["scale"] = scale
        if bias is not None:
            kw["bias"] = bias
        if accum is not None:
            kw["accum_out"] = accum
        return self.S.op("act", lambda e: e.activation(out=out, in_=in_, func=func, **kw), r, w)

    def tt(self, out, a, b, op, r, w, eng="dve"):
        return self.S.op(eng, lambda e: e.tensor_tensor(out=out, in0=a, in1=b, op=op), r, w)

    def ts(self, out, a, s1, op0, r, w, s2=None, op1=None, eng="dve"):
        if op1 is None:
            return self.S.op(eng, lambda e: e.tensor_scalar(out=out, in0=a, scalar1=s1, scalar2=None, op0=op0), r, w)
        return self.S.op(eng, lambda e: e.tensor_scalar(out=out, in0=a, scalar1=s1, scalar2=s2, op0=op0, op1=op1), r, w)

    def stt(self, out, a, s, b, op0, op1, r, w):
        return self.S.op("dve", lambda e: e.scalar_tensor_tensor(out=out, in0=a, scalar=s, in1=b, op0=op0, op1=op1), r, w)

    def cp(self, out, in_, r, w, eng="dve"):
        if eng == "act":
            return self.S.op("act", lambda e: e.copy(out=out, in_=in_), r, w)
        return self.S.op(eng, lambda e: e.tensor_copy(out=out, in_=in_), r, w)

    def ms(self, ap, val, w, eng="pool"):
        return self.S.op(eng, lambda e: e.memset(ap, val), (), w)

    def mm(self, out, lhsT, rhs, start, stop, r, w, skip=False):
        return self.S.op("pe", lambda e: e.matmul(out, lhsT, rhs, start=start, stop=stop, skip_group_check=skip), r, w)

    def tr(self, out, in_, ident, r, w):
        return self.S.op("pe", lambda e: e.transpose(out, in_, ident), r, w)

    def scan(self, out, d0, d1, init, r, w):
        return self.S.op("dve", lambda e: e.tensor_tensor_scan(out=out, data0=d0, data1=d1, initial=init,
                                                               op0=ALU.mult, op1=ALU.add), r, w)

    def dma(self, q, out, in_, dst, src, **kw):
        return self.S.dma(q, out, in_, dst, src, **kw)


def build(T=8192, dbg=(), stop_after=99):
    from contextlib import ExitStack
    k = K(T, dbg)
    nc, S = k.nc, k.S
    TT, NT = k.TT, k.NT
    x = k.din("x", [T, D]); c = k.din("c", [D]); ctx = k.din("ctx", [CT, D]); c_ctx = k.din("c_ctx", [D])
    ada_w = k.din("ada_w", [2, D, 6 * D]); ada_b = k.din("ada_b", [2, 6 * D])
    norm1_g = k.din("norm1_g", [2, D]); norm2_g = k.din("norm2_g", [2, D])
    rec_w_in = k.din("rec_w_in", [D, 1536]); rec_conv_w = k.din("rec_conv_w", [4, 512]); rec_conv_b = k.din("rec_conv_b", [512])
    lru_wa = k.din("lru_wa", [2, 8, 64, 64]); lru_ba = k.din("lru_ba", [2, 512])
    lru_wx = k.din("lru_wx", [2, 8, 64, 64]); lru_bx = k.din("lru_bx", [2, 512]); lru_lambda = k.din("lru_lambda", [2, 512])
    s5_a_re = k.din("s5_a_re", [2, 32, 64]); s5_a_im = k.din("s5_a_im", [2, 32, 64]); s5_log_dt = k.din("s5_log_dt", [2, 32])
    s5_b_re = k.din("s5_b_re", [2, 32, 64, 16]); s5_b_im = k.din("s5_b_im", [2, 32, 64, 16])
    s5_c_re = k.din("s5_c_re", [2, 32, 16, 64]); s5_c_im = k.din("s5_c_im", [2, 32, 16, 64])
    s5_d = k.din("s5_d", [512]); s5_glu_w = k.din("s5_glu_w", [512, 512]); s5_glu_b = k.din("s5_glu_b", [512])
    rec_w_out = k.din("rec_w_out", [D, D])
    na_w_qkv = k.din("na_w_qkv", [D, 3 * D]); na_w_out = k.din("na_w_out", [D, D]); na_rpb = k.din("na_rpb", [16, 15, 31])
    moe_r1_w = k.din("moe_r1_w", [2, D, 4]); moe_r1_b = k.din("moe_r1_b", [2, 4])
    moe_r2_w = k.din("moe_r2_w", [2, 4, D, 8]); moe_r2_b = k.din("moe_r2_b", [2, 4, 8])
    moe_w_gate = k.din("moe_w_gate", [2, 32, D, 512]); moe_w_up = k.din("moe_w_up", [2, 32, D, 512])
    moe_w_down = k.din("moe_w_down", [2, 32, 512, D]); final_norm_g = k.din("final_norm_g", [D])
    out = nc.dram_tensor("out", [T, D], F32, kind="ExternalOutput").ap()
    obuf = Buf("out")

    modv, modvB = k.dscr("modv", [2, 2, 6 * D])
    projT, projTB = k.dscr("projT", [1536, TT])
    ymixT, ymixTB = k.dscr("ymixT", [512, TT])
    ysgT, ysgTB = k.dscr("ysgT", [512, TT])
    x1, x1B = k.dscr("x1", [TT, D])
    x2, x2B = k.dscr("x2", [TT, D])
    x3, x3B = k.dscr("x3", [T, D])

    gst = ExitStack()
    psb = []
    for i in range(8):
        t = gst.enter_context(nc.psum_tensor(f"psb{i}", [128, 512], F32))
        psb.append((t.ap(), Buf(f"psb{i}")))
    ident, identB = k.sb(gst, "ident", [128, 128])
    epsb, epsB = k.sb(gst, "epsb", [128, 1])
    halfpi, halfpiB = k.sb(gst, "halfpi", [128, 1])
    k.ms(ident, 0.0, [identB])
    S.op("pool", lambda e: e.memset(epsb, 1e-6), (), [epsB])
    S.op("pool", lambda e: e.memset(halfpi, math.pi / 2), (), [halfpiB])
    ones_t, onesB = k.sb(gst, "ones", [128, 128])
    k.ms(ones_t, 1.0, [onesB])
    S.op("pool", lambda e: e.affine_select(out=ident, in_=ones_t, pattern=[[-1, 128]], compare_op=ALU.is_equal,
                                            fill=0.0, base=0, channel_multiplier=1), [onesB], [identB])

    def xsrc(ti):
        if ti < 2:
            return ctx[ti * 128:(ti + 1) * 128, :], 1
        return x[(ti - 2) * 128:(ti - 1) * 128, :], 0

    with ExitStack() as st:
        cs, csB = k.sb(st, "cs", [128, 2, 8])
        srep, srepB = k.sb(st, "srep", [128, 2, 8, 128])
        k.dma("sp", cs[:, 0, :], c.rearrange("(kc p) -> p kc", p=128), csB, None, allow_slow_non_contiguous=True)
        k.dma("sp", cs[:, 1, :], c_ctx.rearrange("(kc p) -> p kc", p=128), csB, None, allow_slow_non_contiguous=True)
        k.act(cs, cs, AF.Silu, [csB], [csB])
        k.cp(srep, cs.unsqueeze(3).broadcast_to([128, 2, 8, 128]), [csB], [srepB])
        adab, adabB = k.sb(st, "adab", [128, 6 * D])
        modt = [k.sb(st, f"modt{i}", [128, 6 * D]) for i in range(2)]
        gb = [k.sb(st, f"gb{i}", [128, D]) for i in range(2)]
        wblk = [k.sb(st, f"wblk{i}", [128, 8, 512]) for i in range(2)]
        for layer in range(2):
            k.dma("sp", adab, ada_b[layer, :].partition_broadcast(128), adabB, None)
            k.dma("sp", gb[0][0], norm1_g[layer, :].partition_broadcast(128), gb[0][1], None)
            k.dma("sp", gb[1][0], norm2_g[layer, :].partition_broadcast(128), gb[1][1], None)
            for nb in range(12):
                wt, wB = wblk[nb % 2]
                k.dma("sp", wt, ada_w[layer, :, nb * 512:(nb + 1) * 512].rearrange("(kc p) n -> p kc n", p=128), wB, None)
                for kind in range(2):
                    ps, psB = psb[(nb * 2 + kind) % 4]
                    for kc in range(8):
                        k.mm(ps, srep[:, kind, kc, :], wt[:, kc, :], kc == 0, kc == 7, [srepB, wB], [psB])
                    k.tt(modt[kind][0][:, nb * 512:(nb + 1) * 512], ps, adab[:, nb * 512:(nb + 1) * 512], ALU.add,
                         [psB, adabB], [modt[kind][1]])
            for kind in range(2):
                mt, mB = modt[kind]
                k.stt(mt[:, D:2 * D], mt[:, D:2 * D], 1.0, gb[0][0], ALU.add, ALU.mult, [mB, gb[0][1]], [mB])
                k.stt(mt[:, 4 * D:5 * D], mt[:, 4 * D:5 * D], 1.0, gb[1][0], ALU.add, ALU.mult, [mB, gb[1][1]], [mB])
                k.dma("sp", modv[layer, kind, :].rearrange("(o n) -> o n", o=1), mt[0:1, :], modvB, mB)
    S.barrier()

    def load_mod(st, q, layer, kind, slot, name):
        t, B = k.sb(st, name, [128, D])
        k.dma(q, t, modv[layer, kind, slot * D:(slot + 1) * D].partition_broadcast(128), B, modvB)
        return t, B

    def norm_mod(xt, xB, ht, hB, At, AB, Bt, BB, tmp):
        jt, jB, ss, ssB = tmp
        k.act(jt, xt, AF.Square, [xB], [jB, ssB], accum=ss)
        k.act(ss, ss, AF.Sqrt, [ssB, epsB], [ssB], scale=1.0 / D, bias=epsb)
        S.op("dve", lambda e: e.reciprocal(out=ss, in_=ss), [ssB], [ssB])
        k.stt(ht, xt, ss, At, ALU.mult, ALU.mult, [xB, ssB, AB], [hB])
        k.tt(ht, ht, Bt, ALU.add, [hB, BB], [hB], eng="pool")

    def groups_of(tiles, gsz=4):
        gs = []
        ctxs = [t for t in tiles if t < 2]
        lat = [t for t in tiles if t >= 2]
        if ctxs:
            gs.append(ctxs)
        for i in range(0, len(lat), gsz):
            gs.append(lat[i:i + gsz])
        return gs

    with ExitStack() as st:
        win, winB = k.sb(st, "win", [128, 8, 1536], BF16)
        k.dma("pool", win, rec_w_in.rearrange("(kc p) n -> p kc n", p=128), winB, None)
        AA = [load_mod(st, "sp", 0, kd, 1, f"A{kd}") for kd in range(2)]
        BBm = [load_mod(st, "sp", 0, kd, 0, f"B{kd}") for kd in range(2)]
        xt2 = [k.sb(st, f"xt{i}", [128, D]) for i in range(2)]
        ht2 = [k.sb(st, f"ht{i}", [128, D]) for i in range(2)]
        jt, jB = k.sb(st, "jt", [128, D])
        ss2 = [k.sb(st, f"ss{i}", [128, 1]) for i in range(2)]
        hT2 = [k.sb(st, f"hT{i}", [128, 8, 512], BF16) for i in range(2)]
        stg2 = [k.sb(st, f"stg{i}", [128, 12, 512]) for i in range(2)]
        tcount = 0
        for gi, g in enumerate(groups_of(list(range(NT)))):
            hT, hTB = hT2[gi % 2]
            n = 128 * len(g)
            col0 = g[0] * 128
            for si, ti in enumerate(g):
                xt, xB = xt2[tcount % 2]; ht, hB = ht2[tcount % 2]; ss, ssB = ss2[tcount % 2]
                src, kd = xsrc(ti)
                k.dma("sp", xt, src, xB, None)
                norm_mod(xt, xB, ht, hB, AA[kd][0], AA[kd][1], BBm[kd][0], BBm[kd][1], (jt, jB, ss, ssB))
                for b in range(2):
                    ps, psB = psb[(tcount * 2 + b) % 4]
                    for j in range(4):
                        kc = b * 4 + j
                        k.tr(ps[:, j * 128:(j + 1) * 128], ht[:, kc * 128:(kc + 1) * 128], ident, [hB, identB], [psB])
                    k.cp(hT[:, b * 4:(b + 1) * 4, si * 128:(si + 1) * 128], ps.rearrange("p (j t) -> p j t", j=4),
                         [psB], [hTB], eng=("act" if b == 0 else "dve"))
                tcount += 1
            stg, stgB = stg2[gi % 2]
            for oc in range(12):
                ps, psB = psb[4 + oc % 4]
                for kc in range(8):
                    k.mm(ps[:, :n], win[:, kc, oc * 128:(oc + 1) * 128], hT[:, kc, :n], kc == 0, kc == 7, [winB, hTB], [psB])
                k.cp(stg[:, oc, :n], ps[:, :n], [psB], [stgB], eng=("act" if oc % 2 == 0 else "dve"))
            k.dma("sp", projT[:, col0:col0 + n].rearrange("(oc p) t -> p oc t", p=128), stg[:, :, :n], projTB, stgB)
    S.barrier()

    def blocks_fwd():
        bl = [(0, 256)]
        for c0 in range(256, TT, 512):
            bl.append((c0, 512))
        return bl

    with ExitStack() as st:
        cw, cwB = k.sb(st, "cw", [128, 4, 4])
        cb, cbB = k.sb(st, "cb", [128, 4])
        gba, gbaB = k.sb(st, "gba", [128, 2, 4]); gbx, gbxB = k.sb(st, "gbx", [128, 2, 4])
        lam, lamB = k.sb(st, "lam", [128, 2, 4])
        k.dma("sp", cw, rec_conv_w.rearrange("k (j p) -> p k j", p=128), cwB, None, allow_slow_non_contiguous=True)
        k.dma("sp", cb, rec_conv_b.rearrange("(j p) -> p j", p=128), cbB, None, allow_slow_non_contiguous=True)
        k.dma("sp", gba, lru_ba.rearrange("d (j p) -> p d j", p=128), gbaB, None, allow_slow_non_contiguous=True)
        k.dma("sp", gbx, lru_bx.rearrange("d (j p) -> p d j", p=128), gbxB, None, allow_slow_non_contiguous=True)
        k.dma("sp", lam, lru_lambda.rearrange("d (j p) -> p d j", p=128), lamB, None, allow_slow_non_contiguous=True)
        t1, t1B = k.sb(st, "t1", [128, 2, 4]); t2, t2B = k.sb(st, "t2", [128, 2, 4]); t3, t3B = k.sb(st, "t3", [128, 2, 4])
        cl, clB = k.sb(st, "cl", [128, 2, 4]); cl2, cl2B = k.sb(st, "cl2", [128, 2, 4])
        k.act(t1, lam, AF.Abs, [lamB], [t1B])
        k.act(t2, t1, AF.Exp, [t1B], [t2B], scale=-1.0)
        k.ts(t3, t2, 2.0, ALU.add, [t2B], [t3B])
        S.op("dve", lambda e: e.reciprocal(out=t3, in_=t3), [t3B], [t3B])
        k.tt(t2, t2, t3, ALU.mult, [t2B, t3B], [t2B])
        k.tt(t3, t2, t2, ALU.mult, [t2B], [t3B])
        k.ts(t1, t3, 1.0 / 9, ALU.mult, [t3B], [t1B], s2=1.0 / 7, op1=ALU.add)
        for cf in (1.0 / 5, 1.0 / 3, 1.0):
            k.tt(t1, t1, t3, ALU.mult, [t1B, t3B], [t1B])
            k.ts(t1, t1, cf, ALU.add, [t1B], [t1B])
        k.tt(t1, t1, t2, ALU.mult, [t1B, t2B], [t1B])
        k.ts(t2, lam, -1.0, ALU.mult, [lamB], [t2B], s2=0.0, op1=ALU.max)
        k.stt(t1, t1, 2.0, t2, ALU.mult, ALU.add, [t1B, t2B], [t1B])
        k.ts(cl, t1, -8.0, ALU.mult, [t1B], [clB])
        k.ts(cl2, t1, -16.0, ALU.mult, [t1B], [cl2B])
        wg, wgB = k.sb(st, "wg", [128, 2, 2, 4, 128])
        k.ms(wg, 0.0, [wgB])
        for gi_, wsrc in enumerate((lru_wa, lru_wx)):
            for d in range(2):
                for h in range(8):
                    r0 = (h % 2) * 64
                    k.dma("sp", wg[r0:r0 + 64, gi_, d, h // 2, r0:r0 + 64], wsrc[d, h, :, :], wgB, None)
        u, uB = k.sb(st, "u", [128, TT]); ya, yaB = k.sb(st, "ya", [128, TT])
        NB = 2
        rt = [k.sb(st, f"rt{i}", [128, 512]) for i in range(NB)]
        it = [k.sb(st, f"it{i}", [128, 512]) for i in range(NB)]
        at = [k.sb(st, f"at{i}", [128, 512]) for i in range(NB)]
        sq = [k.sb(st, f"sq{i}", [128, 512]) for i in range(NB)]
        hb = [k.sb(st, f"hb{i}", [128, 512]) for i in range(NB)]
        zero1, zero1B = k.sb(st, "zero1", [128, 1])
        k.ms(zero1, 0.0, [zero1B])
        for j in range(4):
            k.dma("sp", ya, projT[j * 128:(j + 1) * 128, :], yaB, projTB)
            for (s0, s1) in ((0, 256), (256, TT)):
                k.act(u[:, s0:s1], ya[:, s0:s1], AF.Identity, [yaB, cwB, cbB], [uB], scale=cw[:, 2, j:j + 1], bias=cb[:, j:j + 1])
                k.stt(u[:, s0 + 2:s1], ya[:, s0:s1 - 2], cw[:, 0, j:j + 1], u[:, s0 + 2:s1], ALU.mult, ALU.add, [yaB, cwB, uB], [uB])
                k.stt(u[:, s0 + 1:s1], ya[:, s0:s1 - 1], cw[:, 1, j:j + 1], u[:, s0 + 1:s1], ALU.mult, ALU.add, [yaB, cwB, uB], [uB])
                k.stt(u[:, s0:s1 - 1], ya[:, s0 + 1:s1], cw[:, 3, j:j + 1], u[:, s0:s1 - 1], ALU.mult, ALU.add, [yaB, cwB, uB], [uB])
            bi = 0
            for d in range(2):
                bl = blocks_fwd()
                if d == 1:
                    bl = [bl[0]] + bl[1:][::-1]
                carry = (zero1, zero1B)
                for (c0, n) in bl:
                    pa, paB = psb[(bi * 2) % 8]; px, pxB = psb[(bi * 2 + 1) % 8]
                    r_, rB = rt[bi % NB]; i_, iB = it[bi % NB]; a_, aB = at[bi % NB]; s_, sB = sq[bi % NB]; h_, hB = hb[bi % NB]
                    ub = u[:, c0:c0 + n]
                    k.mm(pa[:, :n], wg[:, 0, d, j, :], ub, True, True, [wgB, uB], [paB])
                    k.mm(px[:, :n], wg[:, 1, d, j, :], ub, True, True, [wgB, uB], [pxB])
                    k.act(r_[:, :n], pa[:, :n], AF.Sigmoid, [paB, gbaB], [rB], bias=gba[:, d, j:j + 1])
                    k.act(i_[:, :n], px[:, :n], AF.Sigmoid, [pxB, gbxB], [iB], bias=gbx[:, d, j:j + 1])
                    k.act(a_[:, :n], r_[:, :n], AF.Exp, [rB, clB], [aB], scale=cl[:, d, j:j + 1])
                    k.act(s_[:, :n], r_[:, :n], AF.Exp, [rB, cl2B], [sB], scale=cl2[:, d, j:j + 1])
                    k.ts(s_[:, :n], s_[:, :n], -1.0, ALU.mult, [sB], [sB], s2=1.0, op1=ALU.add, eng="pool")
                    k.act(s_[:, :n], s_[:, :n], AF.Sqrt, [sB], [sB])
                    k.tt(i_[:, :n], i_[:, :n], ub, ALU.mult, [iB, uB], [iB])
                    k.tt(i_[:, :n], i_[:, :n], s_[:, :n], ALU.mult, [iB, sB], [iB])
                    if d == 0:
                        k.scan(ya[:, c0:c0 + n], a_[:, :n], i_[:, :n], carry[0], [aB, iB, carry[1]], [yaB])
                        carry = (ya[:, c0 + n - 1:c0 + n], yaB)
                    else:
                        k.scan(rv(h_[:, :n]), rv(a_[:, :n]), rv(i_[:, :n]), carry[0], [aB, iB, carry[1]], [hB])
                        carry = (h_[:, 0:1], hB)
                        k.tt(ya[:, c0:c0 + n], ya[:, c0:c0 + n], h_[:, :n], ALU.add, [yaB, hB], [yaB], eng="pool")
                    bi += 1
            for (c0, n) in blocks_fwd():
                g_, gB = rt[bi % NB]
                k.dma("sp", g_[:, :n], projT[512 + j * 128:512 + (j + 1) * 128, c0:c0 + n], gB, projTB)
                k.act(g_[:, :n], g_[:, :n], AF.Gelu, [gB], [gB])
                k.tt(ya[:, c0:c0 + n], ya[:, c0:c0 + n], g_[:, :n], ALU.mult, [yaB, gB], [yaB])
                bi += 1
            k.dma("sp", ymixT[j * 128:(j + 1) * 128, :], ya, ymixTB, yaB)
    S.barrier()

    if stop_after <= 2:
        S.finish()
        return k
    L = LCH
    with ExitStack() as st:
        BT, BTB = k.sb(st, "BT", [128, 2, 2, 4, 128])
        Cpad, CpadB = k.sb(st, "Ccomp", [128, 2, 2, 16, 32])
        maskc, maskcB = k.sb(st, "maskc", [128, 4])
        CTAB, CTABB = k.sb(st, "CTAB", [128, 2, 16, L]); STAB, STABB = k.sb(st, "STAB", [128, 2, 16, L])
        dcol, dcolB = k.sb(st, "dcol", [128, 4])
        mag, magB = k.sb(st, "mag", [128, 2, 16]); nsL, nsLB = k.sb(st, "nsL", [128, 2, 16])
        W2, W2B = k.sb(st, "W2", [128, 2, 16, 2])
        stp = ExitStack()
        are, areB = k.sb(stp, "are", [128, 2, 16]); aim, aimB = k.sb(stp, "aim", [128, 2, 16]); ldt, ldtB = k.sb(stp, "ldt", [128, 2, 16])
        bre, breB = k.sb(stp, "bre", [128, 2, 16, 16]); bim, bimB = k.sb(stp, "bim", [128, 2, 16, 16])
        cre, creB = k.sb(stp, "cre", [128, 2, 16, 16]); cim, cimB = k.sb(stp, "cim", [128, 2, 16, 16])
        for d in range(2):
            k.dma("sp", are[:, d, :], s5_a_re[d].rearrange("(gp g2) n -> (g2 n) gp", g2=2), areB, None, allow_slow_non_contiguous=True)
            k.dma("sp", aim[:, d, :], s5_a_im[d].rearrange("(gp g2) n -> (g2 n) gp", g2=2), aimB, None, allow_slow_non_contiguous=True)
            k.dma("sp", bre[:, d, :, :], s5_b_re[d].rearrange("(gp g2) n p -> (g2 n) gp p", g2=2), breB, None)
            k.dma("sp", bim[:, d, :, :], s5_b_im[d].rearrange("(gp g2) n p -> (g2 n) gp p", g2=2), bimB, None)
            for g2 in range(2):
                src = bass.AP(s5_log_dt.tensor, s5_log_dt.offset + d * 32 + g2, [[0, 64], [2, 16]])
                k.dma("sp", ldt[g2 * 64:(g2 + 1) * 64, d, :], src, ldtB, None, allow_slow_non_contiguous=True)
                for (ct_, cB_, csrc) in ((cre, creB, s5_c_re), (cim, cimB, s5_c_im)):
                    for gp_ in range(16):
                        src = bass.AP(csrc.tensor, csrc.offset + d * 32768 + (2 * gp_ + g2) * 1024, [[1, 64], [64, 16]])
                        k.dma("sp", ct_[g2 * 64:(g2 + 1) * 64, d, gp_, :], src, cB_, None, allow_slow_non_contiguous=True)
        nm = [0]
        def sm(shape=[128, 2, 16], dt=F32):
            nm[0] += 1
            return k.sb(stp, f"sm{nm[0]}", shape, dt)
        dtt, dttB = sm(); th, thB = sm(); ki, kiB = sm(dt=I32); kf, kfB = sm()
        sh, shB = sm(); chh, chhB = sm(); sn, snB = sm(); cs_, csB_ = sm(); lbr, lbrB = sm(); lbi, lbiB = sm()
        den, denB = sm(); tq, tqB = sm(); qre, qreB = sm(); qim, qimB = sm()
        k.act(dtt, ldt, AF.Exp, [ldtB], [dttB])
        k.tt(mag, are, dtt, ALU.mult, [areB, dttB], [magB])
        k.act(mag, mag, AF.Exp, [magB], [magB])
        k.tt(th, aim, dtt, ALU.mult, [aimB, dttB], [thB])
        k.ts(th, th, 1.0 / (2 * math.pi), ALU.mult, [thB], [thB])
        k.cp(ki, th, [thB], [kiB]); k.cp(kf, ki, [kiB], [kfB])
        k.tt(th, th, kf, ALU.subtract, [thB, kfB], [thB])
        k.act(sh, th, AF.Sin, [thB], [shB], scale=math.pi)
        k.act(kf, th, AF.Abs, [thB], [kfB])
        k.act(chh, kf, AF.Sin, [kfB, halfpiB], [chhB], scale=-math.pi, bias=halfpi)
        k.stt(sn, sh, 2.0, chh, ALU.mult, ALU.mult, [shB, chhB], [snB])
        k.tt(cs_, sh, sh, ALU.mult, [shB], [csB_])
        k.ts(cs_, cs_, -2.0, ALU.mult, [csB_], [csB_], s2=1.0, op1=ALU.add)
        k.tt(lbr, mag, cs_, ALU.mult, [magB, csB_], [lbrB]); k.tt(lbi, mag, sn, ALU.mult, [magB, snB], [lbiB])
        k.tt(den, are, are, ALU.mult, [areB], [denB]); k.tt(tq, aim, aim, ALU.mult, [aimB], [tqB])
        k.tt(den, den, tq, ALU.add, [denB, tqB], [denB])
        S.op("dve", lambda e: e.reciprocal(out=den, in_=den), [denB], [denB])
        k.ts(lbr, lbr, -1.0, ALU.add, [lbrB], [lbrB])
        k.tt(qre, lbr, are, ALU.mult, [lbrB, areB], [qreB]); k.tt(tq, lbi, aim, ALU.mult, [lbiB, aimB], [tqB])
        k.tt(qre, qre, tq, ALU.add, [qreB, tqB], [qreB]); k.tt(qre, qre, den, ALU.mult, [qreB, denB], [qreB])
        k.tt(qim, lbi, are, ALU.mult, [lbiB, areB], [qimB]); k.tt(tq, lbr, aim, ALU.mult, [lbrB, aimB], [tqB])
        k.tt(qim, qim, tq, ALU.subtract, [qimB, tqB], [qimB]); k.tt(qim, qim, den, ALU.mult, [qimB, denB], [qimB])
        Bblk, BblkB = k.sb(stp, "Bblk", [128, 2, 2, 16, 32])
        k.ms(Bblk, 0.0, [BblkB])
        pA, pAB = sm([128, 2, 16, 16]); pB_, pBB = sm([128, 2, 16, 16])
        qreb = qre.unsqueeze(3).broadcast_to([128, 2, 16, 16]); qimb = qim.unsqueeze(3).broadcast_to([128, 2, 16, 16])
        k.tt(pA, bre, qreb, ALU.mult, [breB, qreB], [pAB]); k.tt(pB_, bim, qimb, ALU.mult, [bimB, qimB], [pBB])
        for g2 in range(2):
            hs = slice(g2 * 64, (g2 + 1) * 64)
            k.tt(Bblk[hs, :, 0, :, g2 * 16:(g2 + 1) * 16], pA[hs], pB_[hs], ALU.subtract, [pAB, pBB], [BblkB])
        k.tt(pA, bim, qreb, ALU.mult, [bimB, qreB], [pAB]); k.tt(pB_, bre, qimb, ALU.mult, [breB, qimB], [pBB])
        for g2 in range(2):
            hs = slice(g2 * 64, (g2 + 1) * 64)
            k.tt(Bblk[hs, :, 1, :, g2 * 16:(g2 + 1) * 16], pA[hs], pB_[hs], ALU.add, [pAB, pBB], [BblkB])
        ti_ = 0
        for d in range(2):
            for ri in range(2):
                for c_ in range(4):
                    ps, psB = psb[ti_ % 4]; ti_ += 1
                    k.tr(ps[:, 0:128], Bblk[:, d, ri, 4 * c_:4 * c_ + 4, :].rearrange("p a b -> p (a b)"), ident, [BblkB, identB], [psB])
                    k.cp(BT[:, d, ri, c_, :], ps[:, 0:128], [psB], [BTB])
        k.ms(Cpad, 0.0, [CpadB])
        for g2 in range(2):
            hs = slice(g2 * 64, (g2 + 1) * 64)
            k.cp(Cpad[hs, :, 0, :, g2 * 16:(g2 + 1) * 16], cre[hs], [creB], [CpadB])
            k.ts(Cpad[hs, :, 1, :, g2 * 16:(g2 + 1) * 16], cim[hs], -1.0, ALU.mult, [cimB], [CpadB])
        for gq in range(4):
            S.op("dve", lambda e, gq=gq: e.tensor_reduce(out=maskc[:, gq:gq + 1], in_=ident[:, gq * 32:(gq + 1) * 32], axis=AX.X, op=ALU.add), [identB], [maskcB])
        ta, taB = sm([128, 2, 16, L // 2]); tb, tbB = sm([128, 2, 16, L // 2])
        k.cp(CTAB[:, :, :, 0], cs_, [csB_], [CTABB]); k.cp(STAB[:, :, :, 0], sn, [snB], [STABB])
        m = 1
        while m < L:
            cm = CTAB[:, :, :, m - 1:m].broadcast_to([128, 2, 16, m]); smm = STAB[:, :, :, m - 1:m].broadcast_to([128, 2, 16, m])
            k.tt(ta[:, :, :, :m], CTAB[:, :, :, 0:m], cm, ALU.mult, [CTABB], [taB])
            k.tt(tb[:, :, :, :m], STAB[:, :, :, 0:m], smm, ALU.mult, [STABB], [tbB])
            k.tt(CTAB[:, :, :, m:2 * m], ta[:, :, :, :m], tb[:, :, :, :m], ALU.subtract, [taB, tbB], [CTABB])
            k.tt(ta[:, :, :, :m], STAB[:, :, :, 0:m], cm, ALU.mult, [STABB, CTABB], [taB])
            k.tt(tb[:, :, :, :m], CTAB[:, :, :, 0:m], smm, ALU.mult, [STABB, CTABB], [tbB])
            k.tt(STAB[:, :, :, m:2 * m], ta[:, :, :, :m], tb[:, :, :, :m], ALU.add, [taB, tbB], [STABB])
            m *= 2
        k.ts(nsL, STAB[:, :, :, L - 1], -1.0, ALU.mult, [STABB], [nsLB])
        k.cp(W2[:, :, :, 0], nsL, [nsLB], [W2B]); k.cp(W2[:, :, :, 1], STAB[:, :, :, L - 1], [STABB], [W2B])
        k.dma("sp", dcol, s5_d.rearrange("(j p) -> p j", p=128), dcolB, None, allow_slow_non_contiguous=True)
        S.barrier()
        stp.close()
        ubT, ubTB = k.sb(st, "ubT", [128, TT]); yacc, yaccB = k.sb(st, "yacc", [128, TT])
        NCH = 4
        ch = []
        for gq in range(NCH):
            dct = {nm_: k.sb(st, f"{nm_}{gq}", [128, 512]) for nm_ in ("um", "t1", "t2", "gre", "gim")}
            dct["ss"] = k.sb(st, f"ss{gq}", [128, 2, 512])
            dct["car"] = [k.sb(st, f"car{gq}_{i}", [128, 2]) for i in range(2)]
            dct["tmpc"] = k.sb(st, f"tmpc{gq}", [128, 2])
            dct["rho"] = k.sb(st, f"rho{gq}", [128, L])
            ch.append(dct)
        zero2, zero2B = k.sb(st, "zero2", [128, 2])
        k.ms(zero2, 0.0, [zero2B])
        for c_ in range(4):
            k.dma("sp", ubT, projT[1024 + c_ * 128:1024 + (c_ + 1) * 128, :], ubTB, projTB)
            k.ms(yacc, 0.0, [yaccB])
            for d in range(2):
                bl = blocks_fwd()
                if d == 1:
                    bl = [bl[0]] + bl[1:][::-1]
                for gq in range(NCH):
                    gp = 4 * c_ + gq
                    ch[gq]["cur"] = (zero2, zero2B); ch[gq]["cidx"] = 0
                    k.cp(ch[gq]["rho"][0], mag[:, d, gp:gp + 1].broadcast_to([128, L]), [magB], [ch[gq]["rho"][1]], eng="pool")
                for (c0, n) in bl:
                    nch = n // L
                    v3 = lambda t_: t_[:, :n].rearrange("p (a l) -> p a l", l=L)
                    tabs = []
                    for gq in range(NCH):
                        gp = 4 * c_ + gq
                        cosv = CTAB[:, d, gp, :]; sinv = STAB[:, d, gp, :]
                        if d == 1:
                            cosv = rv(cosv); sinv = rv(sinv)
                        tabs.append((cosv.unsqueeze(1).broadcast_to([128, nch, L]), sinv.unsqueeze(1).broadcast_to([128, nch, L])))
                    for gq in range(NCH):
                        C = ch[gq]; um, umB = C["um"]
                        pdr, pdrB = psb[2 * gq]; pdi, pdiB = psb[2 * gq + 1]
                        k.act(um[:, :n], ubT[:, c0:c0 + n], AF.Copy, [ubTB, maskcB], [umB], scale=maskc[:, gq:gq + 1])
                        k.mm(pdr[:, :n], BT[:, d, 0, c_, :], um[:, :n], True, True, [BTB, umB], [pdrB])
                        k.mm(pdi[:, :n], BT[:, d, 1, c_, :], um[:, :n], True, True, [BTB, umB], [pdiB])
                    for step in range(3):
                        for gq in range(NCH):
                            C = ch[gq]; cosb, sinb = tabs[gq]
                            pdr, pdrB = psb[2 * gq]; pdi, pdiB = psb[2 * gq + 1]
                            t1, t1B_ = C["t1"]; t2, t2B_ = C["t2"]; gre, greB = C["gre"]; gim, gimB = C["gim"]
                            if step == 0:
                                k.tt(v3(t1), v3(pdr), cosb, ALU.mult, [pdrB, CTABB], [t1B_])
                                k.tt(v3(t2), v3(pdi), sinb, ALU.mult, [pdiB, STABB], [t2B_])
                                k.tt(gre[:, :n], t1[:, :n], t2[:, :n], ALU.add, [t1B_, t2B_], [greB], eng="pool")
                            elif step == 1:
                                k.tt(v3(t1), v3(pdi), cosb, ALU.mult, [pdiB, CTABB], [t1B_])
                                k.tt(v3(t2), v3(pdr), sinb, ALU.mult, [pdrB, STABB], [t2B_])
                                k.tt(gim[:, :n], t1[:, :n], t2[:, :n], ALU.subtract, [t1B_, t2B_], [gimB], eng="pool")
                    order = list(range(nch)) if d == 0 else list(range(nch - 1, -1, -1))
                    for ci in order:
                        sl = slice(ci * L, (ci + 1) * L)
                        for op_ in range(4):
                            for gq in range(NCH):
                                C = ch[gq]; gp = 4 * c_ + gq
                                gre, greB = C["gre"]; gim, gimB = C["gim"]; ss, ssB = C["ss"]
                                rho, rhoB = C["rho"]; tmpc, tmpcB = C["tmpc"]; cur = C["cur"]
                                cLc = CTAB[:, d, gp, L - 1:L]
                                li_ = (ci + 1) * L - 1 if d == 0 else ci * L
                                last2 = ss[:, :, li_]
                                f = (lambda a_: a_) if d == 0 else rv
                                if op_ == 0:
                                    k.scan(f(ss[:, 0, sl]), rho, f(gre[:, sl]), cur[0][:, 0:1], [rhoB, greB, cur[1]], [ssB])
                                elif op_ == 1:
                                    k.scan(f(ss[:, 1, sl]), rho, f(gim[:, sl]), cur[0][:, 1:2], [rhoB, gimB, cur[1]], [ssB])
                                elif op_ == 2:
                                    C["nxt"] = C["car"][C["cidx"] % 2]; C["cidx"] += 1
                                    k.tt(tmpc, rv(last2), W2[:, d, gp, :], ALU.mult, [ssB, W2B], [tmpcB])
                                else:
                                    k.stt(C["nxt"][0], last2, cLc, tmpc, ALU.mult, ALU.add, [ssB, CTABB, tmpcB], [C["nxt"][1]])
                                    C["cur"] = C["nxt"]
                    for step in range(2):
                        for gq in range(NCH):
                            C = ch[gq]; cosb, sinb = tabs[gq]
                            t1, t1B_ = C["t1"]; t2, t2B_ = C["t2"]; gre, greB = C["gre"]; gim, gimB = C["gim"]
                            ss, ssB = C["ss"]; sre = ss[:, 0, :]; sim = ss[:, 1, :]; sreB = ssB; simB = ssB
                            if step == 0:
                                k.tt(v3(t1), v3(sre), cosb, ALU.mult, [sreB, CTABB], [t1B_], eng="pool")
                                k.tt(v3(t2), v3(sim), sinb, ALU.mult, [simB, STABB], [t2B_], eng="pool")
                                k.tt(gre[:, :n], t1[:, :n], t2[:, :n], ALU.subtract, [t1B_, t2B_], [greB], eng="pool")
                            else:
                                k.tt(v3(t1), v3(sre), sinb, ALU.mult, [sreB, STABB], [t1B_], eng="pool")
                                k.tt(v3(t2), v3(sim), cosb, ALU.mult, [simB, CTABB], [t2B_], eng="pool")
                                k.tt(gim[:, :n], t1[:, :n], t2[:, :n], ALU.add, [t1B_, t2B_], [gimB], eng="pool")
                    cc0 = Cpad[:, d, 0, 4 * c_:4 * c_ + 4, :].rearrange("p a b -> p (a b)")
                    cc1 = Cpad[:, d, 1, 4 * c_:4 * c_ + 4, :].rearrange("p a b -> p (a b)")
                    for gq in range(NCH):
                        C = ch[gq]; gre, greB = C["gre"]; gim, gimB = C["gim"]
                        py, pyB = psb[2 * gq]
                        k.mm(py[:, :n], cc0, gre[:, :n], True, False, [CpadB, greB], [pyB])
                        k.mm(py[:, :n], cc1, gim[:, :n], False, True, [CpadB, gimB], [pyB])
                        k.stt(yacc[:, c0:c0 + n], py[:, :n], maskc[:, gq:gq + 1], yacc[:, c0:c0 + n], ALU.mult, ALU.add,
                              [pyB, maskcB, yaccB], [yaccB])
            k.stt(yacc, ubT, dcol[:, c_:c_ + 1], yacc, ALU.mult, ALU.add, [ubTB, dcolB, yaccB], [yaccB])
            k.act(yacc, yacc, AF.Gelu, [yaccB], [yaccB])
            k.dma("sp", ysgT[c_ * 128:(c_ + 1) * 128, :], yacc, ysgTB, yaccB)
    S.barrier()
    if stop_after <= 3:
        S.finish()
        return k
    def load_wbf(st, name, src2d, kchunks, ncols, q="pool"):
        t, B = k.sb(st, name, [128, kchunks, ncols], BF16)
        k.dma(q, t, src2d.rearrange("(kc p) n -> p kc n", p=128), B, None)
        return t, B

    with ExitStack() as st:
        gw, gwB = load_wbf(st, "gw", s5_glu_w, 4, 512)
        wo, woB = load_wbf(st, "wo", rec_w_out, 8, D)
        glub, glubB = k.sb(st, "glub", [128, 4])
        k.dma("sp", glub, s5_glu_b.rearrange("(j p) -> p j", p=128), glubB, None, allow_slow_non_contiguous=True)
        G1 = [load_mod(st, "sp", 0, kd, 2, f"G1{kd}") for kd in range(2)]
        yaf = [k.sb(st, f"yaf{i}", [128, 4, 512]) for i in range(2)]
        ysf = [k.sb(st, f"ysf{i}", [128, 4, 512]) for i in range(2)]
        yab = [k.sb(st, f"yab{i}", [128, 4, 512], BF16) for i in range(2)]
        ysb = [k.sb(st, f"ysb{i}", [128, 4, 512], BF16) for i in range(2)]
        ys2 = [k.sb(st, f"ys2{i}", [128, 4, 512], BF16) for i in range(2)]
        sg = [k.sb(st, f"sg{i}", [128, 512]) for i in range(2)]
        xt2 = [k.sb(st, f"xt{i}", [128, D]) for i in range(2)]
        ot2 = [k.sb(st, f"ot{i}", [128, D]) for i in range(2)]
        tc_ = 0
        pc_ = 0
        for bi, (c0, n) in enumerate(blocks_fwd()):
            ya_, yaB_ = yaf[bi % 2]; ys_, ysB_ = ysf[bi % 2]; yb_, ybB_ = yab[bi % 2]; sb_, sbB_ = ysb[bi % 2]; y2_, y2B_ = ys2[bi % 2]
            k.dma("sp", ya_[:, :, :n], ymixT[:, c0:c0 + n].rearrange("(j p) t -> p j t", p=128), yaB_, ymixTB)
            k.dma("sp", ys_[:, :, :n], ysgT[:, c0:c0 + n].rearrange("(j p) t -> p j t", p=128), ysB_, ysgTB)
            k.cp(yb_[:, :, :n], ya_[:, :, :n], [yaB_], [ybB_], eng="act")
            k.cp(sb_[:, :, :n], ys_[:, :, :n], [ysB_], [sbB_], eng="pool")
            for oc in range(4):
                ps, psB = psb[pc_ % 8]; pc_ += 1
                for kc in range(4):
                    k.mm(ps[:, :n], gw[:, kc, oc * 128:(oc + 1) * 128], sb_[:, kc, :n], kc == 0, kc == 3, [gwB, sbB_], [psB])
                s_, sB_ = sg[oc % 2]
                k.act(s_[:, :n], ps[:, :n], AF.Sigmoid, [psB, glubB], [sB_], bias=glub[:, oc:oc + 1])
                k.tt(y2_[:, oc, :n], ys_[:, oc, :n], s_[:, :n], ALU.mult, [ysB_, sB_], [y2B_])
            for s in range(n // 128):
                ti = c0 // 128 + s
                xt, xB = xt2[tc_ % 2]; ot, oB = ot2[tc_ % 2]; tc_ += 1
                src, kd = xsrc(ti)
                k.dma("sp", xt, src, xB, None)
                for half in range(2):
                    ps, psB = psb[pc_ % 8]; pc_ += 1
                    for kc in range(8):
                        lh = yb_[:, kc, s * 128:(s + 1) * 128] if kc < 4 else y2_[:, kc - 4, s * 128:(s + 1) * 128]
                        k.mm(ps, lh, wo[:, kc, half * 512:(half + 1) * 512], kc == 0, kc == 7, [ybB_, y2B_, woB], [psB])
                    k.tt(ot[:, half * 512:(half + 1) * 512], ps, G1[kd][0][:, half * 512:(half + 1) * 512], ALU.mult, [psB, G1[kd][1]], [oB])
                k.tt(ot, ot, xt, ALU.add, [oB, xB], [oB], eng="pool")
                k.dma("sp", x1[ti * 128:(ti + 1) * 128, :], ot, x1B, oB)
    S.barrier()
    if stop_after <= 4:
        S.finish()
        return k

    def moe(layer, tiles, src_fn, dst_fn, dstB, final):
        with ExitStack() as st:
            kinds = sorted(set(kd for _, kd in tiles))
            A2 = {kd: load_mod(st, "sp", layer, kd, 4, f"A2{kd}") for kd in kinds}
            B2 = {kd: load_mod(st, "sp", layer, kd, 3, f"B2{kd}") for kd in kinds}
            G2 = {kd: load_mod(st, "sp", layer, kd, 5, f"G2{kd}") for kd in kinds}
            Wr, WrB = k.sb(st, "Wr", [128, 8, 36])
            rb, rbB = k.sb(st, "rb", [128, 36])
            k.dma("sp", Wr[:, :, 0:4], moe_r1_w[layer].rearrange("(kc p) g -> p kc g", p=128), WrB, None)
            k.dma("sp", rb[:, 0:4], moe_r1_b[layer, :].partition_broadcast(128), rbB, None)
            for g in range(4):
                k.dma("sp", Wr[:, :, 4 + 8 * g:12 + 8 * g], moe_r2_w[layer, g].rearrange("(kc p) e -> p kc e", p=128), WrB, None)
                k.dma("sp", rb[:, 4 + 8 * g:12 + 8 * g], moe_r2_b[layer, g, :].partition_broadcast(128), rbB, None)
            if final:
                fg, fgB = k.sb(st, "fg", [128, D])
                k.dma("sp", fg, final_norm_g.partition_broadcast(128), fgB, None)
            SUP = 8
            hT, hTB = k.sb(st, "hT", [128, 8, SUP * 128], BF16)
            acc, accB = k.sb(st, "acc", [128, SUP, D])
            gates, gatesB = k.sb(st, "gates", [128, SUP, 32])
            xt2 = [k.sb(st, f"xt{i}", [128, D]) for i in range(2)]
            ht2 = [k.sb(st, f"ht{i}", [128, D]) for i in range(2)]
            jt, jB = k.sb(st, "jt", [128, D])
            ss2 = [k.sb(st, f"ss{i}", [128, 1]) for i in range(2)]
            hTf2 = [k.sb(st, f"hTf{i}", [128, 8, 128]) for i in range(2)]
            wgt = [k.sb(st, f"wgt{i}", [128, 8, 512], BF16) for i in range(2)]
            wut = [k.sb(st, f"wut{i}", [128, 8, 512], BF16) for i in range(2)]
            wdt = [k.sb(st, f"wdt{i}", [128, 4, D], BF16) for i in range(2)]
            sil = [k.sb(st, f"sil{i}", [128, 512]) for i in range(2)]
            hid = [k.sb(st, f"hid{i}", [128, 4, 512], BF16) for i in range(2)]
            lg, lgB = k.sb(st, "lg", [128, 36]); l2m, l2mB = k.sb(st, "l2m", [128, 32])
            sm_ = {nm_: k.sb(st, f"g_{nm_}", [128, 8]) for nm_ in ("m1", "nm1", "e4", "se", "gm", "top", "d12", "w2", "dw")}
            mk1, mk1B = k.sb(st, "mk1", [128, 32]); mk2, mk2B = k.sb(st, "mk2", [128, 32])
            ecount = 0
            pd_i = 0
            for s0 in range(0, len(tiles), SUP):
                sup = tiles[s0:s0 + SUP]
                ns = len(sup)
                for s, (ti, kd) in enumerate(sup):
                    xt, xB = xt2[s % 2]; ht, hB = ht2[s % 2]; ss, ssB = ss2[s % 2]; hTf, hTfB = hTf2[s % 2]
                    src, srcB = src_fn(ti)
                    k.dma("sp", xt, src, xB, srcB)
                    norm_mod(xt, xB, ht, hB, A2[kd][0], A2[kd][1], B2[kd][0], B2[kd][1], (jt, jB, ss, ssB))
                    for b in range(2):
                        ps, psB = psb[(s * 2 + b) % 2]
                        for j in range(4):
                            kc = b * 4 + j
                            k.tr(ps[:, j * 128:(j + 1) * 128], ht[:, kc * 128:(kc + 1) * 128], ident, [hB, identB], [psB])
                        k.cp(hTf[:, b * 4:(b + 1) * 4, :], ps.rearrange("p (j t) -> p j t", j=4), [psB], [hTfB], eng=("act" if b == 0 else "dve"))
                    k.cp(hT[:, :, s * 128:(s + 1) * 128], hTf, [hTfB], [hTB], eng="pool")
                    pr, prB = psb[2 + s % 2]
                    for kc in range(8):
                        k.mm(pr[:, 0:36], hTf[:, kc, :], Wr[:, kc, :], kc == 0, kc == 7, [hTfB, WrB], [prB])
                    k.tt(lg, pr[:, 0:36], rb, ALU.add, [prB, rbB], [lgB])
                    m1, m1B = sm_["m1"]; nm1, nm1B = sm_["nm1"]; e4, e4B = sm_["e4"]; se, seB = sm_["se"]; gm, gmB = sm_["gm"]
                    top, topB = sm_["top"]; d12, d12B = sm_["d12"]; w2, w2B = sm_["w2"]; dw, dwB = sm_["dw"]
                    S.op("dve", lambda e, m1=m1: e.tensor_reduce(out=m1[:, 0:1], in_=lg[:, 0:4], axis=AX.X, op=ALU.max), [lgB], [m1B])
                    k.ts(nm1[:, 0:1], m1[:, 0:1], -1.0, ALU.mult, [m1B], [nm1B])
                    k.act(e4[:, 0:4], lg[:, 0:4], AF.Exp, [lgB, nm1B], [e4B, seB], bias=nm1[:, 0:1], accum=se[:, 0:1])
                    S.op("dve", lambda e, se=se: e.reciprocal(out=se[:, 0:1], in_=se[:, 0:1]), [seB], [seB])
                    k.ts(gm[:, 0:4], lg[:, 0:4], m1[:, 0:1], ALU.is_ge, [lgB, m1B], [gmB])
                    k.ts(gm[:, 0:4], gm[:, 0:4], 30000.0, ALU.mult, [gmB], [gmB], s2=-30000.0, op1=ALU.add)
                    k.tt(l2m.rearrange("p (g e) -> p g e", g=4), lg[:, 4:36].rearrange("p (g e) -> p g e", g=4),
                         gm[:, 0:4].unsqueeze(2).broadcast_to([128, 4, 8]), ALU.add, [lgB, gmB], [l2mB])
                    S.op("dve", lambda e, top=top: e.max(out=top[:, 0:8], in_=l2m), [l2mB], [topB])
                    k.tt(d12[:, 0:1], top[:, 1:2], top[:, 0:1], ALU.subtract, [topB], [d12B])
                    k.act(w2[:, 0:1], d12[:, 0:1], AF.Sigmoid, [d12B], [w2B])
                    k.tt(w2[:, 0:1], w2[:, 0:1], se[:, 0:1], ALU.mult, [w2B, seB], [w2B])
                    k.stt(dw[:, 0:1], w2[:, 0:1], -2.0, se[:, 0:1], ALU.mult, ALU.add, [w2B, seB], [dwB])
                    k.ts(mk2, l2m, top[:, 1:2], ALU.is_ge, [l2mB, topB], [mk2B])
                    k.ts(mk1, l2m, top[:, 0:1], ALU.is_ge, [l2mB, topB], [mk1B])
                    k.ts(mk2, mk2, w2[:, 0:1], ALU.mult, [mk2B, w2B], [mk2B])
                    k.stt(gates[:, s, :], mk1, dw[:, 0:1], mk2, ALU.mult, ALU.add, [mk1B, dwB, mk2B], [gatesB])
                grp = [list(range(i, min(i + 4, ns))) for i in range(0, ns, 4)]
                for e_ in range(32):
                    wg_, wgB_ = wgt[ecount % 2]; wu_, wuB_ = wut[ecount % 2]; wd_, wdB_ = wdt[ecount % 2]; ecount += 1
                    k.dma("pool", wg_, moe_w_gate[layer, e_].rearrange("(kc p) f -> p kc f", p=128), wgB_, None)
                    k.dma("pool", wu_, moe_w_up[layer, e_].rearrange("(kc p) f -> p kc f", p=128), wuB_, None)
                    k.dma("pool", wd_, moe_w_down[layer, e_].rearrange("(fc p) n -> p fc n", p=128), wdB_, None)
                    for gi, gt in enumerate(grp):
                        n = 128 * len(gt); cc = gt[0] * 128
                        hd, hdB = hid[gi % 2]
                        for fc in range(4):
                            pg, pgB = psb[fc % 2]; pu, puB = psb[2 + fc % 2]
                            for kc in range(8):
                                k.mm(pg[:, :n], wg_[:, kc, fc * 128:(fc + 1) * 128], hT[:, kc, cc:cc + n], kc == 0, kc == 7, [wgB_, hTB], [pgB])
                            for kc in range(8):
                                k.mm(pu[:, :n], wu_[:, kc, fc * 128:(fc + 1) * 128], hT[:, kc, cc:cc + n], kc == 0, kc == 7, [wuB_, hTB], [puB])
                            sl_, slB = sil[fc % 2]
                            k.act(sl_[:, :n], pg[:, :n], AF.Silu, [pgB], [slB])
                            k.tt(hd[:, fc, :n], sl_[:, :n], pu[:, :n], ALU.mult, [slB, puB], [hdB])
                    for gi, gt in enumerate(grp):
                        hd, hdB = hid[gi % 2]
                        for si, s in enumerate(gt):
                            for half in range(2):
                                pd, pdB = psb[4 + pd_i % 4]; pd_i += 1
                                for fc in range(4):
                                    k.mm(pd, hd[:, fc, si * 128:(si + 1) * 128], wd_[:, fc, half * 512:(half + 1) * 512], fc == 0, fc == 3, [hdB, wdB_], [pdB])
                                av = acc[:, s, half * 512:(half + 1) * 512]
                                if e_ == 0:
                                    k.ts(av, pd, gates[:, s, e_:e_ + 1], ALU.mult, [pdB, gatesB], [accB])
                                else:
                                    k.stt(av, pd, gates[:, s, e_:e_ + 1], av, ALU.mult, ALU.add, [pdB, gatesB, accB], [accB])
                for s, (ti, kd) in enumerate(sup):
                    xt, xB = xt2[s % 2]; ot, oB = ht2[s % 2]; ss, ssB = ss2[s % 2]
                    src, srcB = src_fn(ti)
                    k.dma("sp", xt, src, xB, srcB)
                    k.tt(ot, acc[:, s, :], G2[kd][0], ALU.mult, [accB, G2[kd][1]], [oB], eng="pool")
                    k.tt(ot, ot, xt, ALU.add, [oB, xB], [oB])
                    if final:
                        k.act(jt, ot, AF.Square, [oB], [jB, ssB], accum=ss)
                        k.act(ss, ss, AF.Sqrt, [ssB, epsB], [ssB], scale=1.0 / D, bias=epsb)
                        S.op("dve", lambda e, ss=ss: e.reciprocal(out=ss, in_=ss), [ssB], [ssB])
                        k.stt(ot, ot, ss, fg, ALU.mult, ALU.mult, [oB, ssB, fgB], [oB])
                    k.dma("sp", dst_fn(ti), ot, dstB, oB)
        S.barrier()

    moe(0, [(ti, 1 if ti < 2 else 0) for ti in range(NT)], lambda ti: (x1[ti * 128:(ti + 1) * 128, :], x1B),
        lambda ti: x2[ti * 128:(ti + 1) * 128, :], x2B, False)
    if stop_after <= 5:
        S.finish()
        return k
    NTL = T // 128
    ROWS = T // 64
    QT, QTB = k.dscr("QT", [D, T], BF16)
    KT, KTB = k.dscr("KT", [D, TT], BF16)
    VA, VAB = k.dscr("VA", [TT, 16, 65], BF16)
    AO, AOB = k.dscr("AO", [T, D])
    REV, REVB = k.dscr("REV", [16, 15, 128])
    with ExitStack() as st:
        wq, wqB = load_wbf(st, "wqkv", na_w_qkv, 8, 3 * D)
        AA = [load_mod(st, "sp", 1, kd, 1, f"A{kd}") for kd in range(2)]
        BBm = [load_mod(st, "sp", 1, kd, 0, f"B{kd}") for kd in range(2)]
        xt2 = [k.sb(st, f"xt{i}", [128, D]) for i in range(2)]
        ht2 = [k.sb(st, f"ht{i}", [128, D]) for i in range(2)]
        jt, jB = k.sb(st, "jt", [128, D])
        ss2 = [k.sb(st, f"ss{i}", [128, 1]) for i in range(2)]
        hT2 = [k.sb(st, f"hT{i}", [128, 8, 512], BF16) for i in range(2)]
        qst2 = [k.sb(st, f"qst{i}", [128, 8, 512], BF16) for i in range(2)]
        kst2 = [k.sb(st, f"kst{i}", [128, 8, 512], BF16) for i in range(2)]
        vst2 = [k.sb(st, f"vst{i}", [128, 16, 65], BF16) for i in range(2)]
        for i in range(2):
            k.ms(vst2[i][0], 1.0, [vst2[i][1]])
        tcount = 0; pc_ = 0
        for gi, g in enumerate(groups_of(list(range(NT)))):
            hT, hTB = hT2[gi % 2]
            n = 128 * len(g); col0 = g[0] * 128
            isctx = g[0] < 2
            for si, ti in enumerate(g):
                xt, xB = xt2[tcount % 2]; ht, hB = ht2[tcount % 2]; ss, ssB = ss2[tcount % 2]
                kd = 1 if ti < 2 else 0
                k.dma("sp", xt, x2[ti * 128:(ti + 1) * 128, :], xB, x2B)
                norm_mod(xt, xB, ht, hB, AA[kd][0], AA[kd][1], BBm[kd][0], BBm[kd][1], (jt, jB, ss, ssB))
                for b in range(2):
                    ps, psB = psb[(tcount * 2 + b) % 2]
                    for j in range(4):
                        kc = b * 4 + j
                        k.tr(ps[:, j * 128:(j + 1) * 128], ht[:, kc * 128:(kc + 1) * 128], ident, [hB, identB], [psB])
                    k.cp(hT[:, b * 4:(b + 1) * 4, si * 128:(si + 1) * 128], ps.rearrange("p (j t) -> p j t", j=4),
                         [psB], [hTB], eng=("act" if b == 0 else "dve"))
                tcount += 1
            qst, qstB = qst2[gi % 2]; kst, kstB = kst2[gi % 2]
            for oc in range(16):
                if isctx and oc < 8:
                    continue
                ps, psB = psb[2 + pc_ % 3]; pc_ += 1
                for kc in range(8):
                    k.mm(ps[:, :n], wq[:, kc, oc * 128:(oc + 1) * 128], hT[:, kc, :n], kc == 0, kc == 7, [wqB, hTB], [psB])
                if oc < 8:
                    k.act(qst[:, oc, :n], ps[:, :n], AF.Copy, [psB], [qstB], scale=0.125)
                else:
                    k.cp(kst[:, oc - 8, :n], ps[:, :n], [psB], [kstB])
            if not isctx:
                k.dma("sp", QT[:, col0 - CT:col0 - CT + n].rearrange("(oc p) t -> p oc t", p=128), qst[:, :, :n], QTB, qstB)
            k.dma("sp", KT[:, col0:col0 + n].rearrange("(oc p) t -> p oc t", p=128), kst[:, :, :n], KTB, kstB)
            for si, ti in enumerate(g):
                vst, vstB = vst2[ti % 2]
                for half in range(2):
                    ps, psB = psb[5 + pc_ % 3]; pc_ += 1
                    for kc in range(8):
                        k.mm(ps, hT[:, kc, si * 128:(si + 1) * 128], wq[:, kc, 2 * D + half * 512:2 * D + (half + 1) * 512], kc == 0, kc == 7, [wqB, hTB], [psB])
                    k.cp(vst[:, half * 8:(half + 1) * 8, 0:64], ps.rearrange("p (h e) -> p h e", h=8), [psB], [vstB], eng=("act" if half == 0 else "dve"))
                k.dma("sp", VA[ti * 128:(ti + 1) * 128, :, :], vst, VAB, vstB)
    S.barrier()
    if stop_after <= 6:
        S.finish()
        return k

    with ExitStack() as st:
        T2, T2B = k.sb(st, "T2", [128, 14, 16, 64])
        zt, ztB = k.sb(st, "zt", [128, 256])
        k.ms(zt, 0.0, [ztB])
        k.dma("sp", REV.rearrange("h d j -> (h d j)").rearrange("(p f) -> p f", p=120), zt[0:120, :], REVB, ztB)
        rpt, rptB = k.sb(st, "rpt", [16, 15, 31])
        k.dma("sp", rpt, na_rpb, rptB, None)
        k.dma("sp", REV[:, :, 48:79], rpt, REVB, rptB)
        with ExitStack() as st2:
            T2r, T2rB = k.sb(st2, "T2r", [128, 7, 16, 64])
            for hf in range(2):
                for dl in range(2):
                    for a_ in range(7):
                        drb = hf * 7 + a_
                        for par in range(2):
                            src = bass.AP(REV.tensor, REV.offset + (drb + dl) * 128 + par * 15 * 128, [[1, 64], [30 * 128, 8], [1, 64]])
                            k.dma("sp", T2r[dl * 64:(dl + 1) * 64, a_, par * 8:(par + 1) * 8, :], src, T2rB, REVB)
                k.cp(T2[:, hf * 7:(hf + 1) * 7, :, :].rearrange("p a h q -> p (a h) q"),
                     rv(T2r.rearrange("p a h q -> p (a h) q")), [T2rB], [T2B], eng="dve")
        S.barrier()
        Mt, MtB = k.sb(st, "Mt", [128, 64])
        k.ms(Mt, 0.0, [MtB])
        NEG = -30000.0
        for dl in range(2):
            hs = slice(dl * 64, (dl + 1) * 64)
            sels = [(slice(0, 8), [[0, 8]], 15, -1), (slice(8, 57), [[-1, 49]], 0, 1), (slice(8, 57), [[1, 49]], 15, -1),
                    (slice(57, 64), [[0, 7]], -48, 1)]
            for (qs, pat, base, cm) in sels:
                S.op("pool", lambda e, hs=hs, qs=qs, pat=pat, base=base, cm=cm: e.affine_select(
                    out=Mt[hs, qs], in_=Mt[hs, qs], pattern=pat, compare_op=ALU.is_ge, fill=NEG, base=base, channel_multiplier=cm), [MtB], [MtB])
        T2v = T2.rearrange("p a h q -> p (a h) q")
        k.tt(T2v, T2v, Mt.unsqueeze(1).broadcast_to([128, 224, 64]), ALU.add, [T2B, MtB], [T2B])
        if "T2d" in k.dbg:
            T2d, T2dB = k.dscr("T2d", [128, 14 * 16 * 64])
            k.dma("sp", T2d, T2.rearrange("p a h q -> p (a h q)"), T2dB, T2B)
        KcT, KcTB = k.sb(st, "KcT", [128, 8, 256], BF16)
        Vc, VcB = k.sb(st, "Vc", [128, 2, 1040], BF16)
        k.dma("sp", KcT, KT[:, 0:256].rearrange("(c p) t -> p c t", p=128), KcTB, KTB)
        k.dma("sp", Vc, VA[0:256].rearrange("(a p) h e -> p a (h e)", p=128), VcB, VAB)
        qT2 = [k.sb(st, f"qT{i}", [128, 8, 64], BF16) for i in range(2)]
        kT2 = [k.sb(st, f"kT{i}", [128, 8, 512], BF16) for i in range(2)]
        vv2 = [k.sb(st, f"vv{i}", [128, 4, 1040], BF16) for i in range(2)]
        sb2 = [k.sb(st, f"sbt{i}", [128, 1024]) for i in range(2)]
        pT2 = [k.sb(st, f"pT{i}", [128, 1024], BF16) for i in range(2)]
        ao2 = [k.sb(st, f"ao{i}", [128, D]) for i in range(2)]
        rec2 = [k.sb(st, f"rec{i}", [128, 16]) for i in range(2)]
        ci_ = 0
        P7 = int(os.environ.get('P7', '9'))
        for r in range(ROWS if P7 >= 1 else 0):
            rs_ = min(max(r - 4, 0), ROWS - 8)
            qT, qTB = qT2[r % 2]; kT, kTB = kT2[r % 2]; vv, vvB = vv2[r % 2]
            k.dma("sp", qT, QT[:, r * 64:(r + 1) * 64].rearrange("(c p) t -> p c t", p=128), qTB, QTB)
            k0 = CT + rs_ * 64
            k.dma("sp", kT, KT[:, k0:k0 + 512].rearrange("(c p) t -> p c t", p=128), kTB, KTB)
            k.dma("sp", vv, VA[k0:k0 + 512].rearrange("(a p) h e -> p a (h e)", p=128), vvB, VAB)
            ph = slice((r % 2) * 64, (r % 2) * 64 + 64)
            for kc in range(6):
                sbank = [psb[(ci_ % 2) * 2], psb[(ci_ % 2) * 2 + 1]]
                sbt, sbtB = sb2[ci_ % 2]; pT, pTB = pT2[ci_ % 2]; ci_ += 1
                for h in range(16):
                    hp = slice((h % 2) * 64, (h % 2) * 64 + 64)
                    if kc < 4:
                        lh = kT[hp, h // 2, kc * 128:(kc + 1) * 128]; lB = kTB
                    else:
                        lh = KcT[hp, h // 2, (kc - 4) * 128:(kc - 3) * 128]; lB = KcTB
                    sp_, spB = sbank[h % 2]
                    k.mm(sp_[:, (h // 2) * 64:(h // 2 + 1) * 64], lh, qT[hp, h // 2, :], True, True, [lB, qTB], [spB])
                if kc < 4:
                    drb = 2 * kc + (rs_ - r + 7)
                    for b in range(2):
                        k.tt(sbt[:, b * 512:(b + 1) * 512], sbank[b][0], T2[:, drb, b * 8:(b + 1) * 8, :].rearrange("p h q -> p (h q)"),
                             ALU.add, [sbank[b][1], T2B], [sbtB])
                    k.act(pT, sbt, AF.Exp, [sbtB], [pTB])
                    vsrc = vv[:, kc, :]; vB_ = vvB
                else:
                    for b in range(2):
                        k.act(pT[:, b * 512:(b + 1) * 512], sbank[b][0], AF.Exp, [sbank[b][1]], [pTB])
                    vsrc = Vc[:, kc - 4, :]; vB_ = VcB
                for h in range(16 if P7 >= 2 else 0):
                    ob, obB = psb[4 + h // 7]
                    hh = (h % 2) * 8 + h // 2
                    k.mm(ob[ph, (h % 7) * 65:(h % 7 + 1) * 65], pT[:, hh * 64:(hh + 1) * 64], vsrc[:, h * 65:(h + 1) * 65],
                         (kc == 0 and h % 7 == 0), kc == 5, [pTB, vB_], [obB], skip=True)
            ao, aoB = ao2[(r // 2) % 2]; rec, recB = rec2[r % 2]
            for b in range(3 if P7 >= 3 else 0):
                nh = 7 if b < 2 else 2
                ob, obB = psb[4 + b]
                ov = ob[ph, 0:nh * 65].rearrange("p (h e) -> p h e", e=65)
                S.op("dve", lambda e, rec=rec, ph=ph, b=b, nh=nh, ov=ov: e.reciprocal(out=rec[ph, b * 7:b * 7 + nh], in_=ov[:, :, 64]), [obB], [recB])
                k.tt(ao[ph, b * 7 * 64:(b * 7 + nh) * 64].rearrange("p (h e) -> p h e", e=64), ov[:, :, 0:64],
                     rec[ph, b * 7:b * 7 + nh].unsqueeze(2).broadcast_to([64, nh, 64]), ALU.mult, [obB, recB], [aoB])
            if r % 2 == 1:
                k.dma("sp", AO[(r // 2) * 128:(r // 2 + 1) * 128, :], ao, AOB, aoB)
    S.barrier()
    if stop_after <= 7:
        S.finish()
        return k

    with ExitStack() as st:
        wo, woB = load_wbf(st, "wo1", na_w_out, 8, D)
        G1, G1B = load_mod(st, "sp", 1, 0, 2, "G1l1")
        at2 = [k.sb(st, f"at{i}", [128, D]) for i in range(2)]
        xt2 = [k.sb(st, f"xt{i}", [128, D]) for i in range(2)]
        ot2 = [k.sb(st, f"ot{i}", [128, D]) for i in range(2)]
        aT2 = [k.sb(st, f"aT{i}", [128, 8, 128], BF16) for i in range(2)]
        for tl in range(NTL):
            at_, atB = at2[tl % 2]; xt, xB = xt2[tl % 2]; ot, oB = ot2[tl % 2]; aT, aTB = aT2[tl % 2]
            k.dma("sp", at_, AO[tl * 128:(tl + 1) * 128, :], atB, AOB)
            k.dma("sp", xt, x2[CT + tl * 128:CT + (tl + 1) * 128, :], xB, x2B)
            for b in range(2):
                ps, psB = psb[(tl * 2 + b) % 4]
                for j in range(4):
                    kc = b * 4 + j
                    k.tr(ps[:, j * 128:(j + 1) * 128], at_[:, kc * 128:(kc + 1) * 128], ident, [atB, identB], [psB])
                k.cp(aT[:, b * 4:(b + 1) * 4, :], ps.rearrange("p (j t) -> p j t", j=4), [psB], [aTB], eng=("act" if b == 0 else "dve"))
            for half in range(2):
                ps, psB = psb[4 + (tl * 2 + half) % 4]
                for kc in range(8):
                    k.mm(ps, aT[:, kc, :], wo[:, kc, half * 512:(half + 1) * 512], kc == 0, kc == 7, [aTB, woB], [psB])
                k.tt(ot[:, half * 512:(half + 1) * 512], ps, G1[:, half * 512:(half + 1) * 512], ALU.mult, [psB, G1B], [oB])
            k.tt(ot, ot, xt, ALU.add, [oB, xB], [oB], eng="pool")
            k.dma("sp", x3[tl * 128:(tl + 1) * 128, :], ot, x3B, oB)
    S.barrier()
    if stop_after <= 8:
        S.finish()
        return k
    moe(1, [(tl, 0) for tl in range(NTL)], lambda tl: (x3[tl * 128:(tl + 1) * 128, :], x3B),
        lambda tl: out[tl * 128:(tl + 1) * 128, :], obuf, True)
    S.finish()
    return k


_CACHE = {}


def kernel(**inputs):
    T = inputs["x"].shape[1]
    if T not in _CACHE:
        _CACHE[T] = build(T)
    kb = _CACHE[T]
    nb = inputs["x"].shape[0]
    squeeze = ("rec_", "lru_", "s5_", "na_")
    shared = {}
    for name, v in inputs.items():
        if name in ("x", "c", "ctx"):
            continue
        a = np.asarray(v, dtype=np.float32)
        if name.startswith(squeeze):
            a = a[0]
        shared[name] = np.ascontiguousarray(a)
    in_maps = []
    for b in range(nb):
        m = dict(shared)
        m["x"] = np.ascontiguousarray(np.asarray(inputs["x"][b], dtype=np.float32))
        m["c"] = np.ascontiguousarray(np.asarray(inputs["c"][b], dtype=np.float32))
        m["ctx"] = np.ascontiguousarray(np.asarray(inputs["ctx"][b], dtype=np.float32))
        in_maps.append({kk: vv for kk, vv in m.items() if kk in kb.inp})
    res = run_bass_kernel_spmd(kb.nc, in_maps, core_ids=list(range(nb)))
    return np.stack([np.asarray(res.results[b]["out"]) for b in range(nb)], axis=0).astype(np.float32)
```

```python
import math
import os
import numpy as np
import concourse.bass as bass
import concourse.mybir as mybir
from concourse.bass_utils import run_bass_kernel_spmd

F32 = mybir.dt.float32
BF16 = mybir.dt.bfloat16
I32 = mybir.dt.int32
AF = mybir.ActivationFunctionType
ALU = mybir.AluOpType
AX = mybir.AxisListType
SEM_ROT = 30000
D = 1024
CT = 256
LCH = 128


class Buf:
    __slots__ = ("name", "w", "r", "dsem", "dcnt", "gen")

    def __init__(self, name):
        self.name = name
        self.w = None
        self.r = []
        self.dsem = None
        self.dcnt = 0
        self.gen = -1


class Sched:
    def __init__(self, nc):
        self.nc = nc
        self.eobj = {"pe": nc.tensor, "act": nc.scalar, "dve": nc.vector, "pool": nc.gpsimd, "sp": nc.sync}
        self.prog = {k: [] for k in self.eobj}
        self.sem = {}
        self.cnt = {}
        self.seen = {k: {} for k in self.eobj}
        self.nsem = 0
        self.dsemval = {}
        self.dsems = []
        self.allsems = []
        self.freed = []
        self.freed_sw = []
        self.semsw = {}
        self.gen = 0
        for k in self.eobj:
            self._newsem(k)
        self.ninstr = {k: 0 for k in self.eobj}

    def _alloc_sem(self, name):
        self.nsem += 1
        s = self.nc.alloc_semaphore(name=name)
        return s

    def _newsem(self, k):
        self.sem[k] = self._alloc_sem(f"e_{k}_{self.nsem}")
        self.cnt[k] = 0
        self.allsems.append((k, self.sem[k]))

    def _wait(self, eng, ev):
        sem, val = ev
        sid = id(sem)
        if sid in self.dsemval:
            val = self.dsemval[sid]
        if self.seen[eng].get(sid, 0) >= val:
            return
        self.seen[eng][sid] = val
        self.prog[eng].append(lambda e, sem=sem, val=val: e.wait_ge(sem, val))

    def op(self, eng, fn, reads=(), writes=()):
        my = self.sem[eng]
        pe = eng == "pe"
        for b in reads:
            if b.w is not None and not (pe and b.w[0] is my):
                self._wait(eng, b.w)
        for b in writes:
            if b.w is not None and not (pe and b.w[0] is my):
                self._wait(eng, b.w)
            for ev in b.r:
                if not (pe and ev[0] is my):
                    self._wait(eng, ev)
        if self.cnt[eng] >= SEM_ROT:
            self._newsem(eng)
            my = self.sem[eng]
        self.cnt[eng] += 1
        ev = (my, self.cnt[eng])
        self.prog[eng].append(lambda e, fn=fn, my=my: fn(e).then_inc(my, 1))
        self.ninstr[eng] += 1
        for b in writes:
            b.w = ev
            b.r = []
        for b in reads:
            if b not in writes:
                b.r.append(ev)
                if len(b.r) > 16:
                    b.r = b.r[-16:]
        return ev

    def dma(self, q, out_ap, in_ap, dst, src, **kw):
        b = dst if dst is not None else src
        sw = q == "pool"
        if b.dsem is None or b.gen != self.gen or b.dcnt * 16 >= SEM_ROT or self.semsw.get(id(b.dsem)) != sw:
            pool_ = self.freed_sw if sw else self.freed
            while pool_ and pool_[-1][1] * 16 >= SEM_ROT - 4000:
                pool_.pop()
            if pool_:
                b.dsem, b.dcnt = pool_.pop()
            else:
                b.dsem = self._alloc_sem(f"d_{b.name}_{self.nsem}")
                b.dcnt = 0
            self.semsw[id(b.dsem)] = sw
            b.gen = self.gen
            self.dsems.append(b.dsem)
        if src is not None and src.w is not None:
            self._wait(q, src.w)
        if dst is not None:
            if dst.w is not None:
                self._wait(q, dst.w)
            for ev in dst.r:
                self._wait(q, ev)
        b.dcnt += 1
        sem = b.dsem
        ev = (sem, b.dcnt * 16)
        self.dsemval[id(sem)] = b.dcnt * 16
        self.prog[q].append(
            lambda e, o=out_ap, i=in_ap, sem=sem, kw=kw: e.dma_start(out=o, in_=i, **kw).then_inc(sem, 16))
        self.ninstr[q] += 1
        if dst is not None:
            dst.w = ev
            dst.r = []
        if src is not None:
            src.r.append(ev)
        return ev

    def barrier(self):
        evs = [(self.sem[k], self.cnt[k]) for k in self.eobj if self.cnt[k] > 0]
        devs = [(s, self.dsemval[id(s)]) for s in self.dsems]
        for e in self.eobj:
            for ev in evs:
                if ev[0] is self.sem[e] and e == "pe":
                    continue
                self._wait(e, ev)
            for ev in devs:
                self._wait(e, ev)
        for sm_ in self.dsems:
            (self.freed_sw if self.semsw[id(sm_)] else self.freed).append((sm_, self.dsemval[id(sm_)] // 16))
        self.dsems = []
        self.gen += 1

    def finish(self):
        self.barrier()
        nc = self.nc
        prog = self.prog
        with nc.Block() as block:
            @block.sync
            def _(e):
                for t in prog["sp"]:
                    t(e)

            @block.tensor
            def _(e):
                for t in prog["pe"]:
                    t(e)

            @block.scalar
            def _(e):
                for t in prog["act"]:
                    t(e)

            @block.vector
            def _(e):
                for t in prog["dve"]:
                    t(e)

            @block.gpsimd
            def _(e):
                for t in prog["pool"]:
                    t(e)


def rv(ap):
    a = [list(x) for x in ap.ap]
    st, n = a[-1]
    a[-1] = [-st, n]
    return bass.AP(ap.tensor, ap.offset + st * (n - 1), a)


class K:
    def __init__(self, T, dbg=()):
        self.T = T
        self.TT = T + CT
        self.NT = self.TT // 128
        self.dbg = set(dbg)
        self.nc = bass.Bass("TRN2", target_bir_lowering=False)
        self.S = Sched(self.nc)
        self.uid = 0
        self.inp = {}
        self.pending = []

    def din(self, name, shape):
        t = self.nc.dram_tensor(name, list(shape), F32, kind="ExternalInput").ap()
        self.inp[name] = t
        return t

    def dscr(self, name, shape, dt=F32):
        kind = "ExternalOutput" if name in self.dbg else "Internal"
        return self.nc.dram_tensor(name, list(shape), dt, kind=kind).ap(), Buf(name)

    def sb(self, st, name, shape, dt=F32):
        self.uid += 1
        t = st.enter_context(self.nc.sbuf_tensor(f"{name}_{self.uid}", list(shape), dt))
        return t.ap(), Buf(name)

    def act(self, out, in_, func, r, w, scale=None, bias=None, accum=None):
        kw = {}
        if scale is not None:
            kw["scale"] = scale
        if bias is not None:
            kw["bias"] = bias
        if accum is not None:
            kw["accum_out"] = accum
        return self.S.op("act", lambda e: e.activation(out=out, in_=in_, func=func, **kw), r, w)

    def tt(self, out, a, b, op, r, w, eng="dve"):
        return self.S.op(eng, lambda e: e.tensor_tensor(out=out, in0=a, in1=b, op=op), r, w)

    def ts(self, out, a, s1, op0, r, w, s2=None, op1=None, eng="dve"):
        if op1 is None:
            return self.S.op(eng, lambda e: e.tensor_scalar(out=out, in0=a, scalar1=s1, scalar2=None, op0=op0), r, w)
        return self.S.op(eng, lambda e: e.tensor_scalar(out=out, in0=a, scalar1=s1, scalar2=s2, op0=op0, op1=op1), r, w)

    def stt(self, out, a, s, b, op0, op1, r, w):
        return self.S.op("dve", lambda e: e.scalar_tensor_tensor(out=out, in0=a, scalar=s, in1=b, op0=op0, op1=op1), r, w)

    def cp(self, out, in_, r, w, eng="dve"):
        if eng == "act":
            return self.S.op("act", lambda e: e.copy(out=out, in_=in_), r, w)
        return self.S.op(eng, lambda e: e.tensor_copy(out=out, in_=in_), r, w)

    def ms(self, ap, val, w, eng="pool"):
        return self.S.op(eng, lambda e: e.memset(ap, val), (), w)

    def mm(self, out, lhsT, rhs, start, stop, r, w, skip=False):
        return self.S.op("pe", lambda e: e.matmul(out, lhsT, rhs, start=start, stop=stop, skip_group_check=skip), r, w)

    def tr(self, out, in_, ident, r, w):
        return self.S.op("pe", lambda e: e.transpose(out, in_, ident), r, w)

    def scan(self, out, d0, d1, init, r, w):
        return self.S.op("dve", lambda e: e.tensor_tensor_scan(out=out, data0=d0, data1=d1, initial=init,
                                                               op0=ALU.mult, op1=ALU.add), r, w)

    def dma(self, q, out, in_, dst, src, **kw):
        return self.S.dma(q, out, in_, dst, src, **kw)

    def defer(self, *a, **kw):
        self.pending.append((a, kw))

    def flush(self):
        for a, kw in self.pending:
            self.S.dma(*a, **kw)
        self.pending = []


def build(T=8192, dbg=(), stop_after=99):
    from contextlib import ExitStack
    k = K(T, dbg)
    nc, S = k.nc, k.S
    TT, NT = k.TT, k.NT
    x = k.din("x", [T, D]); c = k.din("c", [D]); ctx = k.din("ctx", [CT, D]); c_ctx = k.din("c_ctx", [D])
    ada_w = k.din("ada_w", [2, D, 6 * D]); ada_b = k.din("ada_b", [2, 6 * D])
    norm1_g = k.din("norm1_g", [2, D]); norm2_g = k.din("norm2_g", [2, D])
    rec_w_in = k.din("rec_w_in", [D, 1536]); rec_conv_w = k.din("rec_conv_w", [4, 512]); rec_conv_b = k.din("rec_conv_b", [512])
    lru_wa = k.din("lru_wa", [2, 8, 64, 64]); lru_ba = k.din("lru_ba", [2, 512])
    lru_wx = k.din("lru_wx", [2, 8, 64, 64]); lru_bx = k.din("lru_bx", [2, 512]); lru_lambda = k.din("lru_lambda", [2, 512])
    s5_a_re = k.din("s5_a_re", [2, 32, 64]); s5_a_im = k.din("s5_a_im", [2, 32, 64]); s5_log_dt = k.din("s5_log_dt", [2, 32])
    s5_b_re = k.din("s5_b_re", [2, 32, 64, 16]); s5_b_im = k.din("s5_b_im", [2, 32, 64, 16])
    s5_c_re = k.din("s5_c_re", [2, 32, 16, 64]); s5_c_im = k.din("s5_c_im", [2, 32, 16, 64])
    s5_d = k.din("s5_d", [512]); s5_glu_w = k.din("s5_glu_w", [512, 512]); s5_glu_b = k.din("s5_glu_b", [512])
    rec_w_out = k.din("rec_w_out", [D, D])
    na_w_qkv = k.din("na_w_qkv", [D, 3 * D]); na_w_out = k.din("na_w_out", [D, D]); na_rpb = k.din("na_rpb", [16, 15, 31])
    moe_r1_w = k.din("moe_r1_w", [2, D, 4]); moe_r1_b = k.din("moe_r1_b", [2, 4])
    moe_r2_w = k.din("moe_r2_w", [2, 4, D, 8]); moe_r2_b = k.din("moe_r2_b", [2, 4, 8])
    moe_w_gate = k.din("moe_w_gate", [2, 32, D, 512]); moe_w_up = k.din("moe_w_up", [2, 32, D, 512])
    moe_w_down = k.din("moe_w_down", [2, 32, 512, D]); final_norm_g = k.din("final_norm_g", [D])
    out = nc.dram_tensor("out", [T, D], F32, kind="ExternalOutput").ap()
    obuf = Buf("out")

    modv, modvB = k.dscr("modv", [2, 2, 6 * D])
    projT, projTB = k.dscr("projT", [1536, TT])
    ymixT, ymixTB = k.dscr("ymixT", [512, TT])
    ysgT, ysgTB = k.dscr("ysgT", [512, TT])
    x1, x1B = k.dscr("x1", [TT, D])
    x2, x2B = k.dscr("x2", [TT, D])
    x3, x3B = k.dscr("x3", [T, D])

    gst = ExitStack()
    psb = []
    for i in range(8):
        t = gst.enter_context(nc.psum_tensor(f"psb{i}", [128, 512], F32))
        psb.append((t.ap(), Buf(f"psb{i}")))
    ident, identB = k.sb(gst, "ident", [128, 128])
    epsb, epsB = k.sb(gst, "epsb", [128, 1])
    halfpi, halfpiB = k.sb(gst, "halfpi", [128, 1])
    k.ms(ident, 0.0, [identB])
    S.op("pool", lambda e: e.memset(epsb, 1e-6), (), [epsB])
    S.op("pool", lambda e: e.memset(halfpi, math.pi / 2), (), [halfpiB])
    ones_t, onesB = k.sb(gst, "ones", [128, 128])
    k.ms(ones_t, 1.0, [onesB])
    S.op("pool", lambda e: e.affine_select(out=ident, in_=ones_t, pattern=[[-1, 128]], compare_op=ALU.is_equal,
                                            fill=0.0, base=0, channel_multiplier=1), [onesB], [identB])

    def xsrc(ti):
        if ti < 2:
            return ctx[ti * 128:(ti + 1) * 128, :], 1
        return x[(ti - 2) * 128:(ti - 1) * 128, :], 0

    with ExitStack() as st:
        cs, csB = k.sb(st, "cs", [128, 2, 8])
        srep, srepB = k.sb(st, "srep", [128, 2, 8, 128])
        k.dma("sp", cs[:, 0, :], c.rearrange("(kc p) -> p kc", p=128), csB, None, allow_slow_non_contiguous=True)
        k.dma("sp", cs[:, 1, :], c_ctx.rearrange("(kc p) -> p kc", p=128), csB, None, allow_slow_non_contiguous=True)
        k.act(cs, cs, AF.Silu, [csB], [csB])
        k.cp(srep, cs.unsqueeze(3).broadcast_to([128, 2, 8, 128]), [csB], [srepB])
        adab, adabB = k.sb(st, "adab", [128, 6 * D])
        modt = [k.sb(st, f"modt{i}", [128, 6 * D]) for i in range(2)]
        gb = [k.sb(st, f"gb{i}", [128, D]) for i in range(2)]
        wblk = [k.sb(st, f"wblk{i}", [128, 8, 512]) for i in range(2)]
        for layer in range(2):
            k.dma("sp", adab, ada_b[layer, :].partition_broadcast(128), adabB, None)
            k.dma("sp", gb[0][0], norm1_g[layer, :].partition_broadcast(128), gb[0][1], None)
            k.dma("sp", gb[1][0], norm2_g[layer, :].partition_broadcast(128), gb[1][1], None)
            for nb in range(12):
                wt, wB = wblk[nb % 2]
                k.dma("sp", wt, ada_w[layer, :, nb * 512:(nb + 1) * 512].rearrange("(kc p) n -> p kc n", p=128), wB, None)
                for kind in range(2):
                    ps, psB = psb[(nb * 2 + kind) % 4]
                    for kc in range(8):
                        k.mm(ps, srep[:, kind, kc, :], wt[:, kc, :], kc == 0, kc == 7, [srepB, wB], [psB])
                    k.tt(modt[kind][0][:, nb * 512:(nb + 1) * 512], ps, adab[:, nb * 512:(nb + 1) * 512], ALU.add,
                         [psB, adabB], [modt[kind][1]])
            for kind in range(2):
                mt, mB = modt[kind]
                k.stt(mt[:, D:2 * D], mt[:, D:2 * D], 1.0, gb[0][0], ALU.add, ALU.mult, [mB, gb[0][1]], [mB])
                k.stt(mt[:, 4 * D:5 * D], mt[:, 4 * D:5 * D], 1.0, gb[1][0], ALU.add, ALU.mult, [mB, gb[1][1]], [mB])
                k.dma("sp", modv[layer, kind, :].rearrange("(o n) -> o n", o=1), mt[0:1, :], modvB, mB)
    S.barrier()

    def load_mod(st, q, layer, kind, slot, name):
        t, B = k.sb(st, name, [128, D])
        k.dma(q, t, modv[layer, kind, slot * D:(slot + 1) * D].partition_broadcast(128), B, modvB)
        return t, B

    def norm_mod(xt, xB, ht, hB, At, AB, Bt, BB, tmp):
        jt, jB, ss, ssB = tmp
        k.act(jt, xt, AF.Square, [xB], [jB, ssB], accum=ss)
        k.act(ss, ss, AF.Sqrt, [ssB, epsB], [ssB], scale=1.0 / D, bias=epsb)
        S.op("dve", lambda e: e.reciprocal(out=ss, in_=ss), [ssB], [ssB])
        k.stt(ht, xt, ss, At, ALU.mult, ALU.mult, [xB, ssB, AB], [hB])
        k.tt(ht, ht, Bt, ALU.add, [hB, BB], [hB], eng="pool")

    def groups_of(tiles, gsz=4):
        gs = []
        ctxs = [t for t in tiles if t < 2]
        lat = [t for t in tiles if t >= 2]
        if ctxs:
            gs.append(ctxs)
        for i in range(0, len(lat), gsz):
            gs.append(lat[i:i + gsz])
        return gs

    with ExitStack() as st:
        win, winB = k.sb(st, "win", [128, 8, 1536], BF16)
        k.dma("pool", win, rec_w_in.rearrange("(kc p) n -> p kc n", p=128), winB, None)
        AA = [load_mod(st, "sp", 0, kd, 1, f"A{kd}") for kd in range(2)]
        BBm = [load_mod(st, "sp", 0, kd, 0, f"B{kd}") for kd in range(2)]
        xt2 = [k.sb(st, f"xt{i}", [128, D]) for i in range(2)]
        ht2 = [k.sb(st, f"ht{i}", [128, D]) for i in range(2)]
        jt, jB = k.sb(st, "jt", [128, D])
        ss2 = [k.sb(st, f"ss{i}", [128, 1]) for i in range(2)]
        hT2 = [k.sb(st, f"hT{i}", [128, 8, 512], BF16) for i in range(2)]
        stg2 = [k.sb(st, f"stg{i}", [128, 12, 512]) for i in range(2)]
        tcount = 0
        for gi, g in enumerate(groups_of(list(range(NT)))):
            hT, hTB = hT2[gi % 2]
            n = 128 * len(g)
            col0 = g[0] * 128
            for si, ti in enumerate(g):
                xt, xB = xt2[tcount % 2]; ht, hB = ht2[tcount % 2]; ss, ssB = ss2[tcount % 2]
                src, kd = xsrc(ti)
                k.dma("sp", xt, src, xB, None)
                if si == min(1, len(g) - 1):
                    k.flush()
                norm_mod(xt, xB, ht, hB, AA[kd][0], AA[kd][1], BBm[kd][0], BBm[kd][1], (jt, jB, ss, ssB))
                for b in range(2):
                    ps, psB = psb[(tcount * 2 + b) % 4]
                    for j in range(4):
                        kc = b * 4 + j
                        k.tr(ps[:, j * 128:(j + 1) * 128], ht[:, kc * 128:(kc + 1) * 128], ident, [hB, identB], [psB])
                    k.cp(hT[:, b * 4:(b + 1) * 4, si * 128:(si + 1) * 128], ps.rearrange("p (j t) -> p j t", j=4),
                         [psB], [hTB], eng=("act" if b == 0 else "dve"))
                tcount += 1
            stg, stgB = stg2[gi % 2]
            for oc in range(12):
                ps, psB = psb[4 + oc % 4]
                for kc in range(8):
                    k.mm(ps[:, :n], win[:, kc, oc * 128:(oc + 1) * 128], hT[:, kc, :n], kc == 0, kc == 7, [winB, hTB], [psB])
                k.cp(stg[:, oc, :n], ps[:, :n], [psB], [stgB], eng=("act" if oc % 2 == 0 else "dve"))
            k.defer("sp", projT[:, col0:col0 + n].rearrange("(oc p) t -> p oc t", p=128), stg[:, :, :n], projTB, stgB)
        k.flush()
    S.barrier()

    def blocks_fwd():
        bl = [(0, 256)]
        for c0 in range(256, TT, 512):
            bl.append((c0, 512))
        return bl

    with ExitStack() as st:
        cw, cwB = k.sb(st, "cw", [128, 4, 4])
        cb, cbB = k.sb(st, "cb", [128, 4])
        gba, gbaB = k.sb(st, "gba", [128, 2, 4]); gbx, gbxB = k.sb(st, "gbx", [128, 2, 4])
        lam, lamB = k.sb(st, "lam", [128, 2, 4])
        k.dma("sp", cw, rec_conv_w.rearrange("k (j p) -> p k j", p=128), cwB, None, allow_slow_non_contiguous=True)
        k.dma("sp", cb, rec_conv_b.rearrange("(j p) -> p j", p=128), cbB, None, allow_slow_non_contiguous=True)
        k.dma("sp", gba, lru_ba.rearrange("d (j p) -> p d j", p=128), gbaB, None, allow_slow_non_contiguous=True)
        k.dma("sp", gbx, lru_bx.rearrange("d (j p) -> p d j", p=128), gbxB, None, allow_slow_non_contiguous=True)
        k.dma("sp", lam, lru_lambda.rearrange("d (j p) -> p d j", p=128), lamB, None, allow_slow_non_contiguous=True)
        t1, t1B = k.sb(st, "t1", [128, 2, 4]); t2, t2B = k.sb(st, "t2", [128, 2, 4]); t3, t3B = k.sb(st, "t3", [128, 2, 4])
        cl, clB = k.sb(st, "cl", [128, 2, 4]); cl2, cl2B = k.sb(st, "cl2", [128, 2, 4])
        k.act(t1, lam, AF.Abs, [lamB], [t1B])
        k.act(t2, t1, AF.Exp, [t1B], [t2B], scale=-1.0)
        k.ts(t3, t2, 2.0, ALU.add, [t2B], [t3B])
        S.op("dve", lambda e: e.reciprocal(out=t3, in_=t3), [t3B], [t3B])
        k.tt(t2, t2, t3, ALU.mult, [t2B, t3B], [t2B])
        k.tt(t3, t2, t2, ALU.mult, [t2B], [t3B])
        k.ts(t1, t3, 1.0 / 9, ALU.mult, [t3B], [t1B], s2=1.0 / 7, op1=ALU.add)
        for cf in (1.0 / 5, 1.0 / 3, 1.0):
            k.tt(t1, t1, t3, ALU.mult, [t1B, t3B], [t1B])
            k.ts(t1, t1, cf, ALU.add, [t1B], [t1B])
        k.tt(t1, t1, t2, ALU.mult, [t1B, t2B], [t1B])
        k.ts(t2, lam, -1.0, ALU.mult, [lamB], [t2B], s2=0.0, op1=ALU.max)
        k.stt(t1, t1, 2.0, t2, ALU.mult, ALU.add, [t1B, t2B], [t1B])
        k.ts(cl, t1, -8.0, ALU.mult, [t1B], [clB])
        k.ts(cl2, t1, -16.0, ALU.mult, [t1B], [cl2B])
        wg, wgB = k.sb(st, "wg", [128, 2, 2, 4, 128])
        k.ms(wg, 0.0, [wgB])
        for gi_, wsrc in enumerate((lru_wa, lru_wx)):
            for d in range(2):
                for h in range(8):
                    r0 = (h % 2) * 64
                    k.dma("sp", wg[r0:r0 + 64, gi_, d, h // 2, r0:r0 + 64], wsrc[d, h, :, :], wgB, None)
        u, uB = k.sb(st, "u", [128, TT]); ya, yaB = k.sb(st, "ya", [128, TT])
        NB = 2
        rt = [k.sb(st, f"rt{i}", [128, 512]) for i in range(NB)]
        it = [k.sb(st, f"it{i}", [128, 512]) for i in range(NB)]
        at = [k.sb(st, f"at{i}", [128, 512]) for i in range(NB)]
        sq = [k.sb(st, f"sq{i}", [128, 512]) for i in range(NB)]
        hb = [k.sb(st, f"hb{i}", [128, 512]) for i in range(NB)]
        zero1, zero1B = k.sb(st, "zero1", [128, 1])
        k.ms(zero1, 0.0, [zero1B])
        for j in range(4):
            k.dma("sp", ya, projT[j * 128:(j + 1) * 128, :], yaB, projTB)
            for (s0, s1) in ((0, 256), (256, TT)):
                k.act(u[:, s0:s1], ya[:, s0:s1], AF.Identity, [yaB, cwB, cbB], [uB], scale=cw[:, 2, j:j + 1], bias=cb[:, j:j + 1])
                k.stt(u[:, s0 + 2:s1], ya[:, s0:s1 - 2], cw[:, 0, j:j + 1], u[:, s0 + 2:s1], ALU.mult, ALU.add, [yaB, cwB, uB], [uB])
                k.stt(u[:, s0 + 1:s1], ya[:, s0:s1 - 1], cw[:, 1, j:j + 1], u[:, s0 + 1:s1], ALU.mult, ALU.add, [yaB, cwB, uB], [uB])
                k.stt(u[:, s0:s1 - 1], ya[:, s0 + 1:s1], cw[:, 3, j:j + 1], u[:, s0:s1 - 1], ALU.mult, ALU.add, [yaB, cwB, uB], [uB])
            bi = 0
            for d in range(2):
                bl = blocks_fwd()
                if d == 1:
                    bl = [bl[0]] + bl[1:][::-1]
                carry = (zero1, zero1B)
                for (c0, n) in bl:
                    pa, paB = psb[(bi * 2) % 8]; px, pxB = psb[(bi * 2 + 1) % 8]
                    r_, rB = rt[bi % NB]; i_, iB = it[bi % NB]; a_, aB = at[bi % NB]; s_, sB = sq[bi % NB]; h_, hB = hb[bi % NB]
                    ub = u[:, c0:c0 + n]
                    k.mm(pa[:, :n], wg[:, 0, d, j, :], ub, True, True, [wgB, uB], [paB])
                    k.mm(px[:, :n], wg[:, 1, d, j, :], ub, True, True, [wgB, uB], [pxB])
                    k.act(r_[:, :n], pa[:, :n], AF.Sigmoid, [paB, gbaB], [rB], bias=gba[:, d, j:j + 1])
                    k.act(i_[:, :n], px[:, :n], AF.Sigmoid, [pxB, gbxB], [iB], bias=gbx[:, d, j:j + 1])
                    k.act(a_[:, :n], r_[:, :n], AF.Exp, [rB, clB], [aB], scale=cl[:, d, j:j + 1])
                    k.act(s_[:, :n], r_[:, :n], AF.Exp, [rB, cl2B], [sB], scale=cl2[:, d, j:j + 1])
                    k.ts(s_[:, :n], s_[:, :n], -1.0, ALU.mult, [sB], [sB], s2=1.0, op1=ALU.add, eng="pool")
                    k.act(s_[:, :n], s_[:, :n], AF.Sqrt, [sB], [sB])
                    k.tt(i_[:, :n], i_[:, :n], ub, ALU.mult, [iB, uB], [iB])
                    k.tt(i_[:, :n], i_[:, :n], s_[:, :n], ALU.mult, [iB, sB], [iB])
                    if d == 0:
                        k.scan(ya[:, c0:c0 + n], a_[:, :n], i_[:, :n], carry[0], [aB, iB, carry[1]], [yaB])
                        carry = (ya[:, c0 + n - 1:c0 + n], yaB)
                    else:
                        k.scan(rv(h_[:, :n]), rv(a_[:, :n]), rv(i_[:, :n]), carry[0], [aB, iB, carry[1]], [hB])
                        carry = (h_[:, 0:1], hB)
                        k.tt(ya[:, c0:c0 + n], ya[:, c0:c0 + n], h_[:, :n], ALU.add, [yaB, hB], [yaB], eng="pool")
                    bi += 1
            for (c0, n) in blocks_fwd():
                g_, gB = rt[bi % NB]
                k.dma("sp", g_[:, :n], projT[512 + j * 128:512 + (j + 1) * 128, c0:c0 + n], gB, projTB)
                k.act(g_[:, :n], g_[:, :n], AF.Gelu, [gB], [gB])
                k.tt(ya[:, c0:c0 + n], ya[:, c0:c0 + n], g_[:, :n], ALU.mult, [yaB, gB], [yaB])
                bi += 1
            k.dma("sp", ymixT[j * 128:(j + 1) * 128, :], ya, ymixTB, yaB)
    S.barrier()

    if stop_after <= 2:
        S.finish()
        return k
    L = LCH
    with ExitStack() as st:
        BT, BTB = k.sb(st, "BT", [128, 2, 2, 4, 128])
        Cpad, CpadB = k.sb(st, "Ccomp", [128, 2, 2, 16, 32])
        maskc, maskcB = k.sb(st, "maskc", [128, 4])
        CTAB, CTABB = k.sb(st, "CTAB", [128, 2, 16, L]); STAB, STABB = k.sb(st, "STAB", [128, 2, 16, L])
        dcol, dcolB = k.sb(st, "dcol", [128, 4])
        mag, magB = k.sb(st, "mag", [128, 2, 16]); nsL, nsLB = k.sb(st, "nsL", [128, 2, 16])
        W2, W2B = k.sb(st, "W2", [128, 2, 16, 2])
        stp = ExitStack()
        are, areB = k.sb(stp, "are", [128, 2, 16]); aim, aimB = k.sb(stp, "aim", [128, 2, 16]); ldt, ldtB = k.sb(stp, "ldt", [128, 2, 16])
        bre, breB = k.sb(stp, "bre", [128, 2, 16, 16]); bim, bimB = k.sb(stp, "bim", [128, 2, 16, 16])
        cre, creB = k.sb(stp, "cre", [128, 2, 16, 16]); cim, cimB = k.sb(stp, "cim", [128, 2, 16, 16])
        for d in range(2):
            k.dma("sp", are[:, d, :], s5_a_re[d].rearrange("(gp g2) n -> (g2 n) gp", g2=2), areB, None, allow_slow_non_contiguous=True)
            k.dma("sp", aim[:, d, :], s5_a_im[d].rearrange("(gp g2) n -> (g2 n) gp", g2=2), aimB, None, allow_slow_non_contiguous=True)
            k.dma("sp", bre[:, d, :, :], s5_b_re[d].rearrange("(gp g2) n p -> (g2 n) gp p", g2=2), breB, None)
            k.dma("sp", bim[:, d, :, :], s5_b_im[d].rearrange("(gp g2) n p -> (g2 n) gp p", g2=2), bimB, None)
            for g2 in range(2):
                src = bass.AP(s5_log_dt.tensor, s5_log_dt.offset + d * 32 + g2, [[0, 64], [2, 16]])
                k.dma("sp", ldt[g2 * 64:(g2 + 1) * 64, d, :], src, ldtB, None, allow_slow_non_contiguous=True)
                for (ct_, cB_, csrc) in ((cre, creB, s5_c_re), (cim, cimB, s5_c_im)):
                    for gp_ in range(16):
                        src = bass.AP(csrc.tensor, csrc.offset + d * 32768 + (2 * gp_ + g2) * 1024, [[1, 64], [64, 16]])
                        k.dma("sp", ct_[g2 * 64:(g2 + 1) * 64, d, gp_, :], src, cB_, None, allow_slow_non_contiguous=True)
        nm = [0]
        def sm(shape=[128, 2, 16], dt=F32):
            nm[0] += 1
            return k.sb(stp, f"sm{nm[0]}", shape, dt)
        dtt, dttB = sm(); th, thB = sm(); ki, kiB = sm(dt=I32); kf, kfB = sm()
        sh, shB = sm(); chh, chhB = sm(); sn, snB = sm(); cs_, csB_ = sm(); lbr, lbrB = sm(); lbi, lbiB = sm()
        den, denB = sm(); tq, tqB = sm(); qre, qreB = sm(); qim, qimB = sm()
        k.act(dtt, ldt, AF.Exp, [ldtB], [dttB])
        k.tt(mag, are, dtt, ALU.mult, [areB, dttB], [magB])
        k.act(mag, mag, AF.Exp, [magB], [magB])
        k.tt(th, aim, dtt, ALU.mult, [aimB, dttB], [thB])
        k.ts(th, th, 1.0 / (2 * math.pi), ALU.mult, [thB], [thB])
        k.cp(ki, th, [thB], [kiB]); k.cp(kf, ki, [kiB], [kfB])
        k.tt(th, th, kf, ALU.subtract, [thB, kfB], [thB])
        k.act(sh, th, AF.Sin, [thB], [shB], scale=math.pi)
        k.act(kf, th, AF.Abs, [thB], [kfB])
        k.act(chh, kf, AF.Sin, [kfB, halfpiB], [chhB], scale=-math.pi, bias=halfpi)
        k.stt(sn, sh, 2.0, chh, ALU.mult, ALU.mult, [shB, chhB], [snB])
        k.tt(cs_, sh, sh, ALU.mult, [shB], [csB_])
        k.ts(cs_, cs_, -2.0, ALU.mult, [csB_], [csB_], s2=1.0, op1=ALU.add)
        k.tt(lbr, mag, cs_, ALU.mult, [magB, csB_], [lbrB]); k.tt(lbi, mag, sn, ALU.mult, [magB, snB], [lbiB])
        k.tt(den, are, are, ALU.mult, [areB], [denB]); k.tt(tq, aim, aim, ALU.mult, [aimB], [tqB])
        k.tt(den, den, tq, ALU.add, [denB, tqB], [denB])
        S.op("dve", lambda e: e.reciprocal(out=den, in_=den), [denB], [denB])
        k.ts(lbr, lbr, -1.0, ALU.add, [lbrB], [lbrB])
        k.tt(qre, lbr, are, ALU.mult, [lbrB, areB], [qreB]); k.tt(tq, lbi, aim, ALU.mult, [lbiB, aimB], [tqB])
        k.tt(qre, qre, tq, ALU.add, [qreB, tqB], [qreB]); k.tt(qre, qre, den, ALU.mult, [qreB, denB], [qreB])
        k.tt(qim, lbi, are, ALU.mult, [lbiB, areB], [qimB]); k.tt(tq, lbr, aim, ALU.mult, [lbrB, aimB], [tqB])
        k.tt(qim, qim, tq, ALU.subtract, [qimB, tqB], [qimB]); k.tt(qim, qim, den, ALU.mult, [qimB, denB], [qimB])
        Bblk, BblkB = k.sb(stp, "Bblk", [128, 2, 2, 16, 32])
        k.ms(Bblk, 0.0, [BblkB])
        pA, pAB = sm([128, 2, 16, 16]); pB_, pBB = sm([128, 2, 16, 16])
        qreb = qre.unsqueeze(3).broadcast_to([128, 2, 16, 16]); qimb = qim.unsqueeze(3).broadcast_to([128, 2, 16, 16])
        k.tt(pA, bre, qreb, ALU.mult, [breB, qreB], [pAB]); k.tt(pB_, bim, qimb, ALU.mult, [bimB, qimB], [pBB])
        for g2 in range(2):
            hs = slice(g2 * 64, (g2 + 1) * 64)
            k.tt(Bblk[hs, :, 0, :, g2 * 16:(g2 + 1) * 16], pA[hs], pB_[hs], ALU.subtract, [pAB, pBB], [BblkB])
        k.tt(pA, bim, qreb, ALU.mult, [bimB, qreB], [pAB]); k.tt(pB_, bre, qimb, ALU.mult, [breB, qimB], [pBB])
        for g2 in range(2):
            hs = slice(g2 * 64, (g2 + 1) * 64)
            k.tt(Bblk[hs, :, 1, :, g2 * 16:(g2 + 1) * 16], pA[hs], pB_[hs], ALU.add, [pAB, pBB], [BblkB])
        ti_ = 0
        for d in range(2):
            for ri in range(2):
                for c_ in range(4):
                    ps, psB = psb[ti_ % 4]; ti_ += 1
                    k.tr(ps[:, 0:128], Bblk[:, d, ri, 4 * c_:4 * c_ + 4, :].rearrange("p a b -> p (a b)"), ident, [BblkB, identB], [psB])
                    k.cp(BT[:, d, ri, c_, :], ps[:, 0:128], [psB], [BTB])
        k.ms(Cpad, 0.0, [CpadB])
        for g2 in range(2):
            hs = slice(g2 * 64, (g2 + 1) * 64)
            k.cp(Cpad[hs, :, 0, :, g2 * 16:(g2 + 1) * 16], cre[hs], [creB], [CpadB])
            k.ts(Cpad[hs, :, 1, :, g2 * 16:(g2 + 1) * 16], cim[hs], -1.0, ALU.mult, [cimB], [CpadB])
        for gq in range(4):
            S.op("dve", lambda e, gq=gq: e.tensor_reduce(out=maskc[:, gq:gq + 1], in_=ident[:, gq * 32:(gq + 1) * 32], axis=AX.X, op=ALU.add), [identB], [maskcB])
        ta, taB = sm([128, 2, 16, L // 2]); tb, tbB = sm([128, 2, 16, L // 2])
        k.cp(CTAB[:, :, :, 0], cs_, [csB_], [CTABB]); k.cp(STAB[:, :, :, 0], sn, [snB], [STABB])
        m = 1
        while m < L:
            cm = CTAB[:, :, :, m - 1:m].broadcast_to([128, 2, 16, m]); smm = STAB[:, :, :, m - 1:m].broadcast_to([128, 2, 16, m])
            k.tt(ta[:, :, :, :m], CTAB[:, :, :, 0:m], cm, ALU.mult, [CTABB], [taB])
            k.tt(tb[:, :, :, :m], STAB[:, :, :, 0:m], smm, ALU.mult, [STABB], [tbB])
            k.tt(CTAB[:, :, :, m:2 * m], ta[:, :, :, :m], tb[:, :, :, :m], ALU.subtract, [taB, tbB], [CTABB])
            k.tt(ta[:, :, :, :m], STAB[:, :, :, 0:m], cm, ALU.mult, [STABB, CTABB], [taB])
            k.tt(tb[:, :, :, :m], CTAB[:, :, :, 0:m], smm, ALU.mult, [STABB, CTABB], [tbB])
            k.tt(STAB[:, :, :, m:2 * m], ta[:, :, :, :m], tb[:, :, :, :m], ALU.add, [taB, tbB], [STABB])
            m *= 2
        k.ts(nsL, STAB[:, :, :, L - 1], -1.0, ALU.mult, [STABB], [nsLB])
        k.cp(W2[:, :, :, 0], nsL, [nsLB], [W2B]); k.cp(W2[:, :, :, 1], STAB[:, :, :, L - 1], [STABB], [W2B])
        k.dma("sp", dcol, s5_d.rearrange("(j p) -> p j", p=128), dcolB, None, allow_slow_non_contiguous=True)
        S.barrier()
        stp.close()
        ubT, ubTB = k.sb(st, "ubT", [128, TT]); yacc, yaccB = k.sb(st, "yacc", [128, TT])
        NCH = 4
        ch = []
        for gq in range(NCH):
            dct = {nm_: k.sb(st, f"{nm_}{gq}", [128, 512]) for nm_ in ("um", "t1", "t2", "gre", "gim")}
            dct["ss"] = k.sb(st, f"ss{gq}", [128, 2, 512])
            dct["car"] = [k.sb(st, f"car{gq}_{i}", [128, 2]) for i in range(2)]
            dct["tmpc"] = k.sb(st, f"tmpc{gq}", [128, 2])
            dct["rho"] = k.sb(st, f"rho{gq}", [128, L])
            ch.append(dct)
        zero2, zero2B = k.sb(st, "zero2", [128, 2])
        k.ms(zero2, 0.0, [zero2B])
        for c_ in range(4):
            k.dma("sp", ubT, projT[1024 + c_ * 128:1024 + (c_ + 1) * 128, :], ubTB, projTB)
            k.ms(yacc, 0.0, [yaccB])
            for d in range(2):
                bl = blocks_fwd()
                if d == 1:
                    bl = [bl[0]] + bl[1:][::-1]
                for gq in range(NCH):
                    gp = 4 * c_ + gq
                    ch[gq]["cur"] = (zero2, zero2B); ch[gq]["cidx"] = 0
                    k.cp(ch[gq]["rho"][0], mag[:, d, gp:gp + 1].broadcast_to([128, L]), [magB], [ch[gq]["rho"][1]], eng="pool")
                for (c0, n) in bl:
                    nch = n // L
                    v3 = lambda t_: t_[:, :n].rearrange("p (a l) -> p a l", l=L)
                    tabs = []
                    for gq in range(NCH):
                        gp = 4 * c_ + gq
                        cosv = CTAB[:, d, gp, :]; sinv = STAB[:, d, gp, :]
                        if d == 1:
                            cosv = rv(cosv); sinv = rv(sinv)
                        tabs.append((cosv.unsqueeze(1).broadcast_to([128, nch, L]), sinv.unsqueeze(1).broadcast_to([128, nch, L])))
                    for gq in range(NCH):
                        C = ch[gq]; um, umB = C["um"]
                        pdr, pdrB = psb[2 * gq]; pdi, pdiB = psb[2 * gq + 1]
                        k.act(um[:, :n], ubT[:, c0:c0 + n], AF.Copy, [ubTB, maskcB], [umB], scale=maskc[:, gq:gq + 1])
                        k.mm(pdr[:, :n], BT[:, d, 0, c_, :], um[:, :n], True, True, [BTB, umB], [pdrB])
                        k.mm(pdi[:, :n], BT[:, d, 1, c_, :], um[:, :n], True, True, [BTB, umB], [pdiB])
                    for step in range(3):
                        for gq in range(NCH):
                            C = ch[gq]; cosb, sinb = tabs[gq]
                            pdr, pdrB = psb[2 * gq]; pdi, pdiB = psb[2 * gq + 1]
                            t1, t1B_ = C["t1"]; t2, t2B_ = C["t2"]; gre, greB = C["gre"]; gim, gimB = C["gim"]
                            if step == 0:
                                k.tt(v3(t1), v3(pdr), cosb, ALU.mult, [pdrB, CTABB], [t1B_])
                                k.tt(v3(t2), v3(pdi), sinb, ALU.mult, [pdiB, STABB], [t2B_])
                                k.tt(gre[:, :n], t1[:, :n], t2[:, :n], ALU.add, [t1B_, t2B_], [greB], eng="pool")
                            elif step == 1:
                                k.tt(v3(t1), v3(pdi), cosb, ALU.mult, [pdiB, CTABB], [t1B_])
                                k.tt(v3(t2), v3(pdr), sinb, ALU.mult, [pdrB, STABB], [t2B_])
                                k.tt(gim[:, :n], t1[:, :n], t2[:, :n], ALU.subtract, [t1B_, t2B_], [gimB], eng="pool")
                    order = list(range(nch)) if d == 0 else list(range(nch - 1, -1, -1))
                    for ci in order:
                        sl = slice(ci * L, (ci + 1) * L)
                        for op_ in range(4):
                            for gq in range(NCH):
                                C = ch[gq]; gp = 4 * c_ + gq
                                gre, greB = C["gre"]; gim, gimB = C["gim"]; ss, ssB = C["ss"]
                                rho, rhoB = C["rho"]; tmpc, tmpcB = C["tmpc"]; cur = C["cur"]
                                cLc = CTAB[:, d, gp, L - 1:L]
                                li_ = (ci + 1) * L - 1 if d == 0 else ci * L
                                last2 = ss[:, :, li_]
                                f = (lambda a_: a_) if d == 0 else rv
                                if op_ == 0:
                                    k.scan(f(ss[:, 0, sl]), rho, f(gre[:, sl]), cur[0][:, 0:1], [rhoB, greB, cur[1]], [ssB])
                                elif op_ == 1:
                                    k.scan(f(ss[:, 1, sl]), rho, f(gim[:, sl]), cur[0][:, 1:2], [rhoB, gimB, cur[1]], [ssB])
                                elif op_ == 2:
                                    C["nxt"] = C["car"][C["cidx"] % 2]; C["cidx"] += 1
                                    k.tt(tmpc, rv(last2), W2[:, d, gp, :], ALU.mult, [ssB, W2B], [tmpcB])
                                else:
                                    k.stt(C["nxt"][0], last2, cLc, tmpc, ALU.mult, ALU.add, [ssB, CTABB, tmpcB], [C["nxt"][1]])
                                    C["cur"] = C["nxt"]
                    for step in range(2):
                        for gq in range(NCH):
                            C = ch[gq]; cosb, sinb = tabs[gq]
                            t1, t1B_ = C["t1"]; t2, t2B_ = C["t2"]; gre, greB = C["gre"]; gim, gimB = C["gim"]
                            ss, ssB = C["ss"]; sre = ss[:, 0, :]; sim = ss[:, 1, :]; sreB = ssB; simB = ssB
                            if step == 0:
                                k.tt(v3(t1), v3(sre), cosb, ALU.mult, [sreB, CTABB], [t1B_], eng="pool")
                                k.tt(v3(t2), v3(sim), sinb, ALU.mult, [simB, STABB], [t2B_], eng="pool")
                                k.tt(gre[:, :n], t1[:, :n], t2[:, :n], ALU.subtract, [t1B_, t2B_], [greB], eng="pool")
                            else:
                                k.tt(v3(t1), v3(sre), sinb, ALU.mult, [sreB, STABB], [t1B_], eng="pool")
                                k.tt(v3(t2), v3(sim), cosb, ALU.mult, [simB, CTABB], [t2B_], eng="pool")
                                k.tt(gim[:, :n], t1[:, :n], t2[:, :n], ALU.add, [t1B_, t2B_], [gimB], eng="pool")
                    cc0 = Cpad[:, d, 0, 4 * c_:4 * c_ + 4, :].rearrange("p a b -> p (a b)")
                    cc1 = Cpad[:, d, 1, 4 * c_:4 * c_ + 4, :].rearrange("p a b -> p (a b)")
                    for gq in range(NCH):
                        C = ch[gq]; gre, greB = C["gre"]; gim, gimB = C["gim"]
                        py, pyB = psb[2 * gq]
                        k.mm(py[:, :n], cc0, gre[:, :n], True, False, [CpadB, greB], [pyB])
                        k.mm(py[:, :n], cc1, gim[:, :n], False, True, [CpadB, gimB], [pyB])
                        k.stt(yacc[:, c0:c0 + n], py[:, :n], maskc[:, gq:gq + 1], yacc[:, c0:c0 + n], ALU.mult, ALU.add,
                              [pyB, maskcB, yaccB], [yaccB])
            k.stt(yacc, ubT, dcol[:, c_:c_ + 1], yacc, ALU.mult, ALU.add, [ubTB, dcolB, yaccB], [yaccB])
            k.act(yacc, yacc, AF.Gelu, [yaccB], [yaccB])
            k.dma("sp", ysgT[c_ * 128:(c_ + 1) * 128, :], yacc, ysgTB, yaccB)
    S.barrier()
    if stop_after <= 3:
        S.finish()
        return k
    def load_wbf(st, name, src2d, kchunks, ncols, q="pool"):
        t, B = k.sb(st, name, [128, kchunks, ncols], BF16)
        k.dma(q, t, src2d.rearrange("(kc p) n -> p kc n", p=128), B, None)
        return t, B

    with ExitStack() as st:
        gw, gwB = load_wbf(st, "gw", s5_glu_w, 4, 512)
        wo, woB = load_wbf(st, "wo", rec_w_out, 8, D)
        glub, glubB = k.sb(st, "glub", [128, 4])
        k.dma("sp", glub, s5_glu_b.rearrange("(j p) -> p j", p=128), glubB, None, allow_slow_non_contiguous=True)
        G1 = [load_mod(st, "sp", 0, kd, 2, f"G1{kd}") for kd in range(2)]
        yaf = [k.sb(st, f"yaf{i}", [128, 4, 512]) for i in range(2)]
        ysf = [k.sb(st, f"ysf{i}", [128, 4, 512]) for i in range(2)]
        yab = [k.sb(st, f"yab{i}", [128, 4, 512], BF16) for i in range(2)]
        ysb = [k.sb(st, f"ysb{i}", [128, 4, 512], BF16) for i in range(2)]
        ys2 = [k.sb(st, f"ys2{i}", [128, 4, 512], BF16) for i in range(2)]
        sg = [k.sb(st, f"sg{i}", [128, 512]) for i in range(2)]
        xt2 = [k.sb(st, f"xt{i}", [128, D]) for i in range(2)]
        ot2 = [k.sb(st, f"ot{i}", [128, D]) for i in range(2)]
        tc_ = 0
        pc_ = 0
        for bi, (c0, n) in enumerate(blocks_fwd()):
            ya_, yaB_ = yaf[bi % 2]; ys_, ysB_ = ysf[bi % 2]; yb_, ybB_ = yab[bi % 2]; sb_, sbB_ = ysb[bi % 2]; y2_, y2B_ = ys2[bi % 2]
            k.dma("sp", ya_[:, :, :n], ymixT[:, c0:c0 + n].rearrange("(j p) t -> p j t", p=128), yaB_, ymixTB)
            k.dma("sp", ys_[:, :, :n], ysgT[:, c0:c0 + n].rearrange("(j p) t -> p j t", p=128), ysB_, ysgTB)
            k.cp(yb_[:, :, :n], ya_[:, :, :n], [yaB_], [ybB_], eng="act")
            k.cp(sb_[:, :, :n], ys_[:, :, :n], [ysB_], [sbB_], eng="pool")
            for oc in range(4):
                ps, psB = psb[pc_ % 8]; pc_ += 1
                for kc in range(4):
                    k.mm(ps[:, :n], gw[:, kc, oc * 128:(oc + 1) * 128], sb_[:, kc, :n], kc == 0, kc == 3, [gwB, sbB_], [psB])
                s_, sB_ = sg[oc % 2]
                k.act(s_[:, :n], ps[:, :n], AF.Sigmoid, [psB, glubB], [sB_], bias=glub[:, oc:oc + 1])
                k.tt(y2_[:, oc, :n], ys_[:, oc, :n], s_[:, :n], ALU.mult, [ysB_, sB_], [y2B_])
            for s in range(n // 128):
                ti = c0 // 128 + s
                xt, xB = xt2[tc_ % 2]; ot, oB = ot2[tc_ % 2]; tc_ += 1
                src, kd = xsrc(ti)
                k.dma("sp", xt, src, xB, None)
                k.flush()
                for half in range(2):
                    ps, psB = psb[pc_ % 8]; pc_ += 1
                    for kc in range(8):
                        lh = yb_[:, kc, s * 128:(s + 1) * 128] if kc < 4 else y2_[:, kc - 4, s * 128:(s + 1) * 128]
                        k.mm(ps, lh, wo[:, kc, half * 512:(half + 1) * 512], kc == 0, kc == 7, [ybB_, y2B_, woB], [psB])
                    k.tt(ot[:, half * 512:(half + 1) * 512], ps, G1[kd][0][:, half * 512:(half + 1) * 512], ALU.mult, [psB, G1[kd][1]], [oB])
                k.tt(ot, ot, xt, ALU.add, [oB, xB], [oB], eng="pool")
                k.defer("sp", x1[ti * 128:(ti + 1) * 128, :], ot, x1B, oB)
        k.flush()
    S.barrier()
    if stop_after <= 4:
        S.finish()
        return k

    def moe(layer, tiles, src_fn, dst_fn, dstB, final):
        with ExitStack() as st:
            kinds = sorted(set(kd for _, kd in tiles))
            A2 = {kd: load_mod(st, "sp", layer, kd, 4, f"A2{kd}") for kd in kinds}
            B2 = {kd: load_mod(st, "sp", layer, kd, 3, f"B2{kd}") for kd in kinds}
            G2 = {kd: load_mod(st, "sp", layer, kd, 5, f"G2{kd}") for kd in kinds}
            Wr, WrB = k.sb(st, "Wr", [128, 8, 36])
            rb, rbB = k.sb(st, "rb", [128, 36])
            k.dma("sp", Wr[:, :, 0:4], moe_r1_w[layer].rearrange("(kc p) g -> p kc g", p=128), WrB, None)
            k.dma("sp", rb[:, 0:4], moe_r1_b[layer, :].partition_broadcast(128), rbB, None)
            for g in range(4):
                k.dma("sp", Wr[:, :, 4 + 8 * g:12 + 8 * g], moe_r2_w[layer, g].rearrange("(kc p) e -> p kc e", p=128), WrB, None)
                k.dma("sp", rb[:, 4 + 8 * g:12 + 8 * g], moe_r2_b[layer, g, :].partition_broadcast(128), rbB, None)
            if final:
                fg, fgB = k.sb(st, "fg", [128, D])
                k.dma("sp", fg, final_norm_g.partition_broadcast(128), fgB, None)
            SUP = 8
            hT, hTB = k.sb(st, "hT", [128, 8, SUP * 128], BF16)
            acc, accB = k.sb(st, "acc", [128, SUP, D])
            gates, gatesB = k.sb(st, "gates", [128, SUP, 32])
            xt2 = [k.sb(st, f"xt{i}", [128, D]) for i in range(2)]
            ht2 = [k.sb(st, f"ht{i}", [128, D]) for i in range(2)]
            jt, jB = k.sb(st, "jt", [128, D])
            ss2 = [k.sb(st, f"ss{i}", [128, 1]) for i in range(2)]
            hTf2 = [k.sb(st, f"hTf{i}", [128, 8, 128]) for i in range(2)]
            wgt = [k.sb(st, f"wgt{i}", [128, 8, 512], BF16) for i in range(2)]
            wut = [k.sb(st, f"wut{i}", [128, 8, 512], BF16) for i in range(2)]
            wdt = [k.sb(st, f"wdt{i}", [128, 4, D], BF16) for i in range(2)]
            sil = [k.sb(st, f"sil{i}", [128, 512]) for i in range(2)]
            hid = [k.sb(st, f"hid{i}", [128, 4, 512], BF16) for i in range(2)]
            lg, lgB = k.sb(st, "lg", [128, 36]); l2m, l2mB = k.sb(st, "l2m", [128, 32])
            sm_ = {nm_: k.sb(st, f"g_{nm_}", [128, 8]) for nm_ in ("m1", "nm1", "e4", "se", "gm", "top", "d12", "w2", "dw")}
            mk1, mk1B = k.sb(st, "mk1", [128, 32]); mk2, mk2B = k.sb(st, "mk2", [128, 32])
            ecount = 0
            pd_i = 0
            for s0 in range(0, len(tiles), SUP):
                sup = tiles[s0:s0 + SUP]
                ns = len(sup)
                for s, (ti, kd) in enumerate(sup):
                    xt, xB = xt2[s % 2]; ht, hB = ht2[s % 2]; ss, ssB = ss2[s % 2]; hTf, hTfB = hTf2[s % 2]
                    src, srcB = src_fn(ti)
                    k.dma("sp", xt, src, xB, srcB)
                    norm_mod(xt, xB, ht, hB, A2[kd][0], A2[kd][1], B2[kd][0], B2[kd][1], (jt, jB, ss, ssB))
                    for b in range(2):
                        ps, psB = psb[(s * 2 + b) % 2]
                        for j in range(4):
                            kc = b * 4 + j
                            k.tr(ps[:, j * 128:(j + 1) * 128], ht[:, kc * 128:(kc + 1) * 128], ident, [hB, identB], [psB])
                        k.cp(hTf[:, b * 4:(b + 1) * 4, :], ps.rearrange("p (j t) -> p j t", j=4), [psB], [hTfB], eng=("act" if b == 0 else "dve"))
                    k.cp(hT[:, :, s * 128:(s + 1) * 128], hTf, [hTfB], [hTB], eng="pool")
                    pr, prB = psb[2 + s % 2]
                    for kc in range(8):
                        k.mm(pr[:, 0:36], hTf[:, kc, :], Wr[:, kc, :], kc == 0, kc == 7, [hTfB, WrB], [prB])
                    k.tt(lg, pr[:, 0:36], rb, ALU.add, [prB, rbB], [lgB])
                    m1, m1B = sm_["m1"]; nm1, nm1B = sm_["nm1"]; e4, e4B = sm_["e4"]; se, seB = sm_["se"]; gm, gmB = sm_["gm"]
                    top, topB = sm_["top"]; d12, d12B = sm_["d12"]; w2, w2B = sm_["w2"]; dw, dwB = sm_["dw"]
                    S.op("dve", lambda e, m1=m1: e.tensor_reduce(out=m1[:, 0:1], in_=lg[:, 0:4], axis=AX.X, op=ALU.max), [lgB], [m1B])
                    k.ts(nm1[:, 0:1], m1[:, 0:1], -1.0, ALU.mult, [m1B], [nm1B])
                    k.act(e4[:, 0:4], lg[:, 0:4], AF.Exp, [lgB, nm1B], [e4B, seB], bias=nm1[:, 0:1], accum=se[:, 0:1])
                    S.op("dve", lambda e, se=se: e.reciprocal(out=se[:, 0:1], in_=se[:, 0:1]), [seB], [seB])
                    k.ts(gm[:, 0:4], lg[:, 0:4], m1[:, 0:1], ALU.is_ge, [lgB, m1B], [gmB])
                    k.ts(gm[:, 0:4], gm[:, 0:4], 30000.0, ALU.mult, [gmB], [gmB], s2=-30000.0, op1=ALU.add)
                    k.tt(l2m.rearrange("p (g e) -> p g e", g=4), lg[:, 4:36].rearrange("p (g e) -> p g e", g=4),
                         gm[:, 0:4].unsqueeze(2).broadcast_to([128, 4, 8]), ALU.add, [lgB, gmB], [l2mB])
                    S.op("dve", lambda e, top=top: e.max(out=top[:, 0:8], in_=l2m), [l2mB], [topB])
                    k.tt(d12[:, 0:1], top[:, 1:2], top[:, 0:1], ALU.subtract, [topB], [d12B])
                    k.act(w2[:, 0:1], d12[:, 0:1], AF.Sigmoid, [d12B], [w2B])
                    k.tt(w2[:, 0:1], w2[:, 0:1], se[:, 0:1], ALU.mult, [w2B, seB], [w2B])
                    k.stt(dw[:, 0:1], w2[:, 0:1], -2.0, se[:, 0:1], ALU.mult, ALU.add, [w2B, seB], [dwB])
                    k.ts(mk2, l2m, top[:, 1:2], ALU.is_ge, [l2mB, topB], [mk2B])
                    k.ts(mk1, l2m, top[:, 0:1], ALU.is_ge, [l2mB, topB], [mk1B])
                    k.ts(mk2, mk2, w2[:, 0:1], ALU.mult, [mk2B, w2B], [mk2B])
                    k.stt(gates[:, s, :], mk1, dw[:, 0:1], mk2, ALU.mult, ALU.add, [mk1B, dwB, mk2B], [gatesB])
                grp = [list(range(i, min(i + 4, ns))) for i in range(0, ns, 4)]
                for e_ in range(32):
                    wg_, wgB_ = wgt[ecount % 2]; wu_, wuB_ = wut[ecount % 2]; wd_, wdB_ = wdt[ecount % 2]; ecount += 1
                    k.dma("pool", wg_, moe_w_gate[layer, e_].rearrange("(kc p) f -> p kc f", p=128), wgB_, None)
                    k.dma("pool", wu_, moe_w_up[layer, e_].rearrange("(kc p) f -> p kc f", p=128), wuB_, None)
                    k.dma("pool", wd_, moe_w_down[layer, e_].rearrange("(fc p) n -> p fc n", p=128), wdB_, None)
                    for gi, gt in enumerate(grp):
                        n = 128 * len(gt); cc = gt[0] * 128
                        hd, hdB = hid[gi % 2]
                        for fc in range(4):
                            pg, pgB = psb[fc % 2]; pu, puB = psb[2 + fc % 2]
                            for kc in range(8):
                                k.mm(pg[:, :n], wg_[:, kc, fc * 128:(fc + 1) * 128], hT[:, kc, cc:cc + n], kc == 0, kc == 7, [wgB_, hTB], [pgB])
                            for kc in range(8):
                                k.mm(pu[:, :n], wu_[:, kc, fc * 128:(fc + 1) * 128], hT[:, kc, cc:cc + n], kc == 0, kc == 7, [wuB_, hTB], [puB])
                            sl_, slB = sil[fc % 2]
                            k.act(sl_[:, :n], pg[:, :n], AF.Silu, [pgB], [slB])
                            k.tt(hd[:, fc, :n], sl_[:, :n], pu[:, :n], ALU.mult, [slB, puB], [hdB])
                    for gi, gt in enumerate(grp):
                        hd, hdB = hid[gi % 2]
                        for si, s in enumerate(gt):
                            for half in range(2):
                                pd, pdB = psb[4 + pd_i % 4]; pd_i += 1
                                for fc in range(4):
                                    k.mm(pd, hd[:, fc, si * 128:(si + 1) * 128], wd_[:, fc, half * 512:(half + 1) * 512], fc == 0, fc == 3, [hdB, wdB_], [pdB])
                                av = acc[:, s, half * 512:(half + 1) * 512]
                                if e_ == 0:
                                    k.ts(av, pd, gates[:, s, e_:e_ + 1], ALU.mult, [pdB, gatesB], [accB])
                                else:
                                    k.stt(av, pd, gates[:, s, e_:e_ + 1], av, ALU.mult, ALU.add, [pdB, gatesB, accB], [accB])
                for s, (ti, kd) in enumerate(sup):
                    xt, xB = xt2[s % 2]; ot, oB = ht2[s % 2]; ss, ssB = ss2[s % 2]
                    src, srcB = src_fn(ti)
                    k.dma("sp", xt, src, xB, srcB)
                    k.flush()
                    k.tt(ot, acc[:, s, :], G2[kd][0], ALU.mult, [accB, G2[kd][1]], [oB], eng="pool")
                    k.tt(ot, ot, xt, ALU.add, [oB, xB], [oB])
                    if final:
                        k.act(jt, ot, AF.Square, [oB], [jB, ssB], accum=ss)
                        k.act(ss, ss, AF.Sqrt, [ssB, epsB], [ssB], scale=1.0 / D, bias=epsb)
                        S.op("dve", lambda e, ss=ss: e.reciprocal(out=ss, in_=ss), [ssB], [ssB])
                        k.stt(ot, ot, ss, fg, ALU.mult, ALU.mult, [oB, ssB, fgB], [oB])
                    k.defer("sp", dst_fn(ti), ot, dstB, oB)
                k.flush()
        S.barrier()

    moe(0, [(ti, 1 if ti < 2 else 0) for ti in range(NT)], lambda ti: (x1[ti * 128:(ti + 1) * 128, :], x1B),
        lambda ti: x2[ti * 128:(ti + 1) * 128, :], x2B, False)
    if stop_after <= 5:
        S.finish()
        return k
    NTL = T // 128
    ROWS = T // 64
    QT, QTB = k.dscr("QT", [D, T], BF16)
    KT, KTB = k.dscr("KT", [D, TT], BF16)
    VA, VAB = k.dscr("VA", [TT, 16, 65], BF16)
    AO, AOB = k.dscr("AO", [T, D])
    REV, REVB = k.dscr("REV", [16, 15, 128])
    with ExitStack() as st:
        wq, wqB = load_wbf(st, "wqkv", na_w_qkv, 8, 3 * D)
        AA = [load_mod(st, "sp", 1, kd, 1, f"A{kd}") for kd in range(2)]
        BBm = [load_mod(st, "sp", 1, kd, 0, f"B{kd}") for kd in range(2)]
        xt2 = [k.sb(st, f"xt{i}", [128, D]) for i in range(2)]
        ht2 = [k.sb(st, f"ht{i}", [128, D]) for i in range(2)]
        jt, jB = k.sb(st, "jt", [128, D])
        ss2 = [k.sb(st, f"ss{i}", [128, 1]) for i in range(2)]
        hT2 = [k.sb(st, f"hT{i}", [128, 8, 512], BF16) for i in range(2)]
        qst2 = [k.sb(st, f"qst{i}", [128, 8, 512], BF16) for i in range(2)]
        kst2 = [k.sb(st, f"kst{i}", [128, 8, 512], BF16) for i in range(2)]
        vst2 = [k.sb(st, f"vst{i}", [128, 16, 65], BF16) for i in range(2)]
        for i in range(2):
            k.ms(vst2[i][0], 1.0, [vst2[i][1]])
        tcount = 0; pc_ = 0
        for gi, g in enumerate(groups_of(list(range(NT)))):
            hT, hTB = hT2[gi % 2]
            n = 128 * len(g); col0 = g[0] * 128
            isctx = g[0] < 2
            for si, ti in enumerate(g):
                xt, xB = xt2[tcount % 2]; ht, hB = ht2[tcount % 2]; ss, ssB = ss2[tcount % 2]
                kd = 1 if ti < 2 else 0
                k.dma("sp", xt, x2[ti * 128:(ti + 1) * 128, :], xB, x2B)
                norm_mod(xt, xB, ht, hB, AA[kd][0], AA[kd][1], BBm[kd][0], BBm[kd][1], (jt, jB, ss, ssB))
                for b in range(2):
                    ps, psB = psb[(tcount * 2 + b) % 2]
                    for j in range(4):
                        kc = b * 4 + j
                        k.tr(ps[:, j * 128:(j + 1) * 128], ht[:, kc * 128:(kc + 1) * 128], ident, [hB, identB], [psB])
                    k.cp(hT[:, b * 4:(b + 1) * 4, si * 128:(si + 1) * 128], ps.rearrange("p (j t) -> p j t", j=4),
                         [psB], [hTB], eng=("act" if b == 0 else "dve"))
                tcount += 1
            qst, qstB = qst2[gi % 2]; kst, kstB = kst2[gi % 2]
            for oc in range(16):
                if isctx and oc < 8:
                    continue
                ps, psB = psb[2 + pc_ % 3]; pc_ += 1
                for kc in range(8):
                    k.mm(ps[:, :n], wq[:, kc, oc * 128:(oc + 1) * 128], hT[:, kc, :n], kc == 0, kc == 7, [wqB, hTB], [psB])
                if oc < 8:
                    k.act(qst[:, oc, :n], ps[:, :n], AF.Copy, [psB], [qstB], scale=0.125)
                else:
                    k.cp(kst[:, oc - 8, :n], ps[:, :n], [psB], [kstB])
            if not isctx:
                k.dma("sp", QT[:, col0 - CT:col0 - CT + n].rearrange("(oc p) t -> p oc t", p=128), qst[:, :, :n], QTB, qstB)
            k.dma("sp", KT[:, col0:col0 + n].rearrange("(oc p) t -> p oc t", p=128), kst[:, :, :n], KTB, kstB)
            for si, ti in enumerate(g):
                vst, vstB = vst2[ti % 2]
                for half in range(2):
                    ps, psB = psb[5 + pc_ % 3]; pc_ += 1
                    for kc in range(8):
                        k.mm(ps, hT[:, kc, si * 128:(si + 1) * 128], wq[:, kc, 2 * D + half * 512:2 * D + (half + 1) * 512], kc == 0, kc == 7, [wqB, hTB], [psB])
                    k.cp(vst[:, half * 8:(half + 1) * 8, 0:64], ps.rearrange("p (h e) -> p h e", h=8), [psB], [vstB], eng=("act" if half == 0 else "dve"))
                k.dma("sp", VA[ti * 128:(ti + 1) * 128, :, :], vst, VAB, vstB)
    S.barrier()
    if stop_after <= 6:
        S.finish()
        return k

    with ExitStack() as st:
        T2, T2B = k.sb(st, "T2", [128, 14, 16, 64])
        zt, ztB = k.sb(st, "zt", [128, 256])
        k.ms(zt, 0.0, [ztB])
        k.dma("sp", REV.rearrange("h d j -> (h d j)").rearrange("(p f) -> p f", p=120), zt[0:120, :], REVB, ztB)
        rpt, rptB = k.sb(st, "rpt", [16, 15, 31])
        k.dma("sp", rpt, na_rpb, rptB, None)
        k.dma("sp", REV[:, :, 48:79], rpt, REVB, rptB)
        with ExitStack() as st2:
            T2r, T2rB = k.sb(st2, "T2r", [128, 7, 16, 64])
            for hf in range(2):
                for dl in range(2):
                    for a_ in range(7):
                        drb = hf * 7 + a_
                        src = bass.AP(REV.tensor, REV.offset + (drb + dl) * 128, [[1, 64], [15 * 128, 16], [1, 64]])
                        k.dma("sp", T2r[dl * 64:(dl + 1) * 64, a_, :, :], src, T2rB, REVB)
                k.cp(T2[:, hf * 7:(hf + 1) * 7, :, :].rearrange("p a h q -> p (a h) q"),
                     rv(T2r.rearrange("p a h q -> p (a h) q")), [T2rB], [T2B], eng="dve")
        S.barrier()
        Mt, MtB = k.sb(st, "Mt", [128, 64])
        k.ms(Mt, 0.0, [MtB])
        NEG = -30000.0
        for dl in range(2):
            hs = slice(dl * 64, (dl + 1) * 64)
            sels = [(slice(0, 8), [[0, 8]], 15, -1), (slice(8, 57), [[-1, 49]], 0, 1), (slice(8, 57), [[1, 49]], 15, -1),
                    (slice(57, 64), [[0, 7]], -48, 1)]
            for (qs, pat, base, cm) in sels:
                S.op("pool", lambda e, hs=hs, qs=qs, pat=pat, base=base, cm=cm: e.affine_select(
                    out=Mt[hs, qs], in_=Mt[hs, qs], pattern=pat, compare_op=ALU.is_ge, fill=NEG, base=base, channel_multiplier=cm), [MtB], [MtB])
        T2v = T2.rearrange("p a h q -> p (a h) q")
        k.tt(T2v, T2v, Mt.unsqueeze(1).broadcast_to([128, 224, 64]), ALU.add, [T2B, MtB], [T2B])
        if "T2d" in k.dbg:
            T2d, T2dB = k.dscr("T2d", [128, 14 * 16 * 64])
            k.dma("sp", T2d, T2.rearrange("p a h q -> p (a h q)"), T2dB, T2B)
        KcT, KcTB = k.sb(st, "KcT", [128, 8, 256], BF16)
        Vc, VcB = k.sb(st, "Vc", [128, 2, 1040], BF16)
        k.dma("sp", KcT, KT[:, 0:256].rearrange("(c p) t -> p c t", p=128), KcTB, KTB)
        k.dma("sp", Vc, VA[0:256].rearrange("(a p) h e -> p a (h e)", p=128), VcB, VAB)
        qbd2 = [k.sb(st, f"qbd{i}", [128, 8, 128], BF16) for i in range(2)]
        for i in range(2):
            k.ms(qbd2[i][0], 0.0, [qbd2[i][1]])
        kT2 = [k.sb(st, f"kT{i}", [128, 8, 512], BF16) for i in range(2)]
        vv2 = [k.sb(st, f"vv{i}", [128, 4, 1040], BF16) for i in range(2)]
        sb2 = [k.sb(st, f"sbt{i}", [128, 1024]) for i in range(2)]
        pT2 = [k.sb(st, f"pT{i}", [128, 1024], BF16) for i in range(2)]
        ao2 = [k.sb(st, f"ao{i}", [128, 8, 64]) for i in range(2)]
        rec2 = [k.sb(st, f"rec{i}", [128, 8]) for i in range(2)]
        ci_ = 0
        P7 = int(os.environ.get('P7', '9'))
        def row_loads(r):
            rs_ = min(max(r - 4, 0), ROWS - 8)
            qbd, qbdB = qbd2[r % 2]; kT, kTB = kT2[r % 2]; vv, vvB = vv2[r % 2]
            qsrc = QT[:, r * 64:(r + 1) * 64].rearrange("(c p) t -> p c t", p=128)
            k.dma("sp", qbd[0:64, :, 0:64], qsrc[0:64], qbdB, QTB)
            k.dma("sp", qbd[64:128, :, 64:128], qsrc[64:128], qbdB, QTB)
            k0 = CT + rs_ * 64
            k.dma("sp", kT, KT[:, k0:k0 + 512].rearrange("(c p) t -> p c t", p=128), kTB, KTB)
            k.dma("sp", vv, VA[k0:k0 + 512].rearrange("(a p) h e -> p a (h e)", p=128), vvB, VAB)
        if P7 >= 1:
            row_loads(0)
        for r in range(ROWS if P7 >= 1 else 0):
            rs_ = min(max(r - 4, 0), ROWS - 8)
            qbd, qbdB = qbd2[r % 2]; kT, kTB = kT2[r % 2]; vv, vvB = vv2[r % 2]
            if r + 1 < ROWS:
                row_loads(r + 1)
            for kc in range(6):
                sbank = [psb[(ci_ % 2) * 2], psb[(ci_ % 2) * 2 + 1]]
                sbt, sbtB = sb2[ci_ % 2]; pT, pTB = pT2[ci_ % 2]; ci_ += 1
                for c in range(8):
                    if kc < 4:
                        lh = kT[:, c, kc * 128:(kc + 1) * 128]; lB = kTB
                    else:
                        lh = KcT[:, c, (kc - 4) * 128:(kc - 3) * 128]; lB = KcTB
                    sp_, spB = sbank[c // 4]
                    k.mm(sp_[:, (c % 4) * 128:(c % 4 + 1) * 128], lh, qbd[:, c, :], True, True, [lB, qbdB], [spB])
                if kc < 4:
                    drb = 2 * kc + (rs_ - r + 7)
                    for b in range(2):
                        k.tt(sbt[:, b * 512:(b + 1) * 512], sbank[b][0], T2[:, drb, b * 8:(b + 1) * 8, :].rearrange("p h q -> p (h q)"),
                             ALU.add, [sbank[b][1], T2B], [sbtB])
                    k.act(pT, sbt, AF.Exp, [sbtB], [pTB])
                    vsrc = vv[:, kc, :]; vB_ = vvB
                else:
                    for b in range(2):
                        k.act(pT[:, b * 512:(b + 1) * 512], sbank[b][0], AF.Exp, [sbank[b][1]], [pTB])
                    vsrc = Vc[:, kc - 4, :]; vB_ = VcB
                for c in range(8 if P7 >= 2 else 0):
                    ob, obB = psb[4 + c // 3]
                    k.mm(ob[:, (c % 3) * 130:(c % 3 + 1) * 130], pT[:, c * 128:(c + 1) * 128], vsrc[:, c * 130:(c + 1) * 130],
                         (kc == 0 and c % 3 == 0), kc == 5, [pTB, vB_], [obB], skip=True)
            ao, aoB = ao2[r % 2]; rec, recB = rec2[r % 2]
            for b in range(3 if P7 >= 3 else 0):
                np_ = 3 if b < 2 else 2
                ob, obB = psb[4 + b]
                for par in range(2):
                    ps_ = slice(par * 64, (par + 1) * 64)
                    ov = ob[ps_, 0:np_ * 130].rearrange("p (c e) -> p c e", e=130)
                    S.op("dve", lambda e, rec=rec, ps_=ps_, b=b, np_=np_, ov=ov, par=par: e.reciprocal(
                        out=rec[ps_, b * 3:b * 3 + np_], in_=ov[:, :, par * 65 + 64]), [obB], [recB])
                    k.tt(ao[ps_, b * 3:b * 3 + np_, :], ov[:, :, par * 65:par * 65 + 64],
                         rec[ps_, b * 3:b * 3 + np_].unsqueeze(2).broadcast_to([64, np_, 64]), ALU.mult, [obB, recB], [aoB])
            if P7 >= 3:
                dstv = AO[r * 64:(r + 1) * 64, :].rearrange("q (c p e) -> q c p e", p=2, e=64)
                for par in range(2):
                    k.dma("sp", dstv[:, :, par, :], ao[par * 64:(par + 1) * 64, :, :], AOB, aoB)
    S.barrier()
    if stop_after <= 7:
        S.finish()
        return k

    with ExitStack() as st:
        wo, woB = load_wbf(st, "wo1", na_w_out, 8, D)
        G1, G1B = load_mod(st, "sp", 1, 0, 2, "G1l1")
        at2 = [k.sb(st, f"at{i}", [128, D]) for i in range(2)]
        xt2 = [k.sb(st, f"xt{i}", [128, D]) for i in range(2)]
        ot2 = [k.sb(st, f"ot{i}", [128, D]) for i in range(2)]
        aT2 = [k.sb(st, f"aT{i}", [128, 8, 128], BF16) for i in range(2)]
        for tl in range(NTL):
            at_, atB = at2[tl % 2]; xt, xB = xt2[tl % 2]; ot, oB = ot2[tl % 2]; aT, aTB = aT2[tl % 2]
            k.dma("sp", at_, AO[tl * 128:(tl + 1) * 128, :], atB, AOB)
            k.dma("sp", xt, x2[CT + tl * 128:CT + (tl + 1) * 128, :], xB, x2B)
            k.flush()
            for b in range(2):
                ps, psB = psb[(tl * 2 + b) % 4]
                for j in range(4):
                    kc = b * 4 + j
                    k.tr(ps[:, j * 128:(j + 1) * 128], at_[:, kc * 128:(kc + 1) * 128], ident, [atB, identB], [psB])
                k.cp(aT[:, b * 4:(b + 1) * 4, :], ps.rearrange("p (j t) -> p j t", j=4), [psB], [aTB], eng=("act" if b == 0 else "dve"))
            for half in range(2):
                ps, psB = psb[4 + (tl * 2 + half) % 4]
                for kc in range(8):
                    k.mm(ps, aT[:, kc, :], wo[:, kc, half * 512:(half + 1) * 512], kc == 0, kc == 7, [aTB, woB], [psB])
                k.tt(ot[:, half * 512:(half + 1) * 512], ps, G1[:, half * 512:(half + 1) * 512], ALU.mult, [psB, G1B], [oB])
            k.tt(ot, ot, xt, ALU.add, [oB, xB], [oB], eng="pool")
            k.defer("sp", x3[tl * 128:(tl + 1) * 128, :], ot, x3B, oB)
        k.flush()
    S.barrier()
    if stop_after <= 8:
        S.finish()
        return k
    moe(1, [(tl, 0) for tl in range(NTL)], lambda tl: (x3[tl * 128:(tl + 1) * 128, :], x3B),
        lambda tl: out[tl * 128:(tl + 1) * 128, :], obuf, True)
    S.finish()
    return k


_CACHE = {}


def kernel(**inputs):
    T = inputs["x"].shape[1]
    if T not in _CACHE:
        _CACHE[T] = build(T)
    kb = _CACHE[T]
    nb = inputs["x"].shape[0]
    squeeze = ("rec_", "lru_", "s5_", "na_")
    shared = {}
    for name, v in inputs.items():
        if name in ("x", "c", "ctx"):
            continue
        a = np.asarray(v, dtype=np.float32)
        if name.startswith(squeeze):
            a = a[0]
        shared[name] = np.ascontiguousarray(a)
    in_maps = []
    for b in range(nb):
        m = dict(shared)
        m["x"] = np.ascontiguousarray(np.asarray(inputs["x"][b], dtype=np.float32))
        m["c"] = np.ascontiguousarray(np.asarray(inputs["c"][b], dtype=np.float32))
        m["ctx"] = np.ascontiguousarray(np.asarray(inputs["ctx"][b], dtype=np.float32))
        in_maps.append({kk: vv for kk, vv in m.items() if kk in kb.inp})
    res = run_bass_kernel_spmd(kb.nc, in_maps, core_ids=list(range(nb)))
    return np.stack([np.asarray(res.results[b]["out"]) for b in range(nb)], axis=0).astype(np.float32)
```

```python
import math
import os
import numpy as np
import concourse.bass as bass
import concourse.mybir as mybir
from concourse.bass_utils import run_bass_kernel_spmd

F32 = mybir.dt.float32
BF16 = mybir.dt.bfloat16
I32 = mybir.dt.int32
AF = mybir.ActivationFunctionType
ALU = mybir.AluOpType
AX = mybir.AxisListType
SEM_ROT = 30000
D = 1024
CT = 256
LCH = 128


class Buf:
    __slots__ = ("name", "w", "r", "dsem", "dcnt", "gen")

    def __init__(self, name):
        self.name = name
        self.w = None
        self.r = []
        self.dsem = None
        self.dcnt = 0
        self.gen = -1


class Sched:
    def __init__(self, nc):
        self.nc = nc
        self.eobj = {"pe": nc.tensor, "act": nc.scalar, "dve": nc.vector, "pool": nc.gpsimd, "sp": nc.sync}
        self.prog = {k: [] for k in self.eobj}
        self.sem = {}
        self.cnt = {}
        self.seen = {k: {} for k in self.eobj}
        self.nsem = 0
        self.dsemval = {}
        self.dsems = []
        self.allsems = []
        self.freed = []
        self.freed_sw = []
        self.semsw = {}
        self.gen = 0
        for k in self.eobj:
            self._newsem(k)
        self.ninstr = {k: 0 for k in self.eobj}

    def _alloc_sem(self, name):
        self.nsem += 1
        s = self.nc.alloc_semaphore(name=name)
        return s

    def _newsem(self, k):
        self.sem[k] = self._alloc_sem(f"e_{k}_{self.nsem}")
        self.cnt[k] = 0
        self.allsems.append((k, self.sem[k]))

    def _wait(self, eng, ev):
        sem, val = ev
        sid = id(sem)
        if sid in self.dsemval:
            val = self.dsemval[sid]
        if self.seen[eng].get(sid, 0) >= val:
            return
        self.seen[eng][sid] = val
        self.prog[eng].append(lambda e, sem=sem, val=val: e.wait_ge(sem, val))

    def op(self, eng, fn, reads=(), writes=()):
        my = self.sem[eng]
        pe = eng == "pe"
        for b in reads:
            if b.w is not None and not (pe and b.w[0] is my):
                self._wait(eng, b.w)
        for b in writes:
            if b.w is not None and not (pe and b.w[0] is my):
                self._wait(eng, b.w)
            for ev in b.r:
                if not (pe and ev[0] is my):
                    self._wait(eng, ev)
        if self.cnt[eng] >= SEM_ROT:
            self._newsem(eng)
            my = self.sem[eng]
        self.cnt[eng] += 1
        ev = (my, self.cnt[eng])
        self.prog[eng].append(lambda e, fn=fn, my=my: fn(e).then_inc(my, 1))
        self.ninstr[eng] += 1
        for b in writes:
            b.w = ev
            b.r = []
        for b in reads:
            if b not in writes:
                b.r.append(ev)
                if len(b.r) > 16:
                    b.r = b.r[-16:]
        return ev

    def dma(self, q, out_ap, in_ap, dst, src, **kw):
        b = dst if dst is not None else src
        sw = q == "pool"
        if b.dsem is None or b.gen != self.gen or b.dcnt * 16 >= SEM_ROT or self.semsw.get(id(b.dsem)) != sw:
            pool_ = self.freed_sw if sw else self.freed
            while pool_ and pool_[-1][1] * 16 >= SEM_ROT - 4000:
                pool_.pop()
            if pool_:
                b.dsem, b.dcnt = pool_.pop()
            else:
                b.dsem = self._alloc_sem(f"d_{b.name}_{self.nsem}")
                b.dcnt = 0
            self.semsw[id(b.dsem)] = sw
            b.gen = self.gen
            self.dsems.append(b.dsem)
        if src is not None and src.w is not None:
            self._wait(q, src.w)
        if dst is not None:
            if dst.w is not None:
                self._wait(q, dst.w)
            for ev in dst.r:
                self._wait(q, ev)
        b.dcnt += 1
        sem = b.dsem
        ev = (sem, b.dcnt * 16)
        self.dsemval[id(sem)] = b.dcnt * 16
        self.prog[q].append(
            lambda e, o=out_ap, i=in_ap, sem=sem, kw=kw: e.dma_start(out=o, in_=i, **kw).then_inc(sem, 16))
        self.ninstr[q] += 1
        if dst is not None:
            dst.w = ev
            dst.r = []
        if src is not None:
            src.r.append(ev)
        return ev

    def barrier(self):
        evs = [(self.sem[k], self.cnt[k]) for k in self.eobj if self.cnt[k] > 0]
        devs = [(s, self.dsemval[id(s)]) for s in self.dsems]
        for e in self.eobj:
            for ev in evs:
                if ev[0] is self.sem[e] and e == "pe":
                    continue
                self._wait(e, ev)
            for ev in devs:
                self._wait(e, ev)
        for sm_ in self.dsems:
            (self.freed_sw if self.semsw[id(sm_)] else self.freed).append((sm_, self.dsemval[id(sm_)] // 16))
        self.dsems = []
        self.gen += 1

    def finish(self):
        self.barrier()
        nc = self.nc
        prog = self.prog
        with nc.Block() as block:
            @block.sync
            def _(e):
                for t in prog["sp"]:
                    t(e)

            @block.tensor
            def _(e):
                for t in prog["pe"]:
                    t(e)

            @block.scalar
            def _(e):
                for t in prog["act"]:
                    t(e)

            @block.vector
            def _(e):
                for t in prog["dve"]:
                    t(e)

            @block.gpsimd
            def _(e):
                for t in prog["pool"]:
                    t(e)


def rv(ap):
    a = [list(x) for x in ap.ap]
    st, n = a[-1]
    a[-1] = [-st, n]
    return bass.AP(ap.tensor, ap.offset + st * (n - 1), a)


class K:
    def __init__(self, T, dbg=()):
        self.T = T
        self.TT = T + CT
        self.NT = self.TT // 128
        self.dbg = set(dbg)
        self.nc = bass.Bass("TRN2", target_bir_lowering=False)
        self.S = Sched(self.nc)
        self.uid = 0
        self.inp = {}
        self.pending = []

    def din(self, name, shape):
        t = self.nc.dram_tensor(name, list(shape), F32, kind="ExternalInput").ap()
        self.inp[name] = t
        return t

    def dscr(self, name, shape, dt=F32):
        kind = "ExternalOutput" if name in self.dbg else "Internal"
        return self.nc.dram_tensor(name, list(shape), dt, kind=kind).ap(), Buf(name)

    def sb(self, st, name, shape, dt=F32):
        self.uid += 1
        t = st.enter_context(self.nc.sbuf_tensor(f"{name}_{self.uid}", list(shape), dt))
        return t.ap(), Buf(name)

    def act(self, out, in_, func, r, w, scale=None, bias=None, accum=None):
        kw = {}
        if scale is not None:
            kw["scale"] = scale
        if bias is not None:
            kw["bias"] = bias
        if accum is not None:
            kw["accum_out"] = accum
        return self.S.op("act", lambda e: e.activation(out=out, in_=in_, func=func, **kw), r, w)

    def tt(self, out, a, b, op, r, w, eng="dve"):
        return self.S.op(eng, lambda e: e.tensor_tensor(out=out, in0=a, in1=b, op=op), r, w)

    def ts(self, out, a, s1, op0, r, w, s2=None, op1=None, eng="dve"):
        if op1 is None:
            return self.S.op(eng, lambda e: e.tensor_scalar(out=out, in0=a, scalar1=s1, scalar2=None, op0=op0), r, w)
        return self.S.op(eng, lambda e: e.tensor_scalar(out=out, in0=a, scalar1=s1, scalar2=s2, op0=op0, op1=op1), r, w)

    def stt(self, out, a, s, b, op0, op1, r, w):
        return self.S.op("dve", lambda e: e.scalar_tensor_tensor(out=out, in0=a, scalar=s, in1=b, op0=op0, op1=op1), r, w)

    def cp(self, out, in_, r, w, eng="dve"):
        if eng == "act":
            return self.S.op("act", lambda e: e.copy(out=out, in_=in_), r, w)
        return self.S.op(eng, lambda e: e.tensor_copy(out=out, in_=in_), r, w)

    def ms(self, ap, val, w, eng="pool"):
        return self.S.op(eng, lambda e: e.memset(ap, val), (), w)

    def mm(self, out, lhsT, rhs, start, stop, r, w, skip=False):
        return self.S.op("pe", lambda e: e.matmul(out, lhsT, rhs, start=start, stop=stop, skip_group_check=skip), r, w)

    def tr(self, out, in_, ident, r, w):
        return self.S.op("pe", lambda e: e.transpose(out, in_, ident), r, w)

    def scan(self, out, d0, d1, init, r, w):
        return self.S.op("dve", lambda e: e.tensor_tensor_scan(out=out, data0=d0, data1=d1, initial=init,
                                                               op0=ALU.mult, op1=ALU.add), r, w)

    def dma(self, q, out, in_, dst, src, **kw):
        return self.S.dma(q, out, in_, dst, src, **kw)

    def defer(self, *a, **kw):
        self.pending.append((a, kw))

    def flush(self):
        for a, kw in self.pending:
            self.S.dma(*a, **kw)
        self.pending = []


def build(T=8192, dbg=(), stop_after=99):
    from contextlib import ExitStack
    k = K(T, dbg)
    nc, S = k.nc, k.S
    TT, NT = k.TT, k.NT
    x = k.din("x", [T, D]); c = k.din("c", [D]); ctx = k.din("ctx", [CT, D]); c_ctx = k.din("c_ctx", [D])
    ada_w = k.din("ada_w", [2, D, 6 * D]); ada_b = k.din("ada_b", [2, 6 * D])
    norm1_g = k.din("norm1_g", [2, D]); norm2_g = k.din("norm2_g", [2, D])
    rec_w_in = k.din("rec_w_in", [D, 1536]); rec_conv_w = k.din("rec_conv_w", [4, 512]); rec_conv_b = k.din("rec_conv_b", [512])
    lru_wa = k.din("lru_wa", [2, 8, 64, 64]); lru_ba = k.din("lru_ba", [2, 512])
    lru_wx = k.din("lru_wx", [2, 8, 64, 64]); lru_bx = k.din("lru_bx", [2, 512]); lru_lambda = k.din("lru_lambda", [2, 512])
    s5_a_re = k.din("s5_a_re", [2, 32, 64]); s5_a_im = k.din("s5_a_im", [2, 32, 64]); s5_log_dt = k.din("s5_log_dt", [2, 32])
    s5_b_re = k.din("s5_b_re", [2, 32, 64, 16]); s5_b_im = k.din("s5_b_im", [2, 32, 64, 16])
    s5_c_re = k.din("s5_c_re", [2, 32, 16, 64]); s5_c_im = k.din("s5_c_im", [2, 32, 16, 64])
    s5_d = k.din("s5_d", [512]); s5_glu_w = k.din("s5_glu_w", [512, 512]); s5_glu_b = k.din("s5_glu_b", [512])
    rec_w_out = k.din("rec_w_out", [D, D])
    na_w_qkv = k.din("na_w_qkv", [D, 3 * D]); na_w_out = k.din("na_w_out", [D, D]); na_rpb = k.din("na_rpb", [16, 15, 31])
    moe_r1_w = k.din("moe_r1_w", [2, D, 4]); moe_r1_b = k.din("moe_r1_b", [2, 4])
    moe_r2_w = k.din("moe_r2_w", [2, 4, D, 8]); moe_r2_b = k.din("moe_r2_b", [2, 4, 8])
    moe_w_gate = k.din("moe_w_gate", [2, 32, D, 512]); moe_w_up = k.din("moe_w_up", [2, 32, D, 512])
    moe_w_down = k.din("moe_w_down", [2, 32, 512, D]); final_norm_g = k.din("final_norm_g", [D])
    out = nc.dram_tensor("out", [T, D], F32, kind="ExternalOutput").ap()
    obuf = Buf("out")

    modv, modvB = k.dscr("modv", [2, 2, 6 * D])
    projT, projTB = k.dscr("projT", [1536, TT])
    ymixT, ymixTB = k.dscr("ymixT", [512, TT])
    ysgT, ysgTB = k.dscr("ysgT", [512, TT])
    x1, x1B = k.dscr("x1", [TT, D])
    x2, x2B = k.dscr("x2", [TT, D])
    x3, x3B = k.dscr("x3", [T, D])

    gst = ExitStack()
    psb = []
    for i in range(8):
        t = gst.enter_context(nc.psum_tensor(f"psb{i}", [128, 512], F32))
        psb.append((t.ap(), Buf(f"psb{i}")))
    ident, identB = k.sb(gst, "ident", [128, 128])
    epsb, epsB = k.sb(gst, "epsb", [128, 1])
    halfpi, halfpiB = k.sb(gst, "halfpi", [128, 1])
    k.ms(ident, 0.0, [identB])
    S.op("pool", lambda e: e.memset(epsb, 1e-6), (), [epsB])
    S.op("pool", lambda e: e.memset(halfpi, math.pi / 2), (), [halfpiB])
    ones_t, onesB = k.sb(gst, "ones", [128, 128])
    k.ms(ones_t, 1.0, [onesB])
    S.op("pool", lambda e: e.affine_select(out=ident, in_=ones_t, pattern=[[-1, 128]], compare_op=ALU.is_equal,
                                            fill=0.0, base=0, channel_multiplier=1), [onesB], [identB])

    def xsrc(ti):
        if ti < 2:
            return ctx[ti * 128:(ti + 1) * 128, :], 1
        return x[(ti - 2) * 128:(ti - 1) * 128, :], 0

    with ExitStack() as st:
        cs, csB = k.sb(st, "cs", [128, 2, 8])
        srep, srepB = k.sb(st, "srep", [128, 2, 8, 128])
        k.dma("sp", cs[:, 0, :], c.rearrange("(kc p) -> p kc", p=128), csB, None, allow_slow_non_contiguous=True)
        k.dma("sp", cs[:, 1, :], c_ctx.rearrange("(kc p) -> p kc", p=128), csB, None, allow_slow_non_contiguous=True)
        k.act(cs, cs, AF.Silu, [csB], [csB])
        k.cp(srep, cs.unsqueeze(3).broadcast_to([128, 2, 8, 128]), [csB], [srepB])
        adab, adabB = k.sb(st, "adab", [128, 6 * D])
        modt = [k.sb(st, f"modt{i}", [128, 6 * D]) for i in range(2)]
        gb = [k.sb(st, f"gb{i}", [128, D]) for i in range(2)]
        wblk = [k.sb(st, f"wblk{i}", [128, 8, 512]) for i in range(2)]
        for layer in range(2):
            k.dma("sp", adab, ada_b[layer, :].partition_broadcast(128), adabB, None)
            k.dma("sp", gb[0][0], norm1_g[layer, :].partition_broadcast(128), gb[0][1], None)
            k.dma("sp", gb[1][0], norm2_g[layer, :].partition_broadcast(128), gb[1][1], None)
            for nb in range(12):
                wt, wB = wblk[nb % 2]
                k.dma("sp", wt, ada_w[layer, :, nb * 512:(nb + 1) * 512].rearrange("(kc p) n -> p kc n", p=128), wB, None)
                for kind in range(2):
                    ps, psB = psb[(nb * 2 + kind) % 4]
                    for kc in range(8):
                        k.mm(ps, srep[:, kind, kc, :], wt[:, kc, :], kc == 0, kc == 7, [srepB, wB], [psB])
                    k.tt(modt[kind][0][:, nb * 512:(nb + 1) * 512], ps, adab[:, nb * 512:(nb + 1) * 512], ALU.add,
                         [psB, adabB], [modt[kind][1]])
            for kind in range(2):
                mt, mB = modt[kind]
                k.stt(mt[:, D:2 * D], mt[:, D:2 * D], 1.0, gb[0][0], ALU.add, ALU.mult, [mB, gb[0][1]], [mB])
                k.stt(mt[:, 4 * D:5 * D], mt[:, 4 * D:5 * D], 1.0, gb[1][0], ALU.add, ALU.mult, [mB, gb[1][1]], [mB])
                k.dma("sp", modv[layer, kind, :].rearrange("(o n) -> o n", o=1), mt[0:1, :], modvB, mB)
    S.barrier()

    def load_mod(st, q, layer, kind, slot, name):
        t, B = k.sb(st, name, [128, D])
        k.dma(q, t, modv[layer, kind, slot * D:(slot + 1) * D].partition_broadcast(128), B, modvB)
        return t, B

    def norm_mod(xt, xB, ht, hB, At, AB, Bt, BB, tmp):
        jt, jB, ss, ssB = tmp
        k.act(jt, xt, AF.Square, [xB], [jB, ssB], accum=ss)
        k.act(ss, ss, AF.Sqrt, [ssB, epsB], [ssB], scale=1.0 / D, bias=epsb)
        S.op("dve", lambda e: e.reciprocal(out=ss, in_=ss), [ssB], [ssB])
        k.stt(ht, xt, ss, At, ALU.mult, ALU.mult, [xB, ssB, AB], [hB])
        k.tt(ht, ht, Bt, ALU.add, [hB, BB], [hB], eng="pool")

    def groups_of(tiles, gsz=4):
        gs = []
        ctxs = [t for t in tiles if t < 2]
        lat = [t for t in tiles if t >= 2]
        if ctxs:
            gs.append(ctxs)
        for i in range(0, len(lat), gsz):
            gs.append(lat[i:i + gsz])
        return gs

    with ExitStack() as st:
        win, winB = k.sb(st, "win", [128, 8, 1536], BF16)
        k.dma("pool", win, rec_w_in.rearrange("(kc p) n -> p kc n", p=128), winB, None)
        AA = [load_mod(st, "sp", 0, kd, 1, f"A{kd}") for kd in range(2)]
        BBm = [load_mod(st, "sp", 0, kd, 0, f"B{kd}") for kd in range(2)]
        xt2 = [k.sb(st, f"xt{i}", [128, D]) for i in range(2)]
        ht2 = [k.sb(st, f"ht{i}", [128, D]) for i in range(2)]
        jt, jB = k.sb(st, "jt", [128, D])
        ss2 = [k.sb(st, f"ss{i}", [128, 1]) for i in range(2)]
        hT2 = [k.sb(st, f"hT{i}", [128, 8, 512], BF16) for i in range(2)]
        stg2 = [k.sb(st, f"stg{i}", [128, 12, 512]) for i in range(2)]
        tcount = 0
        for gi, g in enumerate(groups_of(list(range(NT)))):
            hT, hTB = hT2[gi % 2]
            n = 128 * len(g)
            col0 = g[0] * 128
            for si, ti in enumerate(g):
                xt, xB = xt2[tcount % 2]; ht, hB = ht2[tcount % 2]; ss, ssB = ss2[tcount % 2]
                src, kd = xsrc(ti)
                k.dma("sp", xt, src, xB, None)
                if si == min(1, len(g) - 1):
                    k.flush()
                norm_mod(xt, xB, ht, hB, AA[kd][0], AA[kd][1], BBm[kd][0], BBm[kd][1], (jt, jB, ss, ssB))
                for b in range(2):
                    ps, psB = psb[(tcount * 2 + b) % 4]
                    for j in range(4):
                        kc = b * 4 + j
                        k.tr(ps[:, j * 128:(j + 1) * 128], ht[:, kc * 128:(kc + 1) * 128], ident, [hB, identB], [psB])
                    k.cp(hT[:, b * 4:(b + 1) * 4, si * 128:(si + 1) * 128], ps.rearrange("p (j t) -> p j t", j=4),
                         [psB], [hTB], eng=("act" if b == 0 else "dve"))
                tcount += 1
            stg, stgB = stg2[gi % 2]
            for oc in range(12):
                ps, psB = psb[4 + oc % 4]
                for kc in range(8):
                    k.mm(ps[:, :n], win[:, kc, oc * 128:(oc + 1) * 128], hT[:, kc, :n], kc == 0, kc == 7, [winB, hTB], [psB])
                k.cp(stg[:, oc, :n], ps[:, :n], [psB], [stgB], eng=("act" if oc % 2 == 0 else "dve"))
            k.defer("sp", projT[:, col0:col0 + n].rearrange("(oc p) t -> p oc t", p=128), stg[:, :, :n], projTB, stgB)
        k.flush()
    S.barrier()

    def blocks_fwd():
        bl = [(0, 256)]
        for c0 in range(256, TT, 512):
            bl.append((c0, 512))
        return bl

    with ExitStack() as st:
        cw, cwB = k.sb(st, "cw", [128, 4, 4])
        cb, cbB = k.sb(st, "cb", [128, 4])
        gba, gbaB = k.sb(st, "gba", [128, 2, 4]); gbx, gbxB = k.sb(st, "gbx", [128, 2, 4])
        lam, lamB = k.sb(st, "lam", [128, 2, 4])
        k.dma("sp", cw, rec_conv_w.rearrange("k (j p) -> p k j", p=128), cwB, None, allow_slow_non_contiguous=True)
        k.dma("sp", cb, rec_conv_b.rearrange("(j p) -> p j", p=128), cbB, None, allow_slow_non_contiguous=True)
        k.dma("sp", gba, lru_ba.rearrange("d (j p) -> p d j", p=128), gbaB, None, allow_slow_non_contiguous=True)
        k.dma("sp", gbx, lru_bx.rearrange("d (j p) -> p d j", p=128), gbxB, None, allow_slow_non_contiguous=True)
        k.dma("sp", lam, lru_lambda.rearrange("d (j p) -> p d j", p=128), lamB, None, allow_slow_non_contiguous=True)
        t1, t1B = k.sb(st, "t1", [128, 2, 4]); t2, t2B = k.sb(st, "t2", [128, 2, 4]); t3, t3B = k.sb(st, "t3", [128, 2, 4])
        cl, clB = k.sb(st, "cl", [128, 2, 4]); cl2, cl2B = k.sb(st, "cl2", [128, 2, 4])
        k.act(t1, lam, AF.Abs, [lamB], [t1B])
        k.act(t2, t1, AF.Exp, [t1B], [t2B], scale=-1.0)
        k.ts(t3, t2, 2.0, ALU.add, [t2B], [t3B])
        S.op("dve", lambda e: e.reciprocal(out=t3, in_=t3), [t3B], [t3B])
        k.tt(t2, t2, t3, ALU.mult, [t2B, t3B], [t2B])
        k.tt(t3, t2, t2, ALU.mult, [t2B], [t3B])
        k.ts(t1, t3, 1.0 / 9, ALU.mult, [t3B], [t1B], s2=1.0 / 7, op1=ALU.add)
        for cf in (1.0 / 5, 1.0 / 3, 1.0):
            k.tt(t1, t1, t3, ALU.mult, [t1B, t3B], [t1B])
            k.ts(t1, t1, cf, ALU.add, [t1B], [t1B])
        k.tt(t1, t1, t2, ALU.mult, [t1B, t2B], [t1B])
        k.ts(t2, lam, -1.0, ALU.mult, [lamB], [t2B], s2=0.0, op1=ALU.max)
        k.stt(t1, t1, 2.0, t2, ALU.mult, ALU.add, [t1B, t2B], [t1B])
        k.ts(cl, t1, -8.0, ALU.mult, [t1B], [clB])
        k.ts(cl2, t1, -16.0, ALU.mult, [t1B], [cl2B])
        wg, wgB = k.sb(st, "wg", [128, 2, 2, 4, 128])
        k.ms(wg, 0.0, [wgB])
        for gi_, wsrc in enumerate((lru_wa, lru_wx)):
            for d in range(2):
                for h in range(8):
                    r0 = (h % 2) * 64
                    k.dma("sp", wg[r0:r0 + 64, gi_, d, h // 2, r0:r0 + 64], wsrc[d, h, :, :], wgB, None)
        u, uB = k.sb(st, "u", [128, TT]); ya, yaB = k.sb(st, "ya", [128, TT])
        NB = 2
        rt = [k.sb(st, f"rt{i}", [128, 512]) for i in range(NB)]
        it = [k.sb(st, f"it{i}", [128, 512]) for i in range(NB)]
        at = [k.sb(st, f"at{i}", [128, 512]) for i in range(NB)]
        sq = [k.sb(st, f"sq{i}", [128, 512]) for i in range(NB)]
        hb = [k.sb(st, f"hb{i}", [128, 512]) for i in range(NB)]
        zero1, zero1B = k.sb(st, "zero1", [128, 1])
        k.ms(zero1, 0.0, [zero1B])
        for j in range(4):
            k.dma("sp", ya, projT[j * 128:(j + 1) * 128, :], yaB, projTB)
            for (s0, s1) in ((0, 256), (256, TT)):
                k.act(u[:, s0:s1], ya[:, s0:s1], AF.Identity, [yaB, cwB, cbB], [uB], scale=cw[:, 2, j:j + 1], bias=cb[:, j:j + 1])
                k.stt(u[:, s0 + 2:s1], ya[:, s0:s1 - 2], cw[:, 0, j:j + 1], u[:, s0 + 2:s1], ALU.mult, ALU.add, [yaB, cwB, uB], [uB])
                k.stt(u[:, s0 + 1:s1], ya[:, s0:s1 - 1], cw[:, 1, j:j + 1], u[:, s0 + 1:s1], ALU.mult, ALU.add, [yaB, cwB, uB], [uB])
                k.stt(u[:, s0:s1 - 1], ya[:, s0 + 1:s1], cw[:, 3, j:j + 1], u[:, s0:s1 - 1], ALU.mult, ALU.add, [yaB, cwB, uB], [uB])
            bi = 0
            for d in range(2):
                bl = blocks_fwd()
                if d == 1:
                    bl = [bl[0]] + bl[1:][::-1]
                carry = (zero1, zero1B)
                for (c0, n) in bl:
                    pa, paB = psb[(bi * 2) % 8]; px, pxB = psb[(bi * 2 + 1) % 8]
                    r_, rB = rt[bi % NB]; i_, iB = it[bi % NB]; a_, aB = at[bi % NB]; s_, sB = sq[bi % NB]; h_, hB = hb[bi % NB]
                    ub = u[:, c0:c0 + n]
                    k.mm(pa[:, :n], wg[:, 0, d, j, :], ub, True, True, [wgB, uB], [paB])
                    k.mm(px[:, :n], wg[:, 1, d, j, :], ub, True, True, [wgB, uB], [pxB])
                    k.act(r_[:, :n], pa[:, :n], AF.Sigmoid, [paB, gbaB], [rB], bias=gba[:, d, j:j + 1])
                    k.act(i_[:, :n], px[:, :n], AF.Sigmoid, [pxB, gbxB], [iB], bias=gbx[:, d, j:j + 1])
                    k.act(a_[:, :n], r_[:, :n], AF.Exp, [rB, clB], [aB], scale=cl[:, d, j:j + 1])
                    k.act(s_[:, :n], r_[:, :n], AF.Exp, [rB, cl2B], [sB], scale=cl2[:, d, j:j + 1])
                    k.ts(s_[:, :n], s_[:, :n], -1.0, ALU.mult, [sB], [sB], s2=1.0, op1=ALU.add, eng="pool")
                    k.act(s_[:, :n], s_[:, :n], AF.Sqrt, [sB], [sB])
                    k.tt(i_[:, :n], i_[:, :n], ub, ALU.mult, [iB, uB], [iB])
                    k.tt(i_[:, :n], i_[:, :n], s_[:, :n], ALU.mult, [iB, sB], [iB])
                    if d == 0:
                        k.scan(ya[:, c0:c0 + n], a_[:, :n], i_[:, :n], carry[0], [aB, iB, carry[1]], [yaB])
                        carry = (ya[:, c0 + n - 1:c0 + n], yaB)
                    else:
                        k.scan(rv(h_[:, :n]), rv(a_[:, :n]), rv(i_[:, :n]), carry[0], [aB, iB, carry[1]], [hB])
                        carry = (h_[:, 0:1], hB)
                        k.tt(ya[:, c0:c0 + n], ya[:, c0:c0 + n], h_[:, :n], ALU.add, [yaB, hB], [yaB], eng="pool")
                    bi += 1
            for (c0, n) in blocks_fwd():
                g_, gB = rt[bi % NB]
                k.dma("sp", g_[:, :n], projT[512 + j * 128:512 + (j + 1) * 128, c0:c0 + n], gB, projTB)
                k.act(g_[:, :n], g_[:, :n], AF.Gelu, [gB], [gB])
                k.tt(ya[:, c0:c0 + n], ya[:, c0:c0 + n], g_[:, :n], ALU.mult, [yaB, gB], [yaB])
                bi += 1
            k.dma("sp", ymixT[j * 128:(j + 1) * 128, :], ya, ymixTB, yaB)
    S.barrier()

    if stop_after <= 2:
        S.finish()
        return k
    L = LCH
    with ExitStack() as st:
        BT, BTB = k.sb(st, "BT", [128, 2, 2, 4, 128])
        Cpad, CpadB = k.sb(st, "Ccomp", [128, 2, 2, 16, 32])
        maskc, maskcB = k.sb(st, "maskc", [128, 4])
        CTAB, CTABB = k.sb(st, "CTAB", [128, 2, 16, L]); STAB, STABB = k.sb(st, "STAB", [128, 2, 16, L])
        dcol, dcolB = k.sb(st, "dcol", [128, 4])
        mag, magB = k.sb(st, "mag", [128, 2, 16]); nsL, nsLB = k.sb(st, "nsL", [128, 2, 16])
        W2, W2B = k.sb(st, "W2", [128, 2, 16, 2])
        stp = ExitStack()
        are, areB = k.sb(stp, "are", [128, 2, 16]); aim, aimB = k.sb(stp, "aim", [128, 2, 16]); ldt, ldtB = k.sb(stp, "ldt", [128, 2, 16])
        bre, breB = k.sb(stp, "bre", [128, 2, 16, 16]); bim, bimB = k.sb(stp, "bim", [128, 2, 16, 16])
        cre, creB = k.sb(stp, "cre", [128, 2, 16, 16]); cim, cimB = k.sb(stp, "cim", [128, 2, 16, 16])
        for d in range(2):
            k.dma("sp", are[:, d, :], s5_a_re[d].rearrange("(gp g2) n -> (g2 n) gp", g2=2), areB, None, allow_slow_non_contiguous=True)
            k.dma("sp", aim[:, d, :], s5_a_im[d].rearrange("(gp g2) n -> (g2 n) gp", g2=2), aimB, None, allow_slow_non_contiguous=True)
            k.dma("sp", bre[:, d, :, :], s5_b_re[d].rearrange("(gp g2) n p -> (g2 n) gp p", g2=2), breB, None)
            k.dma("sp", bim[:, d, :, :], s5_b_im[d].rearrange("(gp g2) n p -> (g2 n) gp p", g2=2), bimB, None)
            for g2 in range(2):
                src = bass.AP(s5_log_dt.tensor, s5_log_dt.offset + d * 32 + g2, [[0, 64], [2, 16]])
                k.dma("sp", ldt[g2 * 64:(g2 + 1) * 64, d, :], src, ldtB, None, allow_slow_non_contiguous=True)
                for (ct_, cB_, csrc) in ((cre, creB, s5_c_re), (cim, cimB, s5_c_im)):
                    for gp_ in range(16):
                        src = bass.AP(csrc.tensor, csrc.offset + d * 32768 + (2 * gp_ + g2) * 1024, [[1, 64], [64, 16]])
                        k.dma("sp", ct_[g2 * 64:(g2 + 1) * 64, d, gp_, :], src, cB_, None, allow_slow_non_contiguous=True)
        nm = [0]
        def sm(shape=[128, 2, 16], dt=F32):
            nm[0] += 1
            return k.sb(stp, f"sm{nm[0]}", shape, dt)
        dtt, dttB = sm(); th, thB = sm(); ki, kiB = sm(dt=I32); kf, kfB = sm()
        sh, shB = sm(); chh, chhB = sm(); sn, snB = sm(); cs_, csB_ = sm(); lbr, lbrB = sm(); lbi, lbiB = sm()
        den, denB = sm(); tq, tqB = sm(); qre, qreB = sm(); qim, qimB = sm()
        k.act(dtt, ldt, AF.Exp, [ldtB], [dttB])
        k.tt(mag, are, dtt, ALU.mult, [areB, dttB], [magB])
        k.act(mag, mag, AF.Exp, [magB], [magB])
        k.tt(th, aim, dtt, ALU.mult, [aimB, dttB], [thB])
        k.ts(th, th, 1.0 / (2 * math.pi), ALU.mult, [thB], [thB])
        k.cp(ki, th, [thB], [kiB]); k.cp(kf, ki, [kiB], [kfB])
        k.tt(th, th, kf, ALU.subtract, [thB, kfB], [thB])
        k.act(sh, th, AF.Sin, [thB], [shB], scale=math.pi)
        k.act(kf, th, AF.Abs, [thB], [kfB])
        k.act(chh, kf, AF.Sin, [kfB, halfpiB], [chhB], scale=-math.pi, bias=halfpi)
        k.stt(sn, sh, 2.0, chh, ALU.mult, ALU.mult, [shB, chhB], [snB])
        k.tt(cs_, sh, sh, ALU.mult, [shB], [csB_])
        k.ts(cs_, cs_, -2.0, ALU.mult, [csB_], [csB_], s2=1.0, op1=ALU.add)
        k.tt(lbr, mag, cs_, ALU.mult, [magB, csB_], [lbrB]); k.tt(lbi, mag, sn, ALU.mult, [magB, snB], [lbiB])
        k.tt(den, are, are, ALU.mult, [areB], [denB]); k.tt(tq, aim, aim, ALU.mult, [aimB], [tqB])
        k.tt(den, den, tq, ALU.add, [denB, tqB], [denB])
        S.op("dve", lambda e: e.reciprocal(out=den, in_=den), [denB], [denB])
        k.ts(lbr, lbr, -1.0, ALU.add, [lbrB], [lbrB])
        k.tt(qre, lbr, are, ALU.mult, [lbrB, areB], [qreB]); k.tt(tq, lbi, aim, ALU.mult, [lbiB, aimB], [tqB])
        k.tt(qre, qre, tq, ALU.add, [qreB, tqB], [qreB]); k.tt(qre, qre, den, ALU.mult, [qreB, denB], [qreB])
        k.tt(qim, lbi, are, ALU.mult, [lbiB, areB], [qimB]); k.tt(tq, lbr, aim, ALU.mult, [lbrB, aimB], [tqB])
        k.tt(qim, qim, tq, ALU.subtract, [qimB, tqB], [qimB]); k.tt(qim, qim, den, ALU.mult, [qimB, denB], [qimB])
        Bblk, BblkB = k.sb(stp, "Bblk", [128, 2, 2, 16, 32])
        k.ms(Bblk, 0.0, [BblkB])
        pA, pAB = sm([128, 2, 16, 16]); pB_, pBB = sm([128, 2, 16, 16])
        qreb = qre.unsqueeze(3).broadcast_to([128, 2, 16, 16]); qimb = qim.unsqueeze(3).broadcast_to([128, 2, 16, 16])
        k.tt(pA, bre, qreb, ALU.mult, [breB, qreB], [pAB]); k.tt(pB_, bim, qimb, ALU.mult, [bimB, qimB], [pBB])
        for g2 in range(2):
            hs = slice(g2 * 64, (g2 + 1) * 64)
            k.tt(Bblk[hs, :, 0, :, g2 * 16:(g2 + 1) * 16], pA[hs], pB_[hs], ALU.subtract, [pAB, pBB], [BblkB])
        k.tt(pA, bim, qreb, ALU.mult, [bimB, qreB], [pAB]); k.tt(pB_, bre, qimb, ALU.mult, [breB, qimB], [pBB])
        for g2 in range(2):
            hs = slice(g2 * 64, (g2 + 1) * 64)
            k.tt(Bblk[hs, :, 1, :, g2 * 16:(g2 + 1) * 16], pA[hs], pB_[hs], ALU.add, [pAB, pBB], [BblkB])
        ti_ = 0
        for d in range(2):
            for ri in range(2):
                for c_ in range(4):
                    ps, psB = psb[ti_ % 4]; ti_ += 1
                    k.tr(ps[:, 0:128], Bblk[:, d, ri, 4 * c_:4 * c_ + 4, :].rearrange("p a b -> p (a b)"), ident, [BblkB, identB], [psB])
                    k.cp(BT[:, d, ri, c_, :], ps[:, 0:128], [psB], [BTB])
        k.ms(Cpad, 0.0, [CpadB])
        for g2 in range(2):
            hs = slice(g2 * 64, (g2 + 1) * 64)
            k.cp(Cpad[hs, :, 0, :, g2 * 16:(g2 + 1) * 16], cre[hs], [creB], [CpadB])
            k.ts(Cpad[hs, :, 1, :, g2 * 16:(g2 + 1) * 16], cim[hs], -1.0, ALU.mult, [cimB], [CpadB])
        for gq in range(4):
            S.op("dve", lambda e, gq=gq: e.tensor_reduce(out=maskc[:, gq:gq + 1], in_=ident[:, gq * 32:(gq + 1) * 32], axis=AX.X, op=ALU.add), [identB], [maskcB])
        ta, taB = sm([128, 2, 16, L // 2]); tb, tbB = sm([128, 2, 16, L // 2])
        k.cp(CTAB[:, :, :, 0], cs_, [csB_], [CTABB]); k.cp(STAB[:, :, :, 0], sn, [snB], [STABB])
        m = 1
        while m < L:
            cm = CTAB[:, :, :, m - 1:m].broadcast_to([128, 2, 16, m]); smm = STAB[:, :, :, m - 1:m].broadcast_to([128, 2, 16, m])
            k.tt(ta[:, :, :, :m], CTAB[:, :, :, 0:m], cm, ALU.mult, [CTABB], [taB])
            k.tt(tb[:, :, :, :m], STAB[:, :, :, 0:m], smm, ALU.mult, [STABB], [tbB])
            k.tt(CTAB[:, :, :, m:2 * m], ta[:, :, :, :m], tb[:, :, :, :m], ALU.subtract, [taB, tbB], [CTABB])
            k.tt(ta[:, :, :, :m], STAB[:, :, :, 0:m], cm, ALU.mult, [STABB, CTABB], [taB])
            k.tt(tb[:, :, :, :m], CTAB[:, :, :, 0:m], smm, ALU.mult, [STABB, CTABB], [tbB])
            k.tt(STAB[:, :, :, m:2 * m], ta[:, :, :, :m], tb[:, :, :, :m], ALU.add, [taB, tbB], [STABB])
            m *= 2
        k.ts(nsL, STAB[:, :, :, L - 1], -1.0, ALU.mult, [STABB], [nsLB])
        k.cp(W2[:, :, :, 0], nsL, [nsLB], [W2B]); k.cp(W2[:, :, :, 1], STAB[:, :, :, L - 1], [STABB], [W2B])
        k.dma("sp", dcol, s5_d.rearrange("(j p) -> p j", p=128), dcolB, None, allow_slow_non_contiguous=True)
        S.barrier()
        stp.close()
        ubT, ubTB = k.sb(st, "ubT", [128, TT]); yacc, yaccB = k.sb(st, "yacc", [128, TT])
        NCH = 4
        ch = []
        for gq in range(NCH):
            dct = {nm_: k.sb(st, f"{nm_}{gq}", [128, 512]) for nm_ in ("um", "t1", "t2", "gre", "gim")}
            dct["ss"] = k.sb(st, f"ss{gq}", [128, 2, 512])
            dct["car"] = [k.sb(st, f"car{gq}_{i}", [128, 2]) for i in range(2)]
            dct["tmpc"] = k.sb(st, f"tmpc{gq}", [128, 2])
            dct["rho"] = k.sb(st, f"rho{gq}", [128, L])
            ch.append(dct)
        zero2, zero2B = k.sb(st, "zero2", [128, 2])
        k.ms(zero2, 0.0, [zero2B])
        for c_ in range(4):
            k.dma("sp", ubT, projT[1024 + c_ * 128:1024 + (c_ + 1) * 128, :], ubTB, projTB)
            k.ms(yacc, 0.0, [yaccB])
            for d in range(2):
                bl = blocks_fwd()
                if d == 1:
                    bl = [bl[0]] + bl[1:][::-1]
                for gq in range(NCH):
                    gp = 4 * c_ + gq
                    ch[gq]["cur"] = (zero2, zero2B); ch[gq]["cidx"] = 0
                    k.cp(ch[gq]["rho"][0], mag[:, d, gp:gp + 1].broadcast_to([128, L]), [magB], [ch[gq]["rho"][1]], eng="pool")
                for (c0, n) in bl:
                    nch = n // L
                    v3 = lambda t_: t_[:, :n].rearrange("p (a l) -> p a l", l=L)
                    tabs = []
                    for gq in range(NCH):
                        gp = 4 * c_ + gq
                        cosv = CTAB[:, d, gp, :]; sinv = STAB[:, d, gp, :]
                        if d == 1:
                            cosv = rv(cosv); sinv = rv(sinv)
                        tabs.append((cosv.unsqueeze(1).broadcast_to([128, nch, L]), sinv.unsqueeze(1).broadcast_to([128, nch, L])))
                    for gq in range(NCH):
                        C = ch[gq]; um, umB = C["um"]
                        pdr, pdrB = psb[2 * gq]; pdi, pdiB = psb[2 * gq + 1]
                        k.act(um[:, :n], ubT[:, c0:c0 + n], AF.Copy, [ubTB, maskcB], [umB], scale=maskc[:, gq:gq + 1])
                        k.mm(pdr[:, :n], BT[:, d, 0, c_, :], um[:, :n], True, True, [BTB, umB], [pdrB])
                        k.mm(pdi[:, :n], BT[:, d, 1, c_, :], um[:, :n], True, True, [BTB, umB], [pdiB])
                    for step in range(3):
                        for gq in range(NCH):
                            C = ch[gq]; cosb, sinb = tabs[gq]
                            pdr, pdrB = psb[2 * gq]; pdi, pdiB = psb[2 * gq + 1]
                            t1, t1B_ = C["t1"]; t2, t2B_ = C["t2"]; gre, greB = C["gre"]; gim, gimB = C["gim"]
                            if step == 0:
                                k.tt(v3(t1), v3(pdr), cosb, ALU.mult, [pdrB, CTABB], [t1B_])
                                k.tt(v3(t2), v3(pdi), sinb, ALU.mult, [pdiB, STABB], [t2B_])
                                k.tt(gre[:, :n], t1[:, :n], t2[:, :n], ALU.add, [t1B_, t2B_], [greB], eng="pool")
                            elif step == 1:
                                k.tt(v3(t1), v3(pdi), cosb, ALU.mult, [pdiB, CTABB], [t1B_])
                                k.tt(v3(t2), v3(pdr), sinb, ALU.mult, [pdrB, STABB], [t2B_])
                                k.tt(gim[:, :n], t1[:, :n], t2[:, :n], ALU.subtract, [t1B_, t2B_], [gimB], eng="pool")
                    order = list(range(nch)) if d == 0 else list(range(nch - 1, -1, -1))
                    for ci in order:
                        sl = slice(ci * L, (ci + 1) * L)
                        for op_ in range(4):
                            for gq in range(NCH):
                                C = ch[gq]; gp = 4 * c_ + gq
                                gre, greB = C["gre"]; gim, gimB = C["gim"]; ss, ssB = C["ss"]
                                rho, rhoB = C["rho"]; tmpc, tmpcB = C["tmpc"]; cur = C["cur"]
                                cLc = CTAB[:, d, gp, L - 1:L]
                                li_ = (ci + 1) * L - 1 if d == 0 else ci * L
                                last2 = ss[:, :, li_]
                                f = (lambda a_: a_) if d == 0 else rv
                                if op_ == 0:
                                    k.scan(f(ss[:, 0, sl]), rho, f(gre[:, sl]), cur[0][:, 0:1], [rhoB, greB, cur[1]], [ssB])
                                elif op_ == 1:
                                    k.scan(f(ss[:, 1, sl]), rho, f(gim[:, sl]), cur[0][:, 1:2], [rhoB, gimB, cur[1]], [ssB])
                                elif op_ == 2:
                                    C["nxt"] = C["car"][C["cidx"] % 2]; C["cidx"] += 1
                                    k.tt(tmpc, rv(last2), W2[:, d, gp, :], ALU.mult, [ssB, W2B], [tmpcB])
                                else:
                                    k.stt(C["nxt"][0], last2, cLc, tmpc, ALU.mult, ALU.add, [ssB, CTABB, tmpcB], [C["nxt"][1]])
                                    C["cur"] = C["nxt"]
                    for step in range(2):
                        for gq in range(NCH):
                            C = ch[gq]; cosb, sinb = tabs[gq]
                            e4 = "pool" if gq == 3 else "dve"
                            t1, t1B_ = C["t1"]; t2, t2B_ = C["t2"]; gre, greB = C["gre"]; gim, gimB = C["gim"]
                            ss, ssB = C["ss"]; sre = ss[:, 0, :]; sim = ss[:, 1, :]; sreB = ssB; simB = ssB
                            if step == 0:
                                k.tt(v3(t1), v3(sre), cosb, ALU.mult, [sreB, CTABB], [t1B_], eng=e4)
                                k.tt(v3(t2), v3(sim), sinb, ALU.mult, [simB, STABB], [t2B_], eng=e4)
                                k.tt(gre[:, :n], t1[:, :n], t2[:, :n], ALU.subtract, [t1B_, t2B_], [greB], eng=e4)
                            else:
                                k.tt(v3(t1), v3(sre), sinb, ALU.mult, [sreB, STABB], [t1B_], eng=e4)
                                k.tt(v3(t2), v3(sim), cosb, ALU.mult, [simB, CTABB], [t2B_], eng=e4)
                                k.tt(gim[:, :n], t1[:, :n], t2[:, :n], ALU.add, [t1B_, t2B_], [gimB], eng=e4)
                    cc0 = Cpad[:, d, 0, 4 * c_:4 * c_ + 4, :].rearrange("p a b -> p (a b)")
                    cc1 = Cpad[:, d, 1, 4 * c_:4 * c_ + 4, :].rearrange("p a b -> p (a b)")
                    for gq in range(NCH):
                        C = ch[gq]; gre, greB = C["gre"]; gim, gimB = C["gim"]
                        py, pyB = psb[2 * gq]
                        k.mm(py[:, :n], cc0, gre[:, :n], True, False, [CpadB, greB], [pyB])
                        k.mm(py[:, :n], cc1, gim[:, :n], False, True, [CpadB, gimB], [pyB])
                        k.stt(yacc[:, c0:c0 + n], py[:, :n], maskc[:, gq:gq + 1], yacc[:, c0:c0 + n], ALU.mult, ALU.add,
                              [pyB, maskcB, yaccB], [yaccB])
            k.stt(yacc, ubT, dcol[:, c_:c_ + 1], yacc, ALU.mult, ALU.add, [ubTB, dcolB, yaccB], [yaccB])
            k.act(yacc, yacc, AF.Gelu, [yaccB], [yaccB])
            k.dma("sp", ysgT[c_ * 128:(c_ + 1) * 128, :], yacc, ysgTB, yaccB)
    S.barrier()
    if stop_after <= 3:
        S.finish()
        return k
    def load_wbf(st, name, src2d, kchunks, ncols, q="pool"):
        t, B = k.sb(st, name, [128, kchunks, ncols], BF16)
        k.dma(q, t, src2d.rearrange("(kc p) n -> p kc n", p=128), B, None)
        return t, B

    with ExitStack() as st:
        gw, gwB = load_wbf(st, "gw", s5_glu_w, 4, 512)
        wo, woB = load_wbf(st, "wo", rec_w_out, 8, D)
        glub, glubB = k.sb(st, "glub", [128, 4])
        k.dma("sp", glub, s5_glu_b.rearrange("(j p) -> p j", p=128), glubB, None, allow_slow_non_contiguous=True)
        G1 = [load_mod(st, "sp", 0, kd, 2, f"G1{kd}") for kd in range(2)]
        yaf = [k.sb(st, f"yaf{i}", [128, 4, 512]) for i in range(2)]
        ysf = [k.sb(st, f"ysf{i}", [128, 4, 512]) for i in range(2)]
        yab = [k.sb(st, f"yab{i}", [128, 4, 512], BF16) for i in range(2)]
        ysb = [k.sb(st, f"ysb{i}", [128, 4, 512], BF16) for i in range(2)]
        ys2 = [k.sb(st, f"ys2{i}", [128, 4, 512], BF16) for i in range(2)]
        sg = [k.sb(st, f"sg{i}", [128, 512]) for i in range(2)]
        xt2 = [k.sb(st, f"xt{i}", [128, D]) for i in range(2)]
        ot2 = [k.sb(st, f"ot{i}", [128, D]) for i in range(2)]
        tc_ = 0
        pc_ = 0
        for bi, (c0, n) in enumerate(blocks_fwd()):
            ya_, yaB_ = yaf[bi % 2]; ys_, ysB_ = ysf[bi % 2]; yb_, ybB_ = yab[bi % 2]; sb_, sbB_ = ysb[bi % 2]; y2_, y2B_ = ys2[bi % 2]
            k.dma("sp", ya_[:, :, :n], ymixT[:, c0:c0 + n].rearrange("(j p) t -> p j t", p=128), yaB_, ymixTB)
            k.dma("sp", ys_[:, :, :n], ysgT[:, c0:c0 + n].rearrange("(j p) t -> p j t", p=128), ysB_, ysgTB)
            k.cp(yb_[:, :, :n], ya_[:, :, :n], [yaB_], [ybB_], eng="act")
            k.cp(sb_[:, :, :n], ys_[:, :, :n], [ysB_], [sbB_], eng="pool")
            for oc in range(4):
                ps, psB = psb[pc_ % 8]; pc_ += 1
                for kc in range(4):
                    k.mm(ps[:, :n], gw[:, kc, oc * 128:(oc + 1) * 128], sb_[:, kc, :n], kc == 0, kc == 3, [gwB, sbB_], [psB])
                s_, sB_ = sg[oc % 2]
                k.act(s_[:, :n], ps[:, :n], AF.Sigmoid, [psB, glubB], [sB_], bias=glub[:, oc:oc + 1])
                k.tt(y2_[:, oc, :n], ys_[:, oc, :n], s_[:, :n], ALU.mult, [ysB_, sB_], [y2B_])
            for s in range(n // 128):
                ti = c0 // 128 + s
                xt, xB = xt2[tc_ % 2]; ot, oB = ot2[tc_ % 2]; tc_ += 1
                src, kd = xsrc(ti)
                k.dma("sp", xt, src, xB, None)
                k.flush()
                for half in range(2):
                    ps, psB = psb[pc_ % 8]; pc_ += 1
                    for kc in range(8):
                        lh = yb_[:, kc, s * 128:(s + 1) * 128] if kc < 4 else y2_[:, kc - 4, s * 128:(s + 1) * 128]
                        k.mm(ps, lh, wo[:, kc, half * 512:(half + 1) * 512], kc == 0, kc == 7, [ybB_, y2B_, woB], [psB])
                    k.tt(ot[:, half * 512:(half + 1) * 512], ps, G1[kd][0][:, half * 512:(half + 1) * 512], ALU.mult, [psB, G1[kd][1]], [oB])
                k.tt(ot, ot, xt, ALU.add, [oB, xB], [oB], eng="pool")
                k.defer("sp", x1[ti * 128:(ti + 1) * 128, :], ot, x1B, oB)
        k.flush()
    S.barrier()
    if stop_after <= 4:
        S.finish()
        return k

    def moe(layer, tiles, src_fn, dst_fn, dstB, final):
        with ExitStack() as st:
            kinds = sorted(set(kd for _, kd in tiles))
            A2 = {kd: load_mod(st, "sp", layer, kd, 4, f"A2{kd}") for kd in kinds}
            B2 = {kd: load_mod(st, "sp", layer, kd, 3, f"B2{kd}") for kd in kinds}
            G2 = {kd: load_mod(st, "sp", layer, kd, 5, f"G2{kd}") for kd in kinds}
            Wr, WrB = k.sb(st, "Wr", [128, 8, 36])
            rb, rbB = k.sb(st, "rb", [128, 36])
            k.dma("sp", Wr[:, :, 0:4], moe_r1_w[layer].rearrange("(kc p) g -> p kc g", p=128), WrB, None)
            k.dma("sp", rb[:, 0:4], moe_r1_b[layer, :].partition_broadcast(128), rbB, None)
            for g in range(4):
                k.dma("sp", Wr[:, :, 4 + 8 * g:12 + 8 * g], moe_r2_w[layer, g].rearrange("(kc p) e -> p kc e", p=128), WrB, None)
                k.dma("sp", rb[:, 4 + 8 * g:12 + 8 * g], moe_r2_b[layer, g, :].partition_broadcast(128), rbB, None)
            if final:
                fg, fgB = k.sb(st, "fg", [128, D])
                k.dma("sp", fg, final_norm_g.partition_broadcast(128), fgB, None)
            SUP = 8
            hT, hTB = k.sb(st, "hT", [128, 8, SUP * 128], BF16)
            acc, accB = k.sb(st, "acc", [128, SUP, D])
            gates, gatesB = k.sb(st, "gates", [128, SUP, 32])
            xt2 = [k.sb(st, f"xt{i}", [128, D]) for i in range(2)]
            ht2 = [k.sb(st, f"ht{i}", [128, D]) for i in range(2)]
            jt, jB = k.sb(st, "jt", [128, D])
            ss2 = [k.sb(st, f"ss{i}", [128, 1]) for i in range(2)]
            hTf2 = [k.sb(st, f"hTf{i}", [128, 8, 128]) for i in range(2)]
            wgt = [k.sb(st, f"wgt{i}", [128, 8, 512], BF16) for i in range(2)]
            wut = [k.sb(st, f"wut{i}", [128, 8, 512], BF16) for i in range(2)]
            wdt = [k.sb(st, f"wdt{i}", [128, 4, D], BF16) for i in range(2)]
            sil = [k.sb(st, f"sil{i}", [128, 512]) for i in range(2)]
            hid = [k.sb(st, f"hid{i}", [128, 4, 512], BF16) for i in range(2)]
            lg, lgB = k.sb(st, "lg", [128, 36]); l2m, l2mB = k.sb(st, "l2m", [128, 32])
            sm_ = {nm_: k.sb(st, f"g_{nm_}", [128, 8]) for nm_ in ("m1", "nm1", "e4", "se", "gm", "top", "d12", "w2", "dw")}
            mk1, mk1B = k.sb(st, "mk1", [128, 32]); mk2, mk2B = k.sb(st, "mk2", [128, 32])
            ecount = 0
            pd_i = 0
            for s0 in range(0, len(tiles), SUP):
                sup = tiles[s0:s0 + SUP]
                ns = len(sup)
                for s, (ti, kd) in enumerate(sup):
                    xt, xB = xt2[s % 2]; ht, hB = ht2[s % 2]; ss, ssB = ss2[s % 2]; hTf, hTfB = hTf2[s % 2]
                    src, srcB = src_fn(ti)
                    k.dma("sp", xt, src, xB, srcB)
                    norm_mod(xt, xB, ht, hB, A2[kd][0], A2[kd][1], B2[kd][0], B2[kd][1], (jt, jB, ss, ssB))
                    for b in range(2):
                        ps, psB = psb[(s * 2 + b) % 2]
                        for j in range(4):
                            kc = b * 4 + j
                            k.tr(ps[:, j * 128:(j + 1) * 128], ht[:, kc * 128:(kc + 1) * 128], ident, [hB, identB], [psB])
                        k.cp(hTf[:, b * 4:(b + 1) * 4, :], ps.rearrange("p (j t) -> p j t", j=4), [psB], [hTfB], eng=("act" if b == 0 else "dve"))
                    k.cp(hT[:, :, s * 128:(s + 1) * 128], hTf, [hTfB], [hTB], eng="pool")
                    pr, prB = psb[2 + s % 2]
                    for kc in range(8):
                        k.mm(pr[:, 0:36], hTf[:, kc, :], Wr[:, kc, :], kc == 0, kc == 7, [hTfB, WrB], [prB])
                    k.tt(lg, pr[:, 0:36], rb, ALU.add, [prB, rbB], [lgB])
                    m1, m1B = sm_["m1"]; nm1, nm1B = sm_["nm1"]; e4, e4B = sm_["e4"]; se, seB = sm_["se"]; gm, gmB = sm_["gm"]
                    top, topB = sm_["top"]; d12, d12B = sm_["d12"]; w2, w2B = sm_["w2"]; dw, dwB = sm_["dw"]
                    S.op("dve", lambda e, m1=m1: e.tensor_reduce(out=m1[:, 0:1], in_=lg[:, 0:4], axis=AX.X, op=ALU.max), [lgB], [m1B])
                    k.ts(nm1[:, 0:1], m1[:, 0:1], -1.0, ALU.mult, [m1B], [nm1B])
                    k.act(e4[:, 0:4], lg[:, 0:4], AF.Exp, [lgB, nm1B], [e4B, seB], bias=nm1[:, 0:1], accum=se[:, 0:1])
                    S.op("dve", lambda e, se=se: e.reciprocal(out=se[:, 0:1], in_=se[:, 0:1]), [seB], [seB])
                    k.ts(gm[:, 0:4], lg[:, 0:4], m1[:, 0:1], ALU.is_ge, [lgB, m1B], [gmB])
                    k.ts(gm[:, 0:4], gm[:, 0:4], 30000.0, ALU.mult, [gmB], [gmB], s2=-30000.0, op1=ALU.add)
                    k.tt(l2m.rearrange("p (g e) -> p g e", g=4), lg[:, 4:36].rearrange("p (g e) -> p g e", g=4),
                         gm[:, 0:4].unsqueeze(2).broadcast_to([128, 4, 8]), ALU.add, [lgB, gmB], [l2mB])
                    S.op("dve", lambda e, top=top: e.max(out=top[:, 0:8], in_=l2m), [l2mB], [topB])
                    k.tt(d12[:, 0:1], top[:, 1:2], top[:, 0:1], ALU.subtract, [topB], [d12B])
                    k.act(w2[:, 0:1], d12[:, 0:1], AF.Sigmoid, [d12B], [w2B])
                    k.tt(w2[:, 0:1], w2[:, 0:1], se[:, 0:1], ALU.mult, [w2B, seB], [w2B])
                    k.stt(dw[:, 0:1], w2[:, 0:1], -2.0, se[:, 0:1], ALU.mult, ALU.add, [w2B, seB], [dwB])
                    k.ts(mk2, l2m, top[:, 1:2], ALU.is_ge, [l2mB, topB], [mk2B])
                    k.ts(mk1, l2m, top[:, 0:1], ALU.is_ge, [l2mB, topB], [mk1B])
                    k.ts(mk2, mk2, w2[:, 0:1], ALU.mult, [mk2B, w2B], [mk2B])
                    k.stt(gates[:, s, :], mk1, dw[:, 0:1], mk2, ALU.mult, ALU.add, [mk1B, dwB, mk2B], [gatesB])
                grp = [list(range(i, min(i + 4, ns))) for i in range(0, ns, 4)]
                for e_ in range(32):
                    wg_, wgB_ = wgt[ecount % 2]; wu_, wuB_ = wut[ecount % 2]; wd_, wdB_ = wdt[ecount % 2]; ecount += 1
                    k.dma("pool", wg_, moe_w_gate[layer, e_].rearrange("(kc p) f -> p kc f", p=128), wgB_, None)
                    k.dma("pool", wu_, moe_w_up[layer, e_].rearrange("(kc p) f -> p kc f", p=128), wuB_, None)
                    k.dma("pool", wd_, moe_w_down[layer, e_].rearrange("(fc p) n -> p fc n", p=128), wdB_, None)
                    for gi, gt in enumerate(grp):
                        n = 128 * len(gt); cc = gt[0] * 128
                        hd, hdB = hid[gi % 2]
                        for fc in range(4):
                            pg, pgB = psb[fc % 2]; pu, puB = psb[2 + fc % 2]
                            for kc in range(8):
                                k.mm(pg[:, :n], wg_[:, kc, fc * 128:(fc + 1) * 128], hT[:, kc, cc:cc + n], kc == 0, kc == 7, [wgB_, hTB], [pgB])
                            for kc in range(8):
                                k.mm(pu[:, :n], wu_[:, kc, fc * 128:(fc + 1) * 128], hT[:, kc, cc:cc + n], kc == 0, kc == 7, [wuB_, hTB], [puB])
                            sl_, slB = sil[fc % 2]
                            k.act(sl_[:, :n], pg[:, :n], AF.Silu, [pgB], [slB])
                            k.tt(hd[:, fc, :n], sl_[:, :n], pu[:, :n], ALU.mult, [slB, puB], [hdB])
                    for gi, gt in enumerate(grp):
                        hd, hdB = hid[gi % 2]
                        for si, s in enumerate(gt):
                            for half in range(2):
                                pd, pdB = psb[4 + pd_i % 4]; pd_i += 1
                                for fc in range(4):
                                    k.mm(pd, hd[:, fc, si * 128:(si + 1) * 128], wd_[:, fc, half * 512:(half + 1) * 512], fc == 0, fc == 3, [hdB, wdB_], [pdB])
                                av = acc[:, s, half * 512:(half + 1) * 512]
                                if e_ == 0:
                                    k.ts(av, pd, gates[:, s, e_:e_ + 1], ALU.mult, [pdB, gatesB], [accB])
                                else:
                                    k.stt(av, pd, gates[:, s, e_:e_ + 1], av, ALU.mult, ALU.add, [pdB, gatesB, accB], [accB])
                for s, (ti, kd) in enumerate(sup):
                    xt, xB = xt2[s % 2]; ot, oB = ht2[s % 2]; ss, ssB = ss2[s % 2]
                    src, srcB = src_fn(ti)
                    k.dma("sp", xt, src, xB, srcB)
                    k.flush()
                    k.tt(ot, acc[:, s, :], G2[kd][0], ALU.mult, [accB, G2[kd][1]], [oB], eng="pool")
                    k.tt(ot, ot, xt, ALU.add, [oB, xB], [oB])
                    if final:
                        k.act(jt, ot, AF.Square, [oB], [jB, ssB], accum=ss)
                        k.act(ss, ss, AF.Sqrt, [ssB, epsB], [ssB], scale=1.0 / D, bias=epsb)
                        S.op("dve", lambda e, ss=ss: e.reciprocal(out=ss, in_=ss), [ssB], [ssB])
                        k.stt(ot, ot, ss, fg, ALU.mult, ALU.mult, [oB, ssB, fgB], [oB])
                    k.defer("sp", dst_fn(ti), ot, dstB, oB)
                k.flush()
        S.barrier()

    moe(0, [(ti, 1 if ti < 2 else 0) for ti in range(NT)], lambda ti: (x1[ti * 128:(ti + 1) * 128, :], x1B),
        lambda ti: x2[ti * 128:(ti + 1) * 128, :], x2B, False)
    if stop_after <= 5:
        S.finish()
        return k
    NTL = T // 128
    ROWS = T // 64
    QT, QTB = k.dscr("QT", [D, T], BF16)
    KT, KTB = k.dscr("KT", [D, TT], BF16)
    VA, VAB = k.dscr("VA", [TT, 16, 65], BF16)
    AO, AOB = k.dscr("AO", [T, D])
    REV, REVB = k.dscr("REV", [16, 15, 128])
    with ExitStack() as st:
        wq, wqB = load_wbf(st, "wqkv", na_w_qkv, 8, 3 * D)
        AA = [load_mod(st, "sp", 1, kd, 1, f"A{kd}") for kd in range(2)]
        BBm = [load_mod(st, "sp", 1, kd, 0, f"B{kd}") for kd in range(2)]
        xt2 = [k.sb(st, f"xt{i}", [128, D]) for i in range(2)]
        ht2 = [k.sb(st, f"ht{i}", [128, D]) for i in range(2)]
        jt, jB = k.sb(st, "jt", [128, D])
        ss2 = [k.sb(st, f"ss{i}", [128, 1]) for i in range(2)]
        hT2 = [k.sb(st, f"hT{i}", [128, 8, 512], BF16) for i in range(2)]
        qst2 = [k.sb(st, f"qst{i}", [128, 8, 512], BF16) for i in range(2)]
        kst2 = [k.sb(st, f"kst{i}", [128, 8, 512], BF16) for i in range(2)]
        vst2 = [k.sb(st, f"vst{i}", [128, 16, 65], BF16) for i in range(2)]
        for i in range(2):
            k.ms(vst2[i][0], 1.0, [vst2[i][1]])
        tcount = 0; pc_ = 0
        for gi, g in enumerate(groups_of(list(range(NT)))):
            hT, hTB = hT2[gi % 2]
            n = 128 * len(g); col0 = g[0] * 128
            isctx = g[0] < 2
            for si, ti in enumerate(g):
                xt, xB = xt2[tcount % 2]; ht, hB = ht2[tcount % 2]; ss, ssB = ss2[tcount % 2]
                kd = 1 if ti < 2 else 0
                k.dma("sp", xt, x2[ti * 128:(ti + 1) * 128, :], xB, x2B)
                norm_mod(xt, xB, ht, hB, AA[kd][0], AA[kd][1], BBm[kd][0], BBm[kd][1], (jt, jB, ss, ssB))
                for b in range(2):
                    ps, psB = psb[(tcount * 2 + b) % 2]
                    for j in range(4):
                        kc = b * 4 + j
                        k.tr(ps[:, j * 128:(j + 1) * 128], ht[:, kc * 128:(kc + 1) * 128], ident, [hB, identB], [psB])
                    k.cp(hT[:, b * 4:(b + 1) * 4, si * 128:(si + 1) * 128], ps.rearrange("p (j t) -> p j t", j=4),
                         [psB], [hTB], eng=("act" if b == 0 else "dve"))
                tcount += 1
            qst, qstB = qst2[gi % 2]; kst, kstB = kst2[gi % 2]
            for oc in range(16):
                if isctx and oc < 8:
                    continue
                ps, psB = psb[2 + pc_ % 3]; pc_ += 1
                for kc in range(8):
                    k.mm(ps[:, :n], wq[:, kc, oc * 128:(oc + 1) * 128], hT[:, kc, :n], kc == 0, kc == 7, [wqB, hTB], [psB])
                if oc < 8:
                    k.act(qst[:, oc, :n], ps[:, :n], AF.Copy, [psB], [qstB], scale=0.125)
                else:
                    k.cp(kst[:, oc - 8, :n], ps[:, :n], [psB], [kstB])
            if not isctx:
                k.dma("sp", QT[:, col0 - CT:col0 - CT + n].rearrange("(oc p) t -> p oc t", p=128), qst[:, :, :n], QTB, qstB)
            k.dma("sp", KT[:, col0:col0 + n].rearrange("(oc p) t -> p oc t", p=128), kst[:, :, :n], KTB, kstB)
            for si, ti in enumerate(g):
                vst, vstB = vst2[ti % 2]
                for half in range(2):
                    ps, psB = psb[5 + pc_ % 3]; pc_ += 1
                    for kc in range(8):
                        k.mm(ps, hT[:, kc, si * 128:(si + 1) * 128], wq[:, kc, 2 * D + half * 512:2 * D + (half + 1) * 512], kc == 0, kc == 7, [wqB, hTB], [psB])
                    k.cp(vst[:, half * 8:(half + 1) * 8, 0:64], ps.rearrange("p (h e) -> p h e", h=8), [psB], [vstB], eng=("act" if half == 0 else "dve"))
                k.dma("sp", VA[ti * 128:(ti + 1) * 128, :, :], vst, VAB, vstB)
    S.barrier()
    if stop_after <= 6:
        S.finish()
        return k

    with ExitStack() as st:
        T2, T2B = k.sb(st, "T2", [128, 14, 16, 64])
        zt, ztB = k.sb(st, "zt", [128, 256])
        k.ms(zt, 0.0, [ztB])
        k.dma("sp", REV.rearrange("h d j -> (h d j)").rearrange("(p f) -> p f", p=120), zt[0:120, :], REVB, ztB)
        rpt, rptB = k.sb(st, "rpt", [16, 15, 31])
        k.dma("sp", rpt, na_rpb, rptB, None)
        k.dma("sp", REV[:, :, 48:79], rpt, REVB, rptB)
        with ExitStack() as st2:
            T2r, T2rB = k.sb(st2, "T2r", [128, 7, 16, 64])
            for hf in range(2):
                for dl in range(2):
                    for a_ in range(7):
                        drb = hf * 7 + a_
                        src = bass.AP(REV.tensor, REV.offset + (drb + dl) * 128, [[1, 64], [15 * 128, 16], [1, 64]])
                        k.dma("sp", T2r[dl * 64:(dl + 1) * 64, a_, :, :], src, T2rB, REVB)
                k.cp(T2[:, hf * 7:(hf + 1) * 7, :, :].rearrange("p a h q -> p (a h) q"),
                     rv(T2r.rearrange("p a h q -> p (a h) q")), [T2rB], [T2B], eng="dve")
        S.barrier()
        Mt, MtB = k.sb(st, "Mt", [128, 64])
        k.ms(Mt, 0.0, [MtB])
        NEG = -30000.0
        for dl in range(2):
            hs = slice(dl * 64, (dl + 1) * 64)
            sels = [(slice(0, 8), [[0, 8]], 15, -1), (slice(8, 57), [[-1, 49]], 0, 1), (slice(8, 57), [[1, 49]], 15, -1),
                    (slice(57, 64), [[0, 7]], -48, 1)]
            for (qs, pat, base, cm) in sels:
                S.op("pool", lambda e, hs=hs, qs=qs, pat=pat, base=base, cm=cm: e.affine_select(
                    out=Mt[hs, qs], in_=Mt[hs, qs], pattern=pat, compare_op=ALU.is_ge, fill=NEG, base=base, channel_multiplier=cm), [MtB], [MtB])
        T2v = T2.rearrange("p a h q -> p (a h) q")
        k.tt(T2v, T2v, Mt.unsqueeze(1).broadcast_to([128, 224, 64]), ALU.add, [T2B, MtB], [T2B])
        if "T2d" in k.dbg:
            T2d, T2dB = k.dscr("T2d", [128, 14 * 16 * 64])
            k.dma("sp", T2d, T2.rearrange("p a h q -> p (a h q)"), T2dB, T2B)
        KcT, KcTB = k.sb(st, "KcT", [128, 8, 256], BF16)
        Vc, VcB = k.sb(st, "Vc", [128, 2, 1040], BF16)
        k.dma("sp", KcT, KT[:, 0:256].rearrange("(c p) t -> p c t", p=128), KcTB, KTB)
        k.dma("sp", Vc, VA[0:256].rearrange("(a p) h e -> p a (h e)", p=128), VcB, VAB)
        qbd2 = [k.sb(st, f"qbd{i}", [128, 8, 128], BF16) for i in range(2)]
        for i in range(2):
            k.ms(qbd2[i][0], 0.0, [qbd2[i][1]])
        kT2 = [k.sb(st, f"kT{i}", [128, 8, 512], BF16) for i in range(2)]
        vv2 = [k.sb(st, f"vv{i}", [128, 4, 1040], BF16) for i in range(2)]
        sb2 = [k.sb(st, f"sbt{i}", [128, 1024]) for i in range(2)]
        pT2 = [k.sb(st, f"pT{i}", [128, 1024], BF16) for i in range(2)]
        ao2 = [k.sb(st, f"ao{i}", [128, 8, 64]) for i in range(2)]
        rec2 = [k.sb(st, f"rec{i}", [128, 8]) for i in range(2)]
        ci_ = 0
        P7 = int(os.environ.get('P7', '9'))
        def row_loads(r):
            rs_ = min(max(r - 4, 0), ROWS - 8)
            qbd, qbdB = qbd2[r % 2]; kT, kTB = kT2[r % 2]; vv, vvB = vv2[r % 2]
            qsrc = QT[:, r * 64:(r + 1) * 64].rearrange("(c p) t -> p c t", p=128)
            k.dma("sp", qbd[0:64, :, 0:64], qsrc[0:64], qbdB, QTB)
            k.dma("sp", qbd[64:128, :, 64:128], qsrc[64:128], qbdB, QTB)
            k0 = CT + rs_ * 64
            k.dma("sp", kT, KT[:, k0:k0 + 512].rearrange("(c p) t -> p c t", p=128), kTB, KTB)
            k.dma("sp", vv, VA[k0:k0 + 512].rearrange("(a p) h e -> p a (h e)", p=128), vvB, VAB)
        if P7 >= 1:
            row_loads(0)
        for r in range(ROWS if P7 >= 1 else 0):
            rs_ = min(max(r - 4, 0), ROWS - 8)
            qbd, qbdB = qbd2[r % 2]; kT, kTB = kT2[r % 2]; vv, vvB = vv2[r % 2]
            if r + 1 < ROWS:
                row_loads(r + 1)
            for kc in range(6):
                sbank = [psb[(ci_ % 2) * 2], psb[(ci_ % 2) * 2 + 1]]
                sbt, sbtB = sb2[ci_ % 2]; pT, pTB = pT2[ci_ % 2]; ci_ += 1
                for c in range(8):
                    if kc < 4:
                        lh = kT[:, c, kc * 128:(kc + 1) * 128]; lB = kTB
                    else:
                        lh = KcT[:, c, (kc - 4) * 128:(kc - 3) * 128]; lB = KcTB
                    sp_, spB = sbank[c // 4]
                    k.mm(sp_[:, (c % 4) * 128:(c % 4 + 1) * 128], lh, qbd[:, c, :], True, True, [lB, qbdB], [spB])
                if kc < 4:
                    drb = 2 * kc + (rs_ - r + 7)
                    for b in range(2):
                        k.tt(sbt[:, b * 512:(b + 1) * 512], sbank[b][0], T2[:, drb, b * 8:(b + 1) * 8, :].rearrange("p h q -> p (h q)"),
                             ALU.add, [sbank[b][1], T2B], [sbtB])
                    k.act(pT, sbt, AF.Exp, [sbtB], [pTB])
                    vsrc = vv[:, kc, :]; vB_ = vvB
                else:
                    for b in range(2):
                        k.act(pT[:, b * 512:(b + 1) * 512], sbank[b][0], AF.Exp, [sbank[b][1]], [pTB])
                    vsrc = Vc[:, kc - 4, :]; vB_ = VcB
                for c in range(8 if P7 >= 2 else 0):
                    ob, obB = psb[4 + c // 3]
                    k.mm(ob[:, (c % 3) * 130:(c % 3 + 1) * 130], pT[:, c * 128:(c + 1) * 128], vsrc[:, c * 130:(c + 1) * 130],
                         (kc == 0 and c % 3 == 0), kc == 5, [pTB, vB_], [obB], skip=True)
            ao, aoB = ao2[r % 2]; rec, recB = rec2[r % 2]
            for b in range(3 if P7 >= 3 else 0):
                np_ = 3 if b < 2 else 2
                ob, obB = psb[4 + b]
                for par in range(2):
                    ps_ = slice(par * 64, (par + 1) * 64)
                    ov = ob[ps_, 0:np_ * 130].rearrange("p (c e) -> p c e", e=130)
                    S.op("dve", lambda e, rec=rec, ps_=ps_, b=b, np_=np_, ov=ov, par=par: e.reciprocal(
                        out=rec[ps_, b * 3:b * 3 + np_], in_=ov[:, :, par * 65 + 64]), [obB], [recB])
                    k.tt(ao[ps_, b * 3:b * 3 + np_, :], ov[:, :, par * 65:par * 65 + 64],
                         rec[ps_, b * 3:b * 3 + np_].unsqueeze(2).broadcast_to([64, np_, 64]), ALU.mult, [obB, recB], [aoB])
            if P7 >= 3:
                dstv = AO[r * 64:(r + 1) * 64, :].rearrange("q (c p e) -> q c p e", p=2, e=64)
                for par in range(2):
                    k.dma("sp", dstv[:, :, par, :], ao[par * 64:(par + 1) * 64, :, :], AOB, aoB)
    S.barrier()
    if stop_after <= 7:
        S.finish()
        return k

    with ExitStack() as st:
        wo, woB = load_wbf(st, "wo1", na_w_out, 8, D)
        G1, G1B = load_mod(st, "sp", 1, 0, 2, "G1l1")
        at2 = [k.sb(st, f"at{i}", [128, D]) for i in range(2)]
        xt2 = [k.sb(st, f"xt{i}", [128, D]) for i in range(2)]
        ot2 = [k.sb(st, f"ot{i}", [128, D]) for i in range(2)]
        aT2 = [k.sb(st, f"aT{i}", [128, 8, 128], BF16) for i in range(2)]
        for tl in range(NTL):
            at_, atB = at2[tl % 2]; xt, xB = xt2[tl % 2]; ot, oB = ot2[tl % 2]; aT, aTB = aT2[tl % 2]
            k.dma("sp", at_, AO[tl * 128:(tl + 1) * 128, :], atB, AOB)
            k.dma("sp", xt, x2[CT + tl * 128:CT + (tl + 1) * 128, :], xB, x2B)
            k.flush()
            for b in range(2):
                ps, psB = psb[(tl * 2 + b) % 4]
                for j in range(4):
                    kc = b * 4 + j
                    k.tr(ps[:, j * 128:(j + 1) * 128], at_[:, kc * 128:(kc + 1) * 128], ident, [atB, identB], [psB])
                k.cp(aT[:, b * 4:(b + 1) * 4, :], ps.rearrange("p (j t) -> p j t", j=4), [psB], [aTB], eng=("act" if b == 0 else "dve"))
            for half in range(2):
                ps, psB = psb[4 + (tl * 2 + half) % 4]
                for kc in range(8):
                    k.mm(ps, aT[:, kc, :], wo[:, kc, half * 512:(half + 1) * 512], kc == 0, kc == 7, [aTB, woB], [psB])
                k.tt(ot[:, half * 512:(half + 1) * 512], ps, G1[:, half * 512:(half + 1) * 512], ALU.mult, [psB, G1B], [oB])
            k.tt(ot, ot, xt, ALU.add, [oB, xB], [oB], eng="pool")
            k.defer("sp", x3[tl * 128:(tl + 1) * 128, :], ot, x3B, oB)
        k.flush()
    S.barrier()
    if stop_after <= 8:
        S.finish()
        return k
    moe(1, [(tl, 0) for tl in range(NTL)], lambda tl: (x3[tl * 128:(tl + 1) * 128, :], x3B),
        lambda tl: out[tl * 128:(tl + 1) * 128, :], obuf, True)
    S.finish()
    return k


_CACHE = {}


def kernel(**inputs):
    T = inputs["x"].shape[1]
    if T not in _CACHE:
        _CACHE[T] = build(T)
    kb = _CACHE[T]
    nb = inputs["x"].shape[0]
    squeeze = ("rec_", "lru_", "s5_", "na_")
    shared = {}
    for name, v in inputs.items():
        if name in ("x", "c", "ctx"):
            continue
        a = np.asarray(v, dtype=np.float32)
        if name.startswith(squeeze):
            a = a[0]
        shared[name] = np.ascontiguousarray(a)
    in_maps = []
    for b in range(nb):
        m = dict(shared)
        m["x"] = np.ascontiguousarray(np.asarray(inputs["x"][b], dtype=np.float32))
        m["c"] = np.ascontiguousarray(np.asarray(inputs["c"][b], dtype=np.float32))
        m["ctx"] = np.ascontiguousarray(np.asarray(inputs["ctx"][b], dtype=np.float32))
        in_maps.append({kk: vv for kk, vv in m.items() if kk in kb.inp})
    res = run_bass_kernel_spmd(kb.nc, in_maps, core_ids=list(range(nb)))
    return np.stack([np.asarray(res.results[b]["out"]) for b in range(nb)], axis=0).astype(np.float32)
```

```python
import math
import os
import numpy as np
import concourse.bass as bass
import concourse.mybir as mybir
from concourse.bass_utils import run_bass_kernel_spmd

F32 = mybir.dt.float32
BF16 = mybir.dt.bfloat16
I32 = mybir.dt.int32
AF = mybir.ActivationFunctionType
ALU = mybir.AluOpType
AX = mybir.AxisListType
SEM_ROT = 30000
D = 1024
CT = 256
LCH = 128


class Buf:
    __slots__ = ("name", "w", "r", "dsem", "dcnt", "gen")

    def __init__(self, name):
        self.name = name
        self.w = None
        self.r = []
        self.dsem = None
        self.dcnt = 0
        self.gen = -1


class Sched:
    def __init__(self, nc):
        self.nc = nc
        self.eobj = {"pe": nc.tensor, "act": nc.scalar, "dve": nc.vector, "pool": nc.gpsimd, "sp": nc.sync}
        self.prog = {k: [] for k in self.eobj}
        self.sem = {}
        self.cnt = {}
        self.seen = {k: {} for k in self.eobj}
        self.nsem = 0
        self.dsemval = {}
        self.dsems = []
        self.allsems = []
        self.freed = []
        self.freed_sw = []
        self.semsw = {}
        self.gen = 0
        for k in self.eobj:
            self._newsem(k)
        self.ninstr = {k: 0 for k in self.eobj}

    def _alloc_sem(self, name):
        self.nsem += 1
        s = self.nc.alloc_semaphore(name=name)
        return s

    def _newsem(self, k):
        self.sem[k] = self._alloc_sem(f"e_{k}_{self.nsem}")
        self.cnt[k] = 0
        self.allsems.append((k, self.sem[k]))

    def _wait(self, eng, ev):
        sem, val = ev
        sid = id(sem)
        if sid in self.dsemval:
            val = self.dsemval[sid]
        if self.seen[eng].get(sid, 0) >= val:
            return
        self.seen[eng][sid] = val
        self.prog[eng].append(lambda e, sem=sem, val=val: e.wait_ge(sem, val))

    def op(self, eng, fn, reads=(), writes=()):
        my = self.sem[eng]
        pe = eng == "pe"
        for b in reads:
            if b.w is not None and not (pe and b.w[0] is my):
                self._wait(eng, b.w)
        for b in writes:
            if b.w is not None and not (pe and b.w[0] is my):
                self._wait(eng, b.w)
            for ev in b.r:
                if not (pe and ev[0] is my):
                    self._wait(eng, ev)
        if self.cnt[eng] >= SEM_ROT:
            self._newsem(eng)
            my = self.sem[eng]
        self.cnt[eng] += 1
        ev = (my, self.cnt[eng])
        self.prog[eng].append(lambda e, fn=fn, my=my: fn(e).then_inc(my, 1))
        self.ninstr[eng] += 1
        for b in writes:
            b.w = ev
            b.r = []
        for b in reads:
            if b not in writes:
                b.r.append(ev)
                if len(b.r) > 16:
                    b.r = b.r[-16:]
        return ev

    def dma(self, q, out_ap, in_ap, dst, src, **kw):
        b = dst if dst is not None else src
        sw = q == "pool"
        if b.dsem is None or b.gen != self.gen or b.dcnt * 16 >= SEM_ROT or self.semsw.get(id(b.dsem)) != sw:
            pool_ = self.freed_sw if sw else self.freed
            while pool_ and pool_[-1][1] * 16 >= SEM_ROT - 4000:
                pool_.pop()
            if pool_:
                b.dsem, b.dcnt = pool_.pop()
            else:
                b.dsem = self._alloc_sem(f"d_{b.name}_{self.nsem}")
                b.dcnt = 0
            self.semsw[id(b.dsem)] = sw
            b.gen = self.gen
            self.dsems.append(b.dsem)
        if src is not None and src.w is not None:
            self._wait(q, src.w)
        if dst is not None:
            if dst.w is not None:
                self._wait(q, dst.w)
            for ev in dst.r:
                self._wait(q, ev)
        b.dcnt += 1
        sem = b.dsem
        ev = (sem, b.dcnt * 16)
        self.dsemval[id(sem)] = b.dcnt * 16
        self.prog[q].append(
            lambda e, o=out_ap, i=in_ap, sem=sem, kw=kw: e.dma_start(out=o, in_=i, **kw).then_inc(sem, 16))
        self.ninstr[q] += 1
        if dst is not None:
            dst.w = ev
            dst.r = []
        if src is not None:
            src.r.append(ev)
        return ev

    def barrier(self):
        evs = [(self.sem[k], self.cnt[k]) for k in self.eobj if self.cnt[k] > 0]
        devs = [(s, self.dsemval[id(s)]) for s in self.dsems]
        for e in self.eobj:
            for ev in evs:
                if ev[0] is self.sem[e] and e == "pe":
                    continue
                self._wait(e, ev)
            for ev in devs:
                self._wait(e, ev)
        for sm_ in self.dsems:
            (self.freed_sw if self.semsw[id(sm_)] else self.freed).append((sm_, self.dsemval[id(sm_)] // 16))
        self.dsems = []
        self.gen += 1

    def finish(self):
        self.barrier()
        nc = self.nc
        prog = self.prog
        with nc.Block() as block:
            @block.sync
            def _(e):
                for t in prog["sp"]:
                    t(e)

            @block.tensor
            def _(e):
                for t in prog["pe"]:
                    t(e)

            @block.scalar
            def _(e):
                for t in prog["act"]:
                    t(e)

            @block.vector
            def _(e):
                for t in prog["dve"]:
                    t(e)

            @block.gpsimd
            def _(e):
                for t in prog["pool"]:
                    t(e)


def rv(ap):
    a = [list(x) for x in ap.ap]
    st, n = a[-1]
    a[-1] = [-st, n]
    return bass.AP(ap.tensor, ap.offset + st * (n - 1), a)


class K:
    def __init__(self, T, dbg=()):
        self.T = T
        self.TT = T + CT
        self.NT = self.TT // 128
        self.dbg = set(dbg)
        self.nc = bass.Bass("TRN2", target_bir_lowering=False)
        self.S = Sched(self.nc)
        self.uid = 0
        self.inp = {}
        self.pending = []

    def din(self, name, shape):
        t = self.nc.dram_tensor(name, list(shape), F32, kind="ExternalInput").ap()
        self.inp[name] = t
        return t

    def dscr(self, name, shape, dt=F32):
        kind = "ExternalOutput" if name in self.dbg else "Internal"
        return self.nc.dram_tensor(name, list(shape), dt, kind=kind).ap(), Buf(name)

    def sb(self, st, name, shape, dt=F32):
        self.uid += 1
        t = st.enter_context(self.nc.sbuf_tensor(f"{name}_{self.uid}", list(shape), dt))
        return t.ap(), Buf(name)

    def act(self, out, in_, func, r, w, scale=None, bias=None, accum=None):
        kw = {}
        if scale is not None:
            kw["scale"] = scale
        if bias is not None:
            kw["bias"] = bias
        if accum is not None:
            kw["accum_out"] = accum
        return self.S.op("act", lambda e: e.activation(out=out, in_=in_, func=func, **kw), r, w)

    def tt(self, out, a, b, op, r, w, eng="dve"):
        return self.S.op(eng, lambda e: e.tensor_tensor(out=out, in0=a, in1=b, op=op), r, w)

    def ts(self, out, a, s1, op0, r, w, s2=None, op1=None, eng="dve"):
        if op1 is None:
            return self.S.op(eng, lambda e: e.tensor_scalar(out=out, in0=a, scalar1=s1, scalar2=None, op0=op0), r, w)
        return self.S.op(eng, lambda e: e.tensor_scalar(out=out, in0=a, scalar1=s1, scalar2=s2, op0=op0, op1=op1), r, w)

    def stt(self, out, a, s, b, op0, op1, r, w):
        return self.S.op("dve", lambda e: e.scalar_tensor_tensor(out=out, in0=a, scalar=s, in1=b, op0=op0, op1=op1), r, w)

    def cp(self, out, in_, r, w, eng="dve"):
        if eng == "act":
            return self.S.op("act", lambda e: e.copy(out=out, in_=in_), r, w)
        return self.S.op(eng, lambda e: e.tensor_copy(out=out, in_=in_), r, w)

    def ms(self, ap, val, w, eng="pool"):
        return self.S.op(eng, lambda e: e.memset(ap, val), (), w)

    def mm(self, out, lhsT, rhs, start, stop, r, w, skip=False):
        return self.S.op("pe", lambda e: e.matmul(out, lhsT, rhs, start=start, stop=stop, skip_group_check=skip), r, w)

    def tr(self, out, in_, ident, r, w):
        return self.S.op("pe", lambda e: e.transpose(out, in_, ident), r, w)

    def scan(self, out, d0, d1, init, r, w):
        return self.S.op("dve", lambda e: e.tensor_tensor_scan(out=out, data0=d0, data1=d1, initial=init,
                                                               op0=ALU.mult, op1=ALU.add), r, w)

    def dma(self, q, out, in_, dst, src, **kw):
        return self.S.dma(q, out, in_, dst, src, **kw)

    def defer(self, *a, **kw):
        self.pending.append((a, kw))

    def flush(self):
        for a, kw in self.pending:
            self.S.dma(*a, **kw)
        self.pending = []


def build(T=8192, dbg=(), stop_after=99):
    from contextlib import ExitStack
    k = K(T, dbg)
    nc, S = k.nc, k.S
    TT, NT = k.TT, k.NT
    x = k.din("x", [T, D]); c = k.din("c", [D]); ctx = k.din("ctx", [CT, D]); c_ctx = k.din("c_ctx", [D])
    ada_w = k.din("ada_w", [2, D, 6 * D]); ada_b = k.din("ada_b", [2, 6 * D])
    norm1_g = k.din("norm1_g", [2, D]); norm2_g = k.din("norm2_g", [2, D])
    rec_w_in = k.din("rec_w_in", [D, 1536]); rec_conv_w = k.din("rec_conv_w", [4, 512]); rec_conv_b = k.din("rec_conv_b", [512])
    lru_wa = k.din("lru_wa", [2, 8, 64, 64]); lru_ba = k.din("lru_ba", [2, 512])
    lru_wx = k.din("lru_wx", [2, 8, 64, 64]); lru_bx = k.din("lru_bx", [2, 512]); lru_lambda = k.din("lru_lambda", [2, 512])
    s5_a_re = k.din("s5_a_re", [2, 32, 64]); s5_a_im = k.din("s5_a_im", [2, 32, 64]); s5_log_dt = k.din("s5_log_dt", [2, 32])
    s5_b_re = k.din("s5_b_re", [2, 32, 64, 16]); s5_b_im = k.din("s5_b_im", [2, 32, 64, 16])
    s5_c_re = k.din("s5_c_re", [2, 32, 16, 64]); s5_c_im = k.din("s5_c_im", [2, 32, 16, 64])
    s5_d = k.din("s5_d", [512]); s5_glu_w = k.din("s5_glu_w", [512, 512]); s5_glu_b = k.din("s5_glu_b", [512])
    rec_w_out = k.din("rec_w_out", [D, D])
    na_w_qkv = k.din("na_w_qkv", [D, 3 * D]); na_w_out = k.din("na_w_out", [D, D]); na_rpb = k.din("na_rpb", [16, 15, 31])
    moe_r1_w = k.din("moe_r1_w", [2, D, 4]); moe_r1_b = k.din("moe_r1_b", [2, 4])
    moe_r2_w = k.din("moe_r2_w", [2, 4, D, 8]); moe_r2_b = k.din("moe_r2_b", [2, 4, 8])
    moe_w_gate = k.din("moe_w_gate", [2, 32, D, 512]); moe_w_up = k.din("moe_w_up", [2, 32, D, 512])
    moe_w_down = k.din("moe_w_down", [2, 32, 512, D]); final_norm_g = k.din("final_norm_g", [D])
    out = nc.dram_tensor("out", [T, D], F32, kind="ExternalOutput").ap()
    obuf = Buf("out")

    modv, modvB = k.dscr("modv", [2, 2, 6 * D])
    projT, projTB = k.dscr("projT", [1536, TT])
    ymixT, ymixTB = k.dscr("ymixT", [512, TT])
    ysgT, ysgTB = k.dscr("ysgT", [512, TT])
    x1, x1B = k.dscr("x1", [TT, D])
    x2, x2B = k.dscr("x2", [TT, D])
    x3, x3B = k.dscr("x3", [T, D])

    gst = ExitStack()
    psb = []
    for i in range(8):
        t = gst.enter_context(nc.psum_tensor(f"psb{i}", [128, 512], F32))
        psb.append((t.ap(), Buf(f"psb{i}")))
    ident, identB = k.sb(gst, "ident", [128, 128])
    epsb, epsB = k.sb(gst, "epsb", [128, 1])
    halfpi, halfpiB = k.sb(gst, "halfpi", [128, 1])
    k.ms(ident, 0.0, [identB])
    S.op("pool", lambda e: e.memset(epsb, 1e-6), (), [epsB])
    S.op("pool", lambda e: e.memset(halfpi, math.pi / 2), (), [halfpiB])
    ones_t, onesB = k.sb(gst, "ones", [128, 128])
    k.ms(ones_t, 1.0, [onesB])
    S.op("pool", lambda e: e.affine_select(out=ident, in_=ones_t, pattern=[[-1, 128]], compare_op=ALU.is_equal,
                                            fill=0.0, base=0, channel_multiplier=1), [onesB], [identB])

    def xsrc(ti):
        if ti < 2:
            return ctx[ti * 128:(ti + 1) * 128, :], 1
        return x[(ti - 2) * 128:(ti - 1) * 128, :], 0

    with ExitStack() as st:
        cs, csB = k.sb(st, "cs", [128, 2, 8])
        srep, srepB = k.sb(st, "srep", [128, 2, 8, 128])
        k.dma("sp", cs[:, 0, :], c.rearrange("(kc p) -> p kc", p=128), csB, None, allow_slow_non_contiguous=True)
        k.dma("sp", cs[:, 1, :], c_ctx.rearrange("(kc p) -> p kc", p=128), csB, None, allow_slow_non_contiguous=True)
        k.act(cs, cs, AF.Silu, [csB], [csB])
        k.cp(srep, cs.unsqueeze(3).broadcast_to([128, 2, 8, 128]), [csB], [srepB])
        adab, adabB = k.sb(st, "adab", [128, 6 * D])
        modt = [k.sb(st, f"modt{i}", [128, 6 * D]) for i in range(2)]
        gb = [k.sb(st, f"gb{i}", [128, D]) for i in range(2)]
        wblk = [k.sb(st, f"wblk{i}", [128, 8, 512]) for i in range(2)]
        for layer in range(2):
            k.dma("sp", adab, ada_b[layer, :].partition_broadcast(128), adabB, None)
            k.dma("sp", gb[0][0], norm1_g[layer, :].partition_broadcast(128), gb[0][1], None)
            k.dma("sp", gb[1][0], norm2_g[layer, :].partition_broadcast(128), gb[1][1], None)
            for nb in range(12):
                wt, wB = wblk[nb % 2]
                k.dma("sp", wt, ada_w[layer, :, nb * 512:(nb + 1) * 512].rearrange("(kc p) n -> p kc n", p=128), wB, None)
                for kind in range(2):
                    ps, psB = psb[(nb * 2 + kind) % 4]
                    for kc in range(8):
                        k.mm(ps, srep[:, kind, kc, :], wt[:, kc, :], kc == 0, kc == 7, [srepB, wB], [psB])
                    k.tt(modt[kind][0][:, nb * 512:(nb + 1) * 512], ps, adab[:, nb * 512:(nb + 1) * 512], ALU.add,
                         [psB, adabB], [modt[kind][1]])
            for kind in range(2):
                mt, mB = modt[kind]
                k.stt(mt[:, D:2 * D], mt[:, D:2 * D], 1.0, gb[0][0], ALU.add, ALU.mult, [mB, gb[0][1]], [mB])
                k.stt(mt[:, 4 * D:5 * D], mt[:, 4 * D:5 * D], 1.0, gb[1][0], ALU.add, ALU.mult, [mB, gb[1][1]], [mB])
                k.dma("sp", modv[layer, kind, :].rearrange("(o n) -> o n", o=1), mt[0:1, :], modvB, mB)
    S.barrier()

    def load_mod(st, q, layer, kind, slot, name):
        t, B = k.sb(st, name, [128, D])
        k.dma(q, t, modv[layer, kind, slot * D:(slot + 1) * D].partition_broadcast(128), B, modvB)
        return t, B

    def norm_mod(xt, xB, ht, hB, At, AB, Bt, BB, tmp):
        jt, jB, ss, ssB = tmp
        k.act(jt, xt, AF.Square, [xB], [jB, ssB], accum=ss)
        k.act(ss, ss, AF.Sqrt, [ssB, epsB], [ssB], scale=1.0 / D, bias=epsb)
        S.op("dve", lambda e: e.reciprocal(out=ss, in_=ss), [ssB], [ssB])
        k.stt(ht, xt, ss, At, ALU.mult, ALU.mult, [xB, ssB, AB], [hB])
        k.tt(ht, ht, Bt, ALU.add, [hB, BB], [hB])

    def groups_of(tiles, gsz=4):
        gs = []
        ctxs = [t for t in tiles if t < 2]
        lat = [t for t in tiles if t >= 2]
        if ctxs:
            gs.append(ctxs)
        for i in range(0, len(lat), gsz):
            gs.append(lat[i:i + gsz])
        return gs

    with ExitStack() as st:
        win, winB = k.sb(st, "win", [128, 8, 1536], BF16)
        k.dma("pool", win, rec_w_in.rearrange("(kc p) n -> p kc n", p=128), winB, None)
        AA = [load_mod(st, "sp", 0, kd, 1, f"A{kd}") for kd in range(2)]
        BBm = [load_mod(st, "sp", 0, kd, 0, f"B{kd}") for kd in range(2)]
        xt2 = [k.sb(st, f"xt{i}", [128, D]) for i in range(2)]
        ht2 = [k.sb(st, f"ht{i}", [128, D]) for i in range(2)]
        jt, jB = k.sb(st, "jt", [128, D])
        ss2 = [k.sb(st, f"ss{i}", [128, 1]) for i in range(2)]
        hT2 = [k.sb(st, f"hT{i}", [128, 8, 512], BF16) for i in range(2)]
        stg2 = [k.sb(st, f"stg{i}", [128, 12, 512]) for i in range(2)]
        tcount = 0
        for gi, g in enumerate(groups_of(list(range(NT)))):
            hT, hTB = hT2[gi % 2]
            n = 128 * len(g)
            col0 = g[0] * 128
            for si, ti in enumerate(g):
                xt, xB = xt2[tcount % 2]; ht, hB = ht2[tcount % 2]; ss, ssB = ss2[tcount % 2]
                src, kd = xsrc(ti)
                k.dma("sp", xt, src, xB, None)
                if si == min(1, len(g) - 1):
                    k.flush()
                norm_mod(xt, xB, ht, hB, AA[kd][0], AA[kd][1], BBm[kd][0], BBm[kd][1], (jt, jB, ss, ssB))
                for b in range(2):
                    ps, psB = psb[(tcount * 2 + b) % 4]
                    for j in range(4):
                        kc = b * 4 + j
                        k.tr(ps[:, j * 128:(j + 1) * 128], ht[:, kc * 128:(kc + 1) * 128], ident, [hB, identB], [psB])
                    k.cp(hT[:, b * 4:(b + 1) * 4, si * 128:(si + 1) * 128], ps.rearrange("p (j t) -> p j t", j=4),
                         [psB], [hTB], eng=("act" if b == 0 else "dve"))
                tcount += 1
            stg, stgB = stg2[gi % 2]
            for oc in range(12):
                ps, psB = psb[4 + oc % 4]
                for kc in range(8):
                    k.mm(ps[:, :n], win[:, kc, oc * 128:(oc + 1) * 128], hT[:, kc, :n], kc == 0, kc == 7, [winB, hTB], [psB])
                k.cp(stg[:, oc, :n], ps[:, :n], [psB], [stgB], eng=("act" if oc % 2 == 0 else "dve"))
            k.defer("sp", projT[:, col0:col0 + n].rearrange("(oc p) t -> p oc t", p=128), stg[:, :, :n], projTB, stgB)
        k.flush()
    S.barrier()

    def blocks_fwd():
        bl = [(0, 256)]
        for c0 in range(256, TT, 512):
            bl.append((c0, 512))
        return bl

    with ExitStack() as st:
        cw, cwB = k.sb(st, "cw", [128, 4, 4])
        cb, cbB = k.sb(st, "cb", [128, 4])
        gba, gbaB = k.sb(st, "gba", [128, 2, 4]); gbx, gbxB = k.sb(st, "gbx", [128, 2, 4])
        lam, lamB = k.sb(st, "lam", [128, 2, 4])
        k.dma("sp", cw, rec_conv_w.rearrange("k (j p) -> p k j", p=128), cwB, None, allow_slow_non_contiguous=True)
        k.dma("sp", cb, rec_conv_b.rearrange("(j p) -> p j", p=128), cbB, None, allow_slow_non_contiguous=True)
        k.dma("sp", gba, lru_ba.rearrange("d (j p) -> p d j", p=128), gbaB, None, allow_slow_non_contiguous=True)
        k.dma("sp", gbx, lru_bx.rearrange("d (j p) -> p d j", p=128), gbxB, None, allow_slow_non_contiguous=True)
        k.dma("sp", lam, lru_lambda.rearrange("d (j p) -> p d j", p=128), lamB, None, allow_slow_non_contiguous=True)
        t1, t1B = k.sb(st, "t1", [128, 2, 4]); t2, t2B = k.sb(st, "t2", [128, 2, 4]); t3, t3B = k.sb(st, "t3", [128, 2, 4])
        cl, clB = k.sb(st, "cl", [128, 2, 4]); cl2, cl2B = k.sb(st, "cl2", [128, 2, 4])
        k.act(t1, lam, AF.Abs, [lamB], [t1B])
        k.act(t2, t1, AF.Exp, [t1B], [t2B], scale=-1.0)
        k.ts(t3, t2, 2.0, ALU.add, [t2B], [t3B])
        S.op("dve", lambda e: e.reciprocal(out=t3, in_=t3), [t3B], [t3B])
        k.tt(t2, t2, t3, ALU.mult, [t2B, t3B], [t2B])
        k.tt(t3, t2, t2, ALU.mult, [t2B], [t3B])
        k.ts(t1, t3, 1.0 / 9, ALU.mult, [t3B], [t1B], s2=1.0 / 7, op1=ALU.add)
        for cf in (1.0 / 5, 1.0 / 3, 1.0):
            k.tt(t1, t1, t3, ALU.mult, [t1B, t3B], [t1B])
            k.ts(t1, t1, cf, ALU.add, [t1B], [t1B])
        k.tt(t1, t1, t2, ALU.mult, [t1B, t2B], [t1B])
        k.ts(t2, lam, -1.0, ALU.mult, [lamB], [t2B], s2=0.0, op1=ALU.max)
        k.stt(t1, t1, 2.0, t2, ALU.mult, ALU.add, [t1B, t2B], [t1B])
        k.ts(cl, t1, -8.0, ALU.mult, [t1B], [clB])
        k.ts(cl2, t1, -16.0, ALU.mult, [t1B], [cl2B])
        wg, wgB = k.sb(st, "wg", [128, 2, 2, 4, 128])
        k.ms(wg, 0.0, [wgB])
        for gi_, wsrc in enumerate((lru_wa, lru_wx)):
            for d in range(2):
                for h in range(8):
                    r0 = (h % 2) * 64
                    k.dma("sp", wg[r0:r0 + 64, gi_, d, h // 2, r0:r0 + 64], wsrc[d, h, :, :], wgB, None)
        u, uB = k.sb(st, "u", [128, TT]); ya, yaB = k.sb(st, "ya", [128, TT])
        NB = 2
        rt = [k.sb(st, f"rt{i}", [128, 512]) for i in range(NB)]
        it = [k.sb(st, f"it{i}", [128, 512]) for i in range(NB)]
        at = [k.sb(st, f"at{i}", [128, 512]) for i in range(NB)]
        sq = [k.sb(st, f"sq{i}", [128, 512]) for i in range(NB)]
        hb = [k.sb(st, f"hb{i}", [128, 512]) for i in range(NB)]
        zero1, zero1B = k.sb(st, "zero1", [128, 1])
        k.ms(zero1, 0.0, [zero1B])
        for j in range(4):
            k.dma("sp", ya, projT[j * 128:(j + 1) * 128, :], yaB, projTB)
            for (s0, s1) in ((0, 256), (256, TT)):
                k.act(u[:, s0:s1], ya[:, s0:s1], AF.Identity, [yaB, cwB, cbB], [uB], scale=cw[:, 2, j:j + 1], bias=cb[:, j:j + 1])
                k.stt(u[:, s0 + 2:s1], ya[:, s0:s1 - 2], cw[:, 0, j:j + 1], u[:, s0 + 2:s1], ALU.mult, ALU.add, [yaB, cwB, uB], [uB])
                k.stt(u[:, s0 + 1:s1], ya[:, s0:s1 - 1], cw[:, 1, j:j + 1], u[:, s0 + 1:s1], ALU.mult, ALU.add, [yaB, cwB, uB], [uB])
                k.stt(u[:, s0:s1 - 1], ya[:, s0 + 1:s1], cw[:, 3, j:j + 1], u[:, s0:s1 - 1], ALU.mult, ALU.add, [yaB, cwB, uB], [uB])
            bi = 0
            for d in range(2):
                bl = blocks_fwd()
                if d == 1:
                    bl = [bl[0]] + bl[1:][::-1]
                carry = (zero1, zero1B)
                for (c0, n) in bl:
                    pa, paB = psb[(bi * 2) % 8]; px, pxB = psb[(bi * 2 + 1) % 8]
                    r_, rB = rt[bi % NB]; i_, iB = it[bi % NB]; a_, aB = at[bi % NB]; s_, sB = sq[bi % NB]; h_, hB = hb[bi % NB]
                    ub = u[:, c0:c0 + n]
                    k.mm(pa[:, :n], wg[:, 0, d, j, :], ub, True, True, [wgB, uB], [paB])
                    k.mm(px[:, :n], wg[:, 1, d, j, :], ub, True, True, [wgB, uB], [pxB])
                    k.act(r_[:, :n], pa[:, :n], AF.Sigmoid, [paB, gbaB], [rB], bias=gba[:, d, j:j + 1])
                    k.act(i_[:, :n], px[:, :n], AF.Sigmoid, [pxB, gbxB], [iB], bias=gbx[:, d, j:j + 1])
                    k.act(a_[:, :n], r_[:, :n], AF.Exp, [rB, clB], [aB], scale=cl[:, d, j:j + 1])
                    k.act(s_[:, :n], r_[:, :n], AF.Exp, [rB, cl2B], [sB], scale=cl2[:, d, j:j + 1])
                    k.ts(s_[:, :n], s_[:, :n], -1.0, ALU.mult, [sB], [sB], s2=1.0, op1=ALU.add, eng="pool")
                    k.act(s_[:, :n], s_[:, :n], AF.Sqrt, [sB], [sB])
                    k.tt(i_[:, :n], i_[:, :n], ub, ALU.mult, [iB, uB], [iB])
                    k.tt(i_[:, :n], i_[:, :n], s_[:, :n], ALU.mult, [iB, sB], [iB])
                    if d == 0:
                        k.scan(ya[:, c0:c0 + n], a_[:, :n], i_[:, :n], carry[0], [aB, iB, carry[1]], [yaB])
                        carry = (ya[:, c0 + n - 1:c0 + n], yaB)
                    else:
                        k.scan(rv(h_[:, :n]), rv(a_[:, :n]), rv(i_[:, :n]), carry[0], [aB, iB, carry[1]], [hB])
                        carry = (h_[:, 0:1], hB)
                        k.tt(ya[:, c0:c0 + n], ya[:, c0:c0 + n], h_[:, :n], ALU.add, [yaB, hB], [yaB], eng="pool")
                    bi += 1
            for (c0, n) in blocks_fwd():
                g_, gB = rt[bi % NB]
                k.dma("sp", g_[:, :n], projT[512 + j * 128:512 + (j + 1) * 128, c0:c0 + n], gB, projTB)
                k.act(g_[:, :n], g_[:, :n], AF.Gelu, [gB], [gB])
                k.tt(ya[:, c0:c0 + n], ya[:, c0:c0 + n], g_[:, :n], ALU.mult, [yaB, gB], [yaB])
                bi += 1
            k.dma("sp", ymixT[j * 128:(j + 1) * 128, :], ya, ymixTB, yaB)
    S.barrier()

    if stop_after <= 2:
        S.finish()
        return k
    L = LCH
    with ExitStack() as st:
        BT, BTB = k.sb(st, "BT", [128, 2, 2, 4, 128])
        Cpad, CpadB = k.sb(st, "Ccomp", [128, 2, 2, 16, 32])
        maskc, maskcB = k.sb(st, "maskc", [128, 4])
        CTAB, CTABB = k.sb(st, "CTAB", [128, 2, 16, L]); STAB, STABB = k.sb(st, "STAB", [128, 2, 16, L])
        dcol, dcolB = k.sb(st, "dcol", [128, 4])
        mag, magB = k.sb(st, "mag", [128, 2, 16]); nsL, nsLB = k.sb(st, "nsL", [128, 2, 16])
        W2, W2B = k.sb(st, "W2", [128, 2, 16, 2])
        stp = ExitStack()
        are, areB = k.sb(stp, "are", [128, 2, 16]); aim, aimB = k.sb(stp, "aim", [128, 2, 16]); ldt, ldtB = k.sb(stp, "ldt", [128, 2, 16])
        bre, breB = k.sb(stp, "bre", [128, 2, 16, 16]); bim, bimB = k.sb(stp, "bim", [128, 2, 16, 16])
        cre, creB = k.sb(stp, "cre", [128, 2, 16, 16]); cim, cimB = k.sb(stp, "cim", [128, 2, 16, 16])
        for d in range(2):
            k.dma("sp", are[:, d, :], s5_a_re[d].rearrange("(gp g2) n -> (g2 n) gp", g2=2), areB, None, allow_slow_non_contiguous=True)
            k.dma("sp", aim[:, d, :], s5_a_im[d].rearrange("(gp g2) n -> (g2 n) gp", g2=2), aimB, None, allow_slow_non_contiguous=True)
            k.dma("sp", bre[:, d, :, :], s5_b_re[d].rearrange("(gp g2) n p -> (g2 n) gp p", g2=2), breB, None)
            k.dma("sp", bim[:, d, :, :], s5_b_im[d].rearrange("(gp g2) n p -> (g2 n) gp p", g2=2), bimB, None)
            for g2 in range(2):
                src = bass.AP(s5_log_dt.tensor, s5_log_dt.offset + d * 32 + g2, [[0, 64], [2, 16]])
                k.dma("sp", ldt[g2 * 64:(g2 + 1) * 64, d, :], src, ldtB, None, allow_slow_non_contiguous=True)
                for (ct_, cB_, csrc) in ((cre, creB, s5_c_re), (cim, cimB, s5_c_im)):
                    for gp_ in range(16):
                        src = bass.AP(csrc.tensor, csrc.offset + d * 32768 + (2 * gp_ + g2) * 1024, [[1, 64], [64, 16]])
                        k.dma("sp", ct_[g2 * 64:(g2 + 1) * 64, d, gp_, :], src, cB_, None, allow_slow_non_contiguous=True)
        nm = [0]
        def sm(shape=[128, 2, 16], dt=F32):
            nm[0] += 1
            return k.sb(stp, f"sm{nm[0]}", shape, dt)
        dtt, dttB = sm(); th, thB = sm(); ki, kiB = sm(dt=I32); kf, kfB = sm()
        sh, shB = sm(); chh, chhB = sm(); sn, snB = sm(); cs_, csB_ = sm(); lbr, lbrB = sm(); lbi, lbiB = sm()
        den, denB = sm(); tq, tqB = sm(); qre, qreB = sm(); qim, qimB = sm()
        k.act(dtt, ldt, AF.Exp, [ldtB], [dttB])
        k.tt(mag, are, dtt, ALU.mult, [areB, dttB], [magB])
        k.act(mag, mag, AF.Exp, [magB], [magB])
        k.tt(th, aim, dtt, ALU.mult, [aimB, dttB], [thB])
        k.ts(th, th, 1.0 / (2 * math.pi), ALU.mult, [thB], [thB])
        k.cp(ki, th, [thB], [kiB]); k.cp(kf, ki, [kiB], [kfB])
        k.tt(th, th, kf, ALU.subtract, [thB, kfB], [thB])
        k.act(sh, th, AF.Sin, [thB], [shB], scale=math.pi)
        k.act(kf, th, AF.Abs, [thB], [kfB])
        k.act(chh, kf, AF.Sin, [kfB, halfpiB], [chhB], scale=-math.pi, bias=halfpi)
        k.stt(sn, sh, 2.0, chh, ALU.mult, ALU.mult, [shB, chhB], [snB])
        k.tt(cs_, sh, sh, ALU.mult, [shB], [csB_])
        k.ts(cs_, cs_, -2.0, ALU.mult, [csB_], [csB_], s2=1.0, op1=ALU.add)
        k.tt(lbr, mag, cs_, ALU.mult, [magB, csB_], [lbrB]); k.tt(lbi, mag, sn, ALU.mult, [magB, snB], [lbiB])
        k.tt(den, are, are, ALU.mult, [areB], [denB]); k.tt(tq, aim, aim, ALU.mult, [aimB], [tqB])
        k.tt(den, den, tq, ALU.add, [denB, tqB], [denB])
        S.op("dve", lambda e: e.reciprocal(out=den, in_=den), [denB], [denB])
        k.ts(lbr, lbr, -1.0, ALU.add, [lbrB], [lbrB])
        k.tt(qre, lbr, are, ALU.mult, [lbrB, areB], [qreB]); k.tt(tq, lbi, aim, ALU.mult, [lbiB, aimB], [tqB])
        k.tt(qre, qre, tq, ALU.add, [qreB, tqB], [qreB]); k.tt(qre, qre, den, ALU.mult, [qreB, denB], [qreB])
        k.tt(qim, lbi, are, ALU.mult, [lbiB, areB], [qimB]); k.tt(tq, lbr, aim, ALU.mult, [lbrB, aimB], [tqB])
        k.tt(qim, qim, tq, ALU.subtract, [qimB, tqB], [qimB]); k.tt(qim, qim, den, ALU.mult, [qimB, denB], [qimB])
        Bblk, BblkB = k.sb(stp, "Bblk", [128, 2, 2, 16, 32])
        k.ms(Bblk, 0.0, [BblkB])
        pA, pAB = sm([128, 2, 16, 16]); pB_, pBB = sm([128, 2, 16, 16])
        qreb = qre.unsqueeze(3).broadcast_to([128, 2, 16, 16]); qimb = qim.unsqueeze(3).broadcast_to([128, 2, 16, 16])
        k.tt(pA, bre, qreb, ALU.mult, [breB, qreB], [pAB]); k.tt(pB_, bim, qimb, ALU.mult, [bimB, qimB], [pBB])
        for g2 in range(2):
            hs = slice(g2 * 64, (g2 + 1) * 64)
            k.tt(Bblk[hs, :, 0, :, g2 * 16:(g2 + 1) * 16], pA[hs], pB_[hs], ALU.subtract, [pAB, pBB], [BblkB])
        k.tt(pA, bim, qreb, ALU.mult, [bimB, qreB], [pAB]); k.tt(pB_, bre, qimb, ALU.mult, [breB, qimB], [pBB])
        for g2 in range(2):
            hs = slice(g2 * 64, (g2 + 1) * 64)
            k.tt(Bblk[hs, :, 1, :, g2 * 16:(g2 + 1) * 16], pA[hs], pB_[hs], ALU.add, [pAB, pBB], [BblkB])
        ti_ = 0
        for d in range(2):
            for ri in range(2):
                for c_ in range(4):
                    ps, psB = psb[ti_ % 4]; ti_ += 1
                    k.tr(ps[:, 0:128], Bblk[:, d, ri, 4 * c_:4 * c_ + 4, :].rearrange("p a b -> p (a b)"), ident, [BblkB, identB], [psB])
                    k.cp(BT[:, d, ri, c_, :], ps[:, 0:128], [psB], [BTB])
        k.ms(Cpad, 0.0, [CpadB])
        for g2 in range(2):
            hs = slice(g2 * 64, (g2 + 1) * 64)
            k.cp(Cpad[hs, :, 0, :, g2 * 16:(g2 + 1) * 16], cre[hs], [creB], [CpadB])
            k.ts(Cpad[hs, :, 1, :, g2 * 16:(g2 + 1) * 16], cim[hs], -1.0, ALU.mult, [cimB], [CpadB])
        for gq in range(4):
            S.op("dve", lambda e, gq=gq: e.tensor_reduce(out=maskc[:, gq:gq + 1], in_=ident[:, gq * 32:(gq + 1) * 32], axis=AX.X, op=ALU.add), [identB], [maskcB])
        ta, taB = sm([128, 2, 16, L // 2]); tb, tbB = sm([128, 2, 16, L // 2])
        k.cp(CTAB[:, :, :, 0], cs_, [csB_], [CTABB]); k.cp(STAB[:, :, :, 0], sn, [snB], [STABB])
        m = 1
        while m < L:
            cm = CTAB[:, :, :, m - 1:m].broadcast_to([128, 2, 16, m]); smm = STAB[:, :, :, m - 1:m].broadcast_to([128, 2, 16, m])
            k.tt(ta[:, :, :, :m], CTAB[:, :, :, 0:m], cm, ALU.mult, [CTABB], [taB])
            k.tt(tb[:, :, :, :m], STAB[:, :, :, 0:m], smm, ALU.mult, [STABB], [tbB])
            k.tt(CTAB[:, :, :, m:2 * m], ta[:, :, :, :m], tb[:, :, :, :m], ALU.subtract, [taB, tbB], [CTABB])
            k.tt(ta[:, :, :, :m], STAB[:, :, :, 0:m], cm, ALU.mult, [STABB, CTABB], [taB])
            k.tt(tb[:, :, :, :m], CTAB[:, :, :, 0:m], smm, ALU.mult, [STABB, CTABB], [tbB])
            k.tt(STAB[:, :, :, m:2 * m], ta[:, :, :, :m], tb[:, :, :, :m], ALU.add, [taB, tbB], [STABB])
            m *= 2
        k.ts(nsL, STAB[:, :, :, L - 1], -1.0, ALU.mult, [STABB], [nsLB])
        k.cp(W2[:, :, :, 0], nsL, [nsLB], [W2B]); k.cp(W2[:, :, :, 1], STAB[:, :, :, L - 1], [STABB], [W2B])
        k.dma("sp", dcol, s5_d.rearrange("(j p) -> p j", p=128), dcolB, None, allow_slow_non_contiguous=True)
        S.barrier()
        stp.close()
        ubT, ubTB = k.sb(st, "ubT", [128, TT]); yacc, yaccB = k.sb(st, "yacc", [128, TT])
        NCH = 4
        ch = []
        for gq in range(NCH):
            dct = {nm_: k.sb(st, f"{nm_}{gq}", [128, 512]) for nm_ in ("um", "t1", "t2", "gre", "gim")}
            dct["ss"] = k.sb(st, f"ss{gq}", [128, 2, 512])
            dct["car"] = [k.sb(st, f"car{gq}_{i}", [128, 2]) for i in range(2)]
            dct["tmpc"] = k.sb(st, f"tmpc{gq}", [128, 2])
            dct["rho"] = k.sb(st, f"rho{gq}", [128, L])
            ch.append(dct)
        zero2, zero2B = k.sb(st, "zero2", [128, 2])
        k.ms(zero2, 0.0, [zero2B])
        for c_ in range(4):
            k.dma("sp", ubT, projT[1024 + c_ * 128:1024 + (c_ + 1) * 128, :], ubTB, projTB)
            k.ms(yacc, 0.0, [yaccB])
            for d in range(2):
                bl = blocks_fwd()
                if d == 1:
                    bl = [bl[0]] + bl[1:][::-1]
                for gq in range(NCH):
                    gp = 4 * c_ + gq
                    ch[gq]["cur"] = (zero2, zero2B); ch[gq]["cidx"] = 0
                    k.cp(ch[gq]["rho"][0], mag[:, d, gp:gp + 1].broadcast_to([128, L]), [magB], [ch[gq]["rho"][1]], eng="pool")
                for (c0, n) in bl:
                    nch = n // L
                    v3 = lambda t_: t_[:, :n].rearrange("p (a l) -> p a l", l=L)
                    tabs = []
                    for gq in range(NCH):
                        gp = 4 * c_ + gq
                        cosv = CTAB[:, d, gp, :]; sinv = STAB[:, d, gp, :]
                        if d == 1:
                            cosv = rv(cosv); sinv = rv(sinv)
                        tabs.append((cosv.unsqueeze(1).broadcast_to([128, nch, L]), sinv.unsqueeze(1).broadcast_to([128, nch, L])))
                    for gq in range(NCH):
                        C = ch[gq]; um, umB = C["um"]
                        pdr, pdrB = psb[2 * gq]; pdi, pdiB = psb[2 * gq + 1]
                        k.act(um[:, :n], ubT[:, c0:c0 + n], AF.Copy, [ubTB, maskcB], [umB], scale=maskc[:, gq:gq + 1])
                        k.mm(pdr[:, :n], BT[:, d, 0, c_, :], um[:, :n], True, True, [BTB, umB], [pdrB])
                        k.mm(pdi[:, :n], BT[:, d, 1, c_, :], um[:, :n], True, True, [BTB, umB], [pdiB])
                    for step in range(3):
                        for gq in range(NCH):
                            C = ch[gq]; cosb, sinb = tabs[gq]
                            pdr, pdrB = psb[2 * gq]; pdi, pdiB = psb[2 * gq + 1]
                            t1, t1B_ = C["t1"]; t2, t2B_ = C["t2"]; gre, greB = C["gre"]; gim, gimB = C["gim"]
                            if step == 0:
                                k.tt(v3(t1), v3(pdr), cosb, ALU.mult, [pdrB, CTABB], [t1B_])
                                k.tt(v3(t2), v3(pdi), sinb, ALU.mult, [pdiB, STABB], [t2B_])
                                k.tt(gre[:, :n], t1[:, :n], t2[:, :n], ALU.add, [t1B_, t2B_], [greB], eng="pool")
                            elif step == 1:
                                k.tt(v3(t1), v3(pdi), cosb, ALU.mult, [pdiB, CTABB], [t1B_])
                                k.tt(v3(t2), v3(pdr), sinb, ALU.mult, [pdrB, STABB], [t2B_])
                                k.tt(gim[:, :n], t1[:, :n], t2[:, :n], ALU.subtract, [t1B_, t2B_], [gimB], eng="pool")
                    order = list(range(nch)) if d == 0 else list(range(nch - 1, -1, -1))
                    for ci in order:
                        sl = slice(ci * L, (ci + 1) * L)
                        for op_ in range(4):
                            for gq in range(NCH):
                                C = ch[gq]; gp = 4 * c_ + gq
                                gre, greB = C["gre"]; gim, gimB = C["gim"]; ss, ssB = C["ss"]
                                rho, rhoB = C["rho"]; tmpc, tmpcB = C["tmpc"]; cur = C["cur"]
                                cLc = CTAB[:, d, gp, L - 1:L]
                                li_ = (ci + 1) * L - 1 if d == 0 else ci * L
                                last2 = ss[:, :, li_]
                                f = (lambda a_: a_) if d == 0 else rv
                                if op_ == 0:
                                    k.scan(f(ss[:, 0, sl]), rho, f(gre[:, sl]), cur[0][:, 0:1], [rhoB, greB, cur[1]], [ssB])
                                elif op_ == 1:
                                    k.scan(f(ss[:, 1, sl]), rho, f(gim[:, sl]), cur[0][:, 1:2], [rhoB, gimB, cur[1]], [ssB])
                                elif op_ == 2:
                                    C["nxt"] = C["car"][C["cidx"] % 2]; C["cidx"] += 1
                                    k.tt(tmpc, rv(last2), W2[:, d, gp, :], ALU.mult, [ssB, W2B], [tmpcB])
                                else:
                                    k.stt(C["nxt"][0], last2, cLc, tmpc, ALU.mult, ALU.add, [ssB, CTABB, tmpcB], [C["nxt"][1]])
                                    C["cur"] = C["nxt"]
                    for step in range(2):
                        for gq in range(NCH):
                            C = ch[gq]; cosb, sinb = tabs[gq]
                            e4 = "pool" if gq == 3 else "dve"
                            t1, t1B_ = C["t1"]; t2, t2B_ = C["t2"]; gre, greB = C["gre"]; gim, gimB = C["gim"]
                            ss, ssB = C["ss"]; sre = ss[:, 0, :]; sim = ss[:, 1, :]; sreB = ssB; simB = ssB
                            if step == 0:
                                k.tt(v3(t1), v3(sre), cosb, ALU.mult, [sreB, CTABB], [t1B_], eng=e4)
                                k.tt(v3(t2), v3(sim), sinb, ALU.mult, [simB, STABB], [t2B_], eng=e4)
                                k.tt(gre[:, :n], t1[:, :n], t2[:, :n], ALU.subtract, [t1B_, t2B_], [greB], eng=e4)
                            else:
                                k.tt(v3(t1), v3(sre), sinb, ALU.mult, [sreB, STABB], [t1B_], eng=e4)
                                k.tt(v3(t2), v3(sim), cosb, ALU.mult, [simB, CTABB], [t2B_], eng=e4)
                                k.tt(gim[:, :n], t1[:, :n], t2[:, :n], ALU.add, [t1B_, t2B_], [gimB], eng=e4)
                    cc0 = Cpad[:, d, 0, 4 * c_:4 * c_ + 4, :].rearrange("p a b -> p (a b)")
                    cc1 = Cpad[:, d, 1, 4 * c_:4 * c_ + 4, :].rearrange("p a b -> p (a b)")
                    for gq in range(NCH):
                        C = ch[gq]; gre, greB = C["gre"]; gim, gimB = C["gim"]
                        py, pyB = psb[2 * gq]
                        k.mm(py[:, :n], cc0, gre[:, :n], True, False, [CpadB, greB], [pyB])
                        k.mm(py[:, :n], cc1, gim[:, :n], False, True, [CpadB, gimB], [pyB])
                        k.stt(yacc[:, c0:c0 + n], py[:, :n], maskc[:, gq:gq + 1], yacc[:, c0:c0 + n], ALU.mult, ALU.add,
                              [pyB, maskcB, yaccB], [yaccB])
            k.stt(yacc, ubT, dcol[:, c_:c_ + 1], yacc, ALU.mult, ALU.add, [ubTB, dcolB, yaccB], [yaccB])
            k.act(yacc, yacc, AF.Gelu, [yaccB], [yaccB])
            k.dma("sp", ysgT[c_ * 128:(c_ + 1) * 128, :], yacc, ysgTB, yaccB)
    S.barrier()
    if stop_after <= 3:
        S.finish()
        return k
    def load_wbf(st, name, src2d, kchunks, ncols, q="pool"):
        t, B = k.sb(st, name, [128, kchunks, ncols], BF16)
        k.dma(q, t, src2d.rearrange("(kc p) n -> p kc n", p=128), B, None)
        return t, B

    with ExitStack() as st:
        gw, gwB = load_wbf(st, "gw", s5_glu_w, 4, 512)
        wo, woB = load_wbf(st, "wo", rec_w_out, 8, D)
        glub, glubB = k.sb(st, "glub", [128, 4])
        k.dma("sp", glub, s5_glu_b.rearrange("(j p) -> p j", p=128), glubB, None, allow_slow_non_contiguous=True)
        G1 = [load_mod(st, "sp", 0, kd, 2, f"G1{kd}") for kd in range(2)]
        yaf = [k.sb(st, f"yaf{i}", [128, 4, 512]) for i in range(2)]
        ysf = [k.sb(st, f"ysf{i}", [128, 4, 512]) for i in range(2)]
        yab = [k.sb(st, f"yab{i}", [128, 4, 512], BF16) for i in range(2)]
        ysb = [k.sb(st, f"ysb{i}", [128, 4, 512], BF16) for i in range(2)]
        ys2 = [k.sb(st, f"ys2{i}", [128, 4, 512], BF16) for i in range(2)]
        sg = [k.sb(st, f"sg{i}", [128, 512]) for i in range(2)]
        xt2 = [k.sb(st, f"xt{i}", [128, D]) for i in range(2)]
        ot2 = [k.sb(st, f"ot{i}", [128, D]) for i in range(2)]
        tc_ = 0
        pc_ = 0
        for bi, (c0, n) in enumerate(blocks_fwd()):
            ya_, yaB_ = yaf[bi % 2]; ys_, ysB_ = ysf[bi % 2]; yb_, ybB_ = yab[bi % 2]; sb_, sbB_ = ysb[bi % 2]; y2_, y2B_ = ys2[bi % 2]
            k.dma("sp", ya_[:, :, :n], ymixT[:, c0:c0 + n].rearrange("(j p) t -> p j t", p=128), yaB_, ymixTB)
            k.dma("sp", ys_[:, :, :n], ysgT[:, c0:c0 + n].rearrange("(j p) t -> p j t", p=128), ysB_, ysgTB)
            k.cp(yb_[:, :, :n], ya_[:, :, :n], [yaB_], [ybB_], eng="act")
            k.cp(sb_[:, :, :n], ys_[:, :, :n], [ysB_], [sbB_], eng="pool")
            for oc in range(4):
                ps, psB = psb[pc_ % 8]; pc_ += 1
                for kc in range(4):
                    k.mm(ps[:, :n], gw[:, kc, oc * 128:(oc + 1) * 128], sb_[:, kc, :n], kc == 0, kc == 3, [gwB, sbB_], [psB])
                s_, sB_ = sg[oc % 2]
                k.act(s_[:, :n], ps[:, :n], AF.Sigmoid, [psB, glubB], [sB_], bias=glub[:, oc:oc + 1])
                k.tt(y2_[:, oc, :n], ys_[:, oc, :n], s_[:, :n], ALU.mult, [ysB_, sB_], [y2B_])
            for s in range(n // 128):
                ti = c0 // 128 + s
                xt, xB = xt2[tc_ % 2]; ot, oB = ot2[tc_ % 2]; tc_ += 1
                src, kd = xsrc(ti)
                k.dma("sp", xt, src, xB, None)
                k.flush()
                for half in range(2):
                    ps, psB = psb[pc_ % 8]; pc_ += 1
                    for kc in range(8):
                        lh = yb_[:, kc, s * 128:(s + 1) * 128] if kc < 4 else y2_[:, kc - 4, s * 128:(s + 1) * 128]
                        k.mm(ps, lh, wo[:, kc, half * 512:(half + 1) * 512], kc == 0, kc == 7, [ybB_, y2B_, woB], [psB])
                    k.tt(ot[:, half * 512:(half + 1) * 512], ps, G1[kd][0][:, half * 512:(half + 1) * 512], ALU.mult, [psB, G1[kd][1]], [oB])
                k.tt(ot, ot, xt, ALU.add, [oB, xB], [oB], eng="pool")
                k.defer("sp", x1[ti * 128:(ti + 1) * 128, :], ot, x1B, oB)
        k.flush()
    S.barrier()
    if stop_after <= 4:
        S.finish()
        return k

    def moe(layer, tiles, src_fn, dst_fn, dstB, final):
        with ExitStack() as st:
            kinds = sorted(set(kd for _, kd in tiles))
            A2 = {kd: load_mod(st, "sp", layer, kd, 4, f"A2{kd}") for kd in kinds}
            B2 = {kd: load_mod(st, "sp", layer, kd, 3, f"B2{kd}") for kd in kinds}
            G2 = {kd: load_mod(st, "sp", layer, kd, 5, f"G2{kd}") for kd in kinds}
            Wr, WrB = k.sb(st, "Wr", [128, 8, 36])
            rb, rbB = k.sb(st, "rb", [128, 36])
            k.dma("sp", Wr[:, :, 0:4], moe_r1_w[layer].rearrange("(kc p) g -> p kc g", p=128), WrB, None)
            k.dma("sp", rb[:, 0:4], moe_r1_b[layer, :].partition_broadcast(128), rbB, None)
            for g in range(4):
                k.dma("sp", Wr[:, :, 4 + 8 * g:12 + 8 * g], moe_r2_w[layer, g].rearrange("(kc p) e -> p kc e", p=128), WrB, None)
                k.dma("sp", rb[:, 4 + 8 * g:12 + 8 * g], moe_r2_b[layer, g, :].partition_broadcast(128), rbB, None)
            if final:
                fg, fgB = k.sb(st, "fg", [128, D])
                k.dma("sp", fg, final_norm_g.partition_broadcast(128), fgB, None)
            SUP = 8
            hT, hTB = k.sb(st, "hT", [128, 8, SUP * 128], BF16)
            acc, accB = k.sb(st, "acc", [128, SUP, D])
            gates, gatesB = k.sb(st, "gates", [128, SUP, 32])
            xt2 = [k.sb(st, f"xt{i}", [128, D]) for i in range(2)]
            ht2 = [k.sb(st, f"ht{i}", [128, D]) for i in range(2)]
            jt, jB = k.sb(st, "jt", [128, D])
            ss2 = [k.sb(st, f"ss{i}", [128, 1]) for i in range(2)]
            hTf2 = [k.sb(st, f"hTf{i}", [128, 8, 128]) for i in range(2)]
            wgt = [k.sb(st, f"wgt{i}", [128, 8, 512], BF16) for i in range(2)]
            wut = [k.sb(st, f"wut{i}", [128, 8, 512], BF16) for i in range(2)]
            wdt = [k.sb(st, f"wdt{i}", [128, 4, D], BF16) for i in range(2)]
            sil = [k.sb(st, f"sil{i}", [128, 512]) for i in range(2)]
            hid = [k.sb(st, f"hid{i}", [128, 4, 512], BF16) for i in range(2)]
            lg, lgB = k.sb(st, "lg", [128, 36]); l2m, l2mB = k.sb(st, "l2m", [128, 32])
            sm_ = {nm_: k.sb(st, f"g_{nm_}", [128, 8]) for nm_ in ("m1", "nm1", "e4", "se", "gm", "top", "d12", "w2", "dw")}
            mk1, mk1B = k.sb(st, "mk1", [128, 32]); mk2, mk2B = k.sb(st, "mk2", [128, 32])
            ecount = 0
            pd_i = 0
            for s0 in range(0, len(tiles), SUP):
                sup = tiles[s0:s0 + SUP]
                ns = len(sup)
                for s, (ti, kd) in enumerate(sup):
                    xt, xB = xt2[s % 2]; ht, hB = ht2[s % 2]; ss, ssB = ss2[s % 2]; hTf, hTfB = hTf2[s % 2]
                    src, srcB = src_fn(ti)
                    k.dma("sp", xt, src, xB, srcB)
                    norm_mod(xt, xB, ht, hB, A2[kd][0], A2[kd][1], B2[kd][0], B2[kd][1], (jt, jB, ss, ssB))
                    for b in range(2):
                        ps, psB = psb[(s * 2 + b) % 2]
                        for j in range(4):
                            kc = b * 4 + j
                            k.tr(ps[:, j * 128:(j + 1) * 128], ht[:, kc * 128:(kc + 1) * 128], ident, [hB, identB], [psB])
                        k.cp(hTf[:, b * 4:(b + 1) * 4, :], ps.rearrange("p (j t) -> p j t", j=4), [psB], [hTfB], eng=("act" if b == 0 else "dve"))
                    k.cp(hT[:, :, s * 128:(s + 1) * 128], hTf, [hTfB], [hTB], eng="act")
                    pr, prB = psb[2 + s % 2]
                    for kc in range(8):
                        k.mm(pr[:, 0:36], hTf[:, kc, :], Wr[:, kc, :], kc == 0, kc == 7, [hTfB, WrB], [prB])
                    k.tt(lg, pr[:, 0:36], rb, ALU.add, [prB, rbB], [lgB])
                    m1, m1B = sm_["m1"]; nm1, nm1B = sm_["nm1"]; e4, e4B = sm_["e4"]; se, seB = sm_["se"]; gm, gmB = sm_["gm"]
                    top, topB = sm_["top"]; d12, d12B = sm_["d12"]; w2, w2B = sm_["w2"]; dw, dwB = sm_["dw"]
                    S.op("dve", lambda e, m1=m1: e.tensor_reduce(out=m1[:, 0:1], in_=lg[:, 0:4], axis=AX.X, op=ALU.max), [lgB], [m1B])
                    k.ts(nm1[:, 0:1], m1[:, 0:1], -1.0, ALU.mult, [m1B], [nm1B])
                    k.act(e4[:, 0:4], lg[:, 0:4], AF.Exp, [lgB, nm1B], [e4B, seB], bias=nm1[:, 0:1], accum=se[:, 0:1])
                    S.op("dve", lambda e, se=se: e.reciprocal(out=se[:, 0:1], in_=se[:, 0:1]), [seB], [seB])
                    k.ts(gm[:, 0:4], lg[:, 0:4], m1[:, 0:1], ALU.is_ge, [lgB, m1B], [gmB])
                    k.ts(gm[:, 0:4], gm[:, 0:4], 30000.0, ALU.mult, [gmB], [gmB], s2=-30000.0, op1=ALU.add)
                    k.tt(l2m.rearrange("p (g e) -> p g e", g=4), lg[:, 4:36].rearrange("p (g e) -> p g e", g=4),
                         gm[:, 0:4].unsqueeze(2).broadcast_to([128, 4, 8]), ALU.add, [lgB, gmB], [l2mB])
                    S.op("dve", lambda e, top=top: e.max(out=top[:, 0:8], in_=l2m), [l2mB], [topB])
                    k.tt(d12[:, 0:1], top[:, 1:2], top[:, 0:1], ALU.subtract, [topB], [d12B])
                    k.act(w2[:, 0:1], d12[:, 0:1], AF.Sigmoid, [d12B], [w2B])
                    k.tt(w2[:, 0:1], w2[:, 0:1], se[:, 0:1], ALU.mult, [w2B, seB], [w2B])
                    k.stt(dw[:, 0:1], w2[:, 0:1], -2.0, se[:, 0:1], ALU.mult, ALU.add, [w2B, seB], [dwB])
                    k.ts(mk2, l2m, top[:, 1:2], ALU.is_ge, [l2mB, topB], [mk2B])
                    k.ts(mk1, l2m, top[:, 0:1], ALU.is_ge, [l2mB, topB], [mk1B])
                    k.ts(mk2, mk2, w2[:, 0:1], ALU.mult, [mk2B, w2B], [mk2B])
                    k.stt(gates[:, s, :], mk1, dw[:, 0:1], mk2, ALU.mult, ALU.add, [mk1B, dwB, mk2B], [gatesB])
                grp = [list(range(i, min(i + 4, ns))) for i in range(0, ns, 4)]
                for e_ in range(32):
                    wg_, wgB_ = wgt[ecount % 2]; wu_, wuB_ = wut[ecount % 2]; wd_, wdB_ = wdt[ecount % 2]; ecount += 1
                    k.dma("pool", wg_, moe_w_gate[layer, e_].rearrange("(kc p) f -> p kc f", p=128), wgB_, None)
                    k.dma("pool", wu_, moe_w_up[layer, e_].rearrange("(kc p) f -> p kc f", p=128), wuB_, None)
                    k.dma("pool", wd_, moe_w_down[layer, e_].rearrange("(fc p) n -> p fc n", p=128), wdB_, None)
                    for gi, gt in enumerate(grp):
                        n = 128 * len(gt); cc = gt[0] * 128
                        hd, hdB = hid[gi % 2]
                        for fc in range(4):
                            pg, pgB = psb[fc % 2]; pu, puB = psb[2 + fc % 2]
                            for kc in range(8):
                                k.mm(pg[:, :n], wg_[:, kc, fc * 128:(fc + 1) * 128], hT[:, kc, cc:cc + n], kc == 0, kc == 7, [wgB_, hTB], [pgB])
                            for kc in range(8):
                                k.mm(pu[:, :n], wu_[:, kc, fc * 128:(fc + 1) * 128], hT[:, kc, cc:cc + n], kc == 0, kc == 7, [wuB_, hTB], [puB])
                            sl_, slB = sil[fc % 2]
                            k.act(sl_[:, :n], pg[:, :n], AF.Silu, [pgB], [slB])
                            k.tt(hd[:, fc, :n], sl_[:, :n], pu[:, :n], ALU.mult, [slB, puB], [hdB])
                    for gi, gt in enumerate(grp):
                        hd, hdB = hid[gi % 2]
                        for si, s in enumerate(gt):
                            for half in range(2):
                                pd, pdB = psb[4 + pd_i % 4]; pd_i += 1
                                for fc in range(4):
                                    k.mm(pd, hd[:, fc, si * 128:(si + 1) * 128], wd_[:, fc, half * 512:(half + 1) * 512], fc == 0, fc == 3, [hdB, wdB_], [pdB])
                                av = acc[:, s, half * 512:(half + 1) * 512]
                                if e_ == 0:
                                    k.ts(av, pd, gates[:, s, e_:e_ + 1], ALU.mult, [pdB, gatesB], [accB])
                                else:
                                    k.stt(av, pd, gates[:, s, e_:e_ + 1], av, ALU.mult, ALU.add, [pdB, gatesB, accB], [accB])
                for s, (ti, kd) in enumerate(sup):
                    xt, xB = xt2[s % 2]; ot, oB = ht2[s % 2]; ss, ssB = ss2[s % 2]
                    src, srcB = src_fn(ti)
                    k.dma("sp", xt, src, xB, srcB)
                    k.flush()
                    k.tt(ot, acc[:, s, :], G2[kd][0], ALU.mult, [accB, G2[kd][1]], [oB])
                    k.tt(ot, ot, xt, ALU.add, [oB, xB], [oB])
                    if final:
                        k.act(jt, ot, AF.Square, [oB], [jB, ssB], accum=ss)
                        k.act(ss, ss, AF.Sqrt, [ssB, epsB], [ssB], scale=1.0 / D, bias=epsb)
                        S.op("dve", lambda e, ss=ss: e.reciprocal(out=ss, in_=ss), [ssB], [ssB])
                        k.stt(ot, ot, ss, fg, ALU.mult, ALU.mult, [oB, ssB, fgB], [oB])
                    k.defer("sp", dst_fn(ti), ot, dstB, oB)
                k.flush()
        S.barrier()

    moe(0, [(ti, 1 if ti < 2 else 0) for ti in range(NT)], lambda ti: (x1[ti * 128:(ti + 1) * 128, :], x1B),
        lambda ti: x2[ti * 128:(ti + 1) * 128, :], x2B, False)
    if stop_after <= 5:
        S.finish()
        return k
    NTL = T // 128
    ROWS = T // 64
    QT, QTB = k.dscr("QT", [D, T], BF16)
    KT, KTB = k.dscr("KT", [D, TT], BF16)
    VA, VAB = k.dscr("VA", [TT, 16, 65], BF16)
    AO, AOB = k.dscr("AO", [T, D])
    REV, REVB = k.dscr("REV", [16, 15, 128])
    with ExitStack() as st:
        wq, wqB = load_wbf(st, "wqkv", na_w_qkv, 8, 3 * D)
        AA = [load_mod(st, "sp", 1, kd, 1, f"A{kd}") for kd in range(2)]
        BBm = [load_mod(st, "sp", 1, kd, 0, f"B{kd}") for kd in range(2)]
        xt2 = [k.sb(st, f"xt{i}", [128, D]) for i in range(2)]
        ht2 = [k.sb(st, f"ht{i}", [128, D]) for i in range(2)]
        jt, jB = k.sb(st, "jt", [128, D])
        ss2 = [k.sb(st, f"ss{i}", [128, 1]) for i in range(2)]
        hT2 = [k.sb(st, f"hT{i}", [128, 8, 512], BF16) for i in range(2)]
        qst2 = [k.sb(st, f"qst{i}", [128, 8, 512], BF16) for i in range(2)]
        kst2 = [k.sb(st, f"kst{i}", [128, 8, 512], BF16) for i in range(2)]
        vst2 = [k.sb(st, f"vst{i}", [128, 16, 65], BF16) for i in range(2)]
        for i in range(2):
            k.ms(vst2[i][0], 1.0, [vst2[i][1]])
        tcount = 0; pc_ = 0
        for gi, g in enumerate(groups_of(list(range(NT)))):
            hT, hTB = hT2[gi % 2]
            n = 128 * len(g); col0 = g[0] * 128
            isctx = g[0] < 2
            for si, ti in enumerate(g):
                xt, xB = xt2[tcount % 2]; ht, hB = ht2[tcount % 2]; ss, ssB = ss2[tcount % 2]
                kd = 1 if ti < 2 else 0
                k.dma("sp", xt, x2[ti * 128:(ti + 1) * 128, :], xB, x2B)
                norm_mod(xt, xB, ht, hB, AA[kd][0], AA[kd][1], BBm[kd][0], BBm[kd][1], (jt, jB, ss, ssB))
                for b in range(2):
                    ps, psB = psb[(tcount * 2 + b) % 2]
                    for j in range(4):
                        kc = b * 4 + j
                        k.tr(ps[:, j * 128:(j + 1) * 128], ht[:, kc * 128:(kc + 1) * 128], ident, [hB, identB], [psB])
                    k.cp(hT[:, b * 4:(b + 1) * 4, si * 128:(si + 1) * 128], ps.rearrange("p (j t) -> p j t", j=4),
                         [psB], [hTB], eng=("act" if b == 0 else "dve"))
                tcount += 1
            qst, qstB = qst2[gi % 2]; kst, kstB = kst2[gi % 2]
            for oc in range(16):
                if isctx and oc < 8:
                    continue
                ps, psB = psb[2 + pc_ % 3]; pc_ += 1
                for kc in range(8):
                    k.mm(ps[:, :n], wq[:, kc, oc * 128:(oc + 1) * 128], hT[:, kc, :n], kc == 0, kc == 7, [wqB, hTB], [psB])
                if oc < 8:
                    k.act(qst[:, oc, :n], ps[:, :n], AF.Copy, [psB], [qstB], scale=0.125)
                else:
                    k.cp(kst[:, oc - 8, :n], ps[:, :n], [psB], [kstB])
            if not isctx:
                k.dma("sp", QT[:, col0 - CT:col0 - CT + n].rearrange("(oc p) t -> p oc t", p=128), qst[:, :, :n], QTB, qstB)
            k.dma("sp", KT[:, col0:col0 + n].rearrange("(oc p) t -> p oc t", p=128), kst[:, :, :n], KTB, kstB)
            for si, ti in enumerate(g):
                vst, vstB = vst2[ti % 2]
                for half in range(2):
                    ps, psB = psb[5 + pc_ % 3]; pc_ += 1
                    for kc in range(8):
                        k.mm(ps, hT[:, kc, si * 128:(si + 1) * 128], wq[:, kc, 2 * D + half * 512:2 * D + (half + 1) * 512], kc == 0, kc == 7, [wqB, hTB], [psB])
                    k.cp(vst[:, half * 8:(half + 1) * 8, 0:64], ps.rearrange("p (h e) -> p h e", h=8), [psB], [vstB], eng=("act" if half == 0 else "dve"))
                k.dma("sp", VA[ti * 128:(ti + 1) * 128, :, :], vst, VAB, vstB)
    S.barrier()
    if stop_after <= 6:
        S.finish()
        return k

    with ExitStack() as st:
        T2, T2B = k.sb(st, "T2", [128, 14, 16, 64])
        zt, ztB = k.sb(st, "zt", [128, 256])
        k.ms(zt, 0.0, [ztB])
        k.dma("sp", REV.rearrange("h d j -> (h d j)").rearrange("(p f) -> p f", p=120), zt[0:120, :], REVB, ztB)
        rpt, rptB = k.sb(st, "rpt", [16, 15, 31])
        k.dma("sp", rpt, na_rpb, rptB, None)
        k.dma("sp", REV[:, :, 48:79], rpt, REVB, rptB)
        with ExitStack() as st2:
            T2r, T2rB = k.sb(st2, "T2r", [128, 7, 16, 64])
            for hf in range(2):
                for dl in range(2):
                    for a_ in range(7):
                        drb = hf * 7 + a_
                        src = bass.AP(REV.tensor, REV.offset + (drb + dl) * 128, [[1, 64], [15 * 128, 16], [1, 64]])
                        k.dma("sp", T2r[dl * 64:(dl + 1) * 64, a_, :, :], src, T2rB, REVB)
                k.cp(T2[:, hf * 7:(hf + 1) * 7, :, :].rearrange("p a h q -> p (a h) q"),
                     rv(T2r.rearrange("p a h q -> p (a h) q")), [T2rB], [T2B], eng="dve")
        S.barrier()
        Mt, MtB = k.sb(st, "Mt", [128, 64])
        k.ms(Mt, 0.0, [MtB])
        NEG = -30000.0
        for dl in range(2):
            hs = slice(dl * 64, (dl + 1) * 64)
            sels = [(slice(0, 8), [[0, 8]], 15, -1), (slice(8, 57), [[-1, 49]], 0, 1), (slice(8, 57), [[1, 49]], 15, -1),
                    (slice(57, 64), [[0, 7]], -48, 1)]
            for (qs, pat, base, cm) in sels:
                S.op("pool", lambda e, hs=hs, qs=qs, pat=pat, base=base, cm=cm: e.affine_select(
                    out=Mt[hs, qs], in_=Mt[hs, qs], pattern=pat, compare_op=ALU.is_ge, fill=NEG, base=base, channel_multiplier=cm), [MtB], [MtB])
        T2v = T2.rearrange("p a h q -> p (a h) q")
        k.tt(T2v, T2v, Mt.unsqueeze(1).broadcast_to([128, 224, 64]), ALU.add, [T2B, MtB], [T2B])
        if "T2d" in k.dbg:
            T2d, T2dB = k.dscr("T2d", [128, 14 * 16 * 64])
            k.dma("sp", T2d, T2.rearrange("p a h q -> p (a h q)"), T2dB, T2B)
        KcT, KcTB = k.sb(st, "KcT", [128, 8, 256], BF16)
        Vc, VcB = k.sb(st, "Vc", [128, 2, 1040], BF16)
        k.dma("sp", KcT, KT[:, 0:256].rearrange("(c p) t -> p c t", p=128), KcTB, KTB)
        k.dma("sp", Vc, VA[0:256].rearrange("(a p) h e -> p a (h e)", p=128), VcB, VAB)
        qbd2 = [k.sb(st, f"qbd{i}", [128, 8, 128], BF16) for i in range(2)]
        for i in range(2):
            k.ms(qbd2[i][0], 0.0, [qbd2[i][1]])
        kT2 = [k.sb(st, f"kT{i}", [128, 8, 512], BF16) for i in range(2)]
        vv2 = [k.sb(st, f"vv{i}", [128, 4, 1040], BF16) for i in range(2)]
        sb2 = [k.sb(st, f"sbt{i}", [128, 1024]) for i in range(2)]
        pT2 = [k.sb(st, f"pT{i}", [128, 1024], BF16) for i in range(2)]
        ao2 = [k.sb(st, f"ao{i}", [128, 8, 64]) for i in range(2)]
        rec2 = [k.sb(st, f"rec{i}", [128, 8]) for i in range(2)]
        ci_ = 0
        P7 = int(os.environ.get('P7', '9'))
        def row_loads(r):
            rs_ = min(max(r - 4, 0), ROWS - 8)
            qbd, qbdB = qbd2[r % 2]; kT, kTB = kT2[r % 2]; vv, vvB = vv2[r % 2]
            qsrc = QT[:, r * 64:(r + 1) * 64].rearrange("(c p) t -> p c t", p=128)
            k.dma("sp", qbd[0:64, :, 0:64], qsrc[0:64], qbdB, QTB)
            k.dma("sp", qbd[64:128, :, 64:128], qsrc[64:128], qbdB, QTB)
            k0 = CT + rs_ * 64
            k.dma("sp", kT, KT[:, k0:k0 + 512].rearrange("(c p) t -> p c t", p=128), kTB, KTB)
            k.dma("sp", vv, VA[k0:k0 + 512].rearrange("(a p) h e -> p a (h e)", p=128), vvB, VAB)
        if P7 >= 1:
            row_loads(0)
        for r in range(ROWS if P7 >= 1 else 0):
            rs_ = min(max(r - 4, 0), ROWS - 8)
            qbd, qbdB = qbd2[r % 2]; kT, kTB = kT2[r % 2]; vv, vvB = vv2[r % 2]
            if r + 1 < ROWS:
                row_loads(r + 1)
            for kc in range(6):
                sbank = [psb[(ci_ % 2) * 2], psb[(ci_ % 2) * 2 + 1]]
                sbt, sbtB = sb2[ci_ % 2]; pT, pTB = pT2[ci_ % 2]; ci_ += 1
                for c in range(8):
                    if kc < 4:
                        lh = kT[:, c, kc * 128:(kc + 1) * 128]; lB = kTB
                    else:
                        lh = KcT[:, c, (kc - 4) * 128:(kc - 3) * 128]; lB = KcTB
                    sp_, spB = sbank[c // 4]
                    k.mm(sp_[:, (c % 4) * 128:(c % 4 + 1) * 128], lh, qbd[:, c, :], True, True, [lB, qbdB], [spB])
                if kc < 4:
                    drb = 2 * kc + (rs_ - r + 7)
                    for b in range(2):
                        k.tt(sbt[:, b * 512:(b + 1) * 512], sbank[b][0], T2[:, drb, b * 8:(b + 1) * 8, :].rearrange("p h q -> p (h q)"),
                             ALU.add, [sbank[b][1], T2B], [sbtB])
                    k.act(pT, sbt, AF.Exp, [sbtB], [pTB])
                    vsrc = vv[:, kc, :]; vB_ = vvB
                else:
                    for b in range(2):
                        k.act(pT[:, b * 512:(b + 1) * 512], sbank[b][0], AF.Exp, [sbank[b][1]], [pTB])
                    vsrc = Vc[:, kc - 4, :]; vB_ = VcB
                for c in range(8 if P7 >= 2 else 0):
                    ob, obB = psb[4 + c // 3]
                    k.mm(ob[:, (c % 3) * 130:(c % 3 + 1) * 130], pT[:, c * 128:(c + 1) * 128], vsrc[:, c * 130:(c + 1) * 130],
                         (kc == 0 and c % 3 == 0), kc == 5, [pTB, vB_], [obB], skip=True)
            ao, aoB = ao2[r % 2]; rec, recB = rec2[r % 2]
            for b in range(3 if P7 >= 3 else 0):
                np_ = 3 if b < 2 else 2
                ob, obB = psb[4 + b]
                for par in range(2):
                    ps_ = slice(par * 64, (par + 1) * 64)
                    ov = ob[ps_, 0:np_ * 130].rearrange("p (c e) -> p c e", e=130)
                    S.op("dve", lambda e, rec=rec, ps_=ps_, b=b, np_=np_, ov=ov, par=par: e.reciprocal(
                        out=rec[ps_, b * 3:b * 3 + np_], in_=ov[:, :, par * 65 + 64]), [obB], [recB])
                    k.tt(ao[ps_, b * 3:b * 3 + np_, :], ov[:, :, par * 65:par * 65 + 64],
                         rec[ps_, b * 3:b * 3 + np_].unsqueeze(2).broadcast_to([64, np_, 64]), ALU.mult, [obB, recB], [aoB])
            if P7 >= 3:
                dstv = AO[r * 64:(r + 1) * 64, :].rearrange("q (c p e) -> q c p e", p=2, e=64)
                for par in range(2):
                    k.dma("sp", dstv[:, :, par, :], ao[par * 64:(par + 1) * 64, :, :], AOB, aoB)
    S.barrier()
    if stop_after <= 7:
        S.finish()
        return k

    with ExitStack() as st:
        wo, woB = load_wbf(st, "wo1", na_w_out, 8, D)
        G1, G1B = load_mod(st, "sp", 1, 0, 2, "G1l1")
        at2 = [k.sb(st, f"at{i}", [128, D]) for i in range(2)]
        xt2 = [k.sb(st, f"xt{i}", [128, D]) for i in range(2)]
        ot2 = [k.sb(st, f"ot{i}", [128, D]) for i in range(2)]
        aT2 = [k.sb(st, f"aT{i}", [128, 8, 128], BF16) for i in range(2)]
        for tl in range(NTL):
            at_, atB = at2[tl % 2]; xt, xB = xt2[tl % 2]; ot, oB = ot2[tl % 2]; aT, aTB = aT2[tl % 2]
            k.dma("sp", at_, AO[tl * 128:(tl + 1) * 128, :], atB, AOB)
            k.dma("sp", xt, x2[CT + tl * 128:CT + (tl + 1) * 128, :], xB, x2B)
            k.flush()
            for b in range(2):
                ps, psB = psb[(tl * 2 + b) % 4]
                for j in range(4):
                    kc = b * 4 + j
                    k.tr(ps[:, j * 128:(j + 1) * 128], at_[:, kc * 128:(kc + 1) * 128], ident, [atB, identB], [psB])
                k.cp(aT[:, b * 4:(b + 1) * 4, :], ps.rearrange("p (j t) -> p j t", j=4), [psB], [aTB], eng=("act" if b == 0 else "dve"))
            for half in range(2):
                ps, psB = psb[4 + (tl * 2 + half) % 4]
                for kc in range(8):
                    k.mm(ps, aT[:, kc, :], wo[:, kc, half * 512:(half + 1) * 512], kc == 0, kc == 7, [aTB, woB], [psB])
                k.tt(ot[:, half * 512:(half + 1) * 512], ps, G1[:, half * 512:(half + 1) * 512], ALU.mult, [psB, G1B], [oB])
            k.tt(ot, ot, xt, ALU.add, [oB, xB], [oB], eng="pool")
            k.defer("sp", x3[tl * 128:(tl + 1) * 128, :], ot, x3B, oB)
        k.flush()
    S.barrier()
    if stop_after <= 8:
        S.finish()
        return k
    moe(1, [(tl, 0) for tl in range(NTL)], lambda tl: (x3[tl * 128:(tl + 1) * 128, :], x3B),
        lambda tl: out[tl * 128:(tl + 1) * 128, :], obuf, True)
    S.finish()
    return k


_CACHE = {}


def kernel(**inputs):
    T = inputs["x"].shape[1]
    if T not in _CACHE:
        _CACHE[T] = build(T)
    kb = _CACHE[T]
    nb = inputs["x"].shape[0]
    squeeze = ("rec_", "lru_", "s5_", "na_")
    shared = {}
    for name, v in inputs.items():
        if name in ("x", "c", "ctx"):
            continue
        a = np.asarray(v, dtype=np.float32)
        if name.startswith(squeeze):
            a = a[0]
        shared[name] = np.ascontiguousarray(a)
    in_maps = []
    for b in range(nb):
        m = dict(shared)
        m["x"] = np.ascontiguousarray(np.asarray(inputs["x"][b], dtype=np.float32))
        m["c"] = np.ascontiguousarray(np.asarray(inputs["c"][b], dtype=np.float32))
        m["ctx"] = np.ascontiguousarray(np.asarray(inputs["ctx"][b], dtype=np.float32))
        in_maps.append({kk: vv for kk, vv in m.items() if kk in kb.inp})
    res = run_bass_kernel_spmd(kb.nc, in_maps, core_ids=list(range(nb)))
    return np.stack([np.asarray(res.results[b]["out"]) for b in range(nb)], axis=0).astype(np.float32)
```
